# Optimizing a Trainium2 kernel written in Bass

```python
import math
import jax, jax.numpy as jnp
from jax import lax
import numpy as np

D_MODEL = 1024
BATCH = 16
SEQ = 4096
DEPTH = 1

GMLP_WIDTH = 512
GMLP_GROUPS = 8
GMLP_HEAD = GMLP_WIDTH // GMLP_GROUPS
CHUNK = 128
S5_WIDTH = 256
S5_GROUP_CH = 16
S5_GROUPS = S5_WIDTH // S5_GROUP_CH
S5_STATE = 64
P_TOTAL = 2 * GMLP_WIDTH + S5_WIDTH + 2 * D_MODEL
N_EXPERTS = 16
CAPACITY_FACTOR = 2
EXPERT_FF = 2048
EPS = 1e-6

kernel_name = "hybrid_gmlp_s5_expert_choice_encoder_block"


def rms_norm(x, g):
    xf = x.astype(jnp.float32)
    r = lax.rsqrt(jnp.mean(xf * xf, axis=-1, keepdims=True) + EPS)
    return (xf * r * g.astype(jnp.float32)).astype(x.dtype)


def gmlp_spatial_gating(u, v, ln_g, ln_b, w_s, b_s):
    bn, seq, _ = u.shape
    vf = v.astype(jnp.float32)
    mu = jnp.mean(vf, axis=-1, keepdims=True)
    var = jnp.mean(jnp.square(vf - mu), axis=-1, keepdims=True)
    vn = (vf - mu) * lax.rsqrt(var + EPS) * ln_g.astype(jnp.float32) + ln_b.astype(jnp.float32)
    vc = vn.reshape(bn, seq // CHUNK, CHUNK, GMLP_GROUPS, GMLP_HEAD)
    z = jnp.einsum('gts,bnsgc->bntgc', w_s.astype(jnp.float32), vc) \
        + jnp.transpose(b_s.astype(jnp.float32))[:, :, None]
    return u * z.reshape(bn, seq, GMLP_WIDTH).astype(u.dtype)


def _ssm_combine(e1, e2):
    a1, b1 = e1
    a2, b2 = e2
    return a1 * a2, a2 * b1 + b2


def s5_direction(u, lam_re, lam_im, log_dt, b_re, b_im, c_re, c_im, reverse):
    f32 = jnp.float32
    lam = lax.complex(lam_re.astype(f32), lam_im.astype(f32))
    dt = jnp.exp(log_dt.astype(f32))[:, None]
    lam_bar = jnp.exp(lam * dt)
    bmat = lax.complex(b_re.astype(f32), b_im.astype(f32))
    b_bar = ((lam_bar - 1.0) / lam)[:, :, None] * bmat
    bu = jnp.einsum('gph,blgh->blgp', b_bar, u.astype(jnp.complex64))
    a = jnp.broadcast_to(lam_bar, bu.shape)
    _, states = lax.associative_scan(_ssm_combine, (a, bu), axis=1, reverse=reverse)
    cmat = lax.complex(c_re.astype(f32), c_im.astype(f32))
    return jnp.real(jnp.einsum('ghp,blgp->blgh', cmat, states))


def s5_mixer(s, lam_re, lam_im, log_dt, b_re, b_im, c_re, c_im, d_skip, w_glu, b_glu):
    bn, seq, _ = s.shape
    sf = s.astype(jnp.float32).reshape(bn, seq, S5_GROUPS, S5_GROUP_CH)
    y = (s5_direction(sf, lam_re[0], lam_im[0], log_dt[0], b_re[0], b_im[0], c_re[0], c_im[0], False)
         + s5_direction(sf, lam_re[1], lam_im[1], log_dt[1], b_re[1], b_im[1], c_re[1], c_im[1], True)
         + d_skip.astype(jnp.float32) * sf)
    y = jax.nn.gelu(y.reshape(bn, seq, S5_WIDTH), approximate=False)
    y = y * jax.nn.sigmoid(y @ w_glu.astype(jnp.float32) + b_glu.astype(jnp.float32))
    return y.astype(s.dtype)


def expert_choice_ffn(h, w_router, w_gate, w_up, w_down):
    bn, seq, dm = h.shape
    cap = CAPACITY_FACTOR * seq // N_EXPERTS
    probs = jax.nn.softmax(jnp.einsum('bld,de->ble', h, w_router).astype(jnp.float32), axis=-1)
    scores = jnp.transpose(probs, (0, 2, 1))
    gate, idx = lax.top_k(scores, cap)
    xs = jax.vmap(lambda hb, ib: hb[ib])(h, idx)
    a = jnp.einsum('becd,edf->becf', xs, w_gate)
    u = jnp.einsum('becd,edf->becf', xs, w_up)
    y = jnp.einsum('becf,efd->becd', jax.nn.silu(a) * u, w_down)
    y = (y * gate[..., None]).astype(h.dtype)
    seg = (idx + (jnp.arange(bn, dtype=jnp.int32) * seq)[:, None, None]).reshape(-1)
    out = jax.ops.segment_sum(y.reshape(-1, dm), seg, num_segments=bn * seq)
    return out.reshape(bn, seq, dm)


def setup_inputs(seed: int = 0) -> dict:
    key = jax.random.key(seed)
    ks = jax.random.split(key, 32)
    f32 = jnp.float32
    nrm = lambda k, shape, scale: (jax.random.normal(k, shape, f32) * scale)
    L_ = DEPTH
    x = jax.random.normal(ks[0], (BATCH, SEQ, D_MODEL), f32)
    norm1_g = 1.0 + nrm(ks[1], (L_, D_MODEL), 0.02)
    w_in = nrm(ks[2], (L_, D_MODEL, P_TOTAL), D_MODEL ** -0.5)
    b_gate = nrm(ks[3], (L_, 2 * D_MODEL), 0.02)
    gmlp_ln_g = 1.0 + nrm(ks[4], (L_, GMLP_WIDTH), 0.02)
    gmlp_ln_b = nrm(ks[5], (L_, GMLP_WIDTH), 0.02)
    gmlp_w_s = nrm(ks[6], (L_, GMLP_GROUPS, CHUNK, CHUNK), CHUNK ** -0.5)
    gmlp_b_s = 1.0 + nrm(ks[7], (L_, GMLP_GROUPS, CHUNK), 0.1)
    dshape = (L_, 2, S5_GROUPS, S5_STATE)
    s5_lam_re = -0.5 + nrm(ks[8], dshape, 0.01)
    n_idx = jnp.arange(S5_STATE, dtype=f32)
    s5_lam_im = jnp.pi * n_idx + nrm(ks[9], dshape, 0.01)
    s5_log_dt = jax.random.uniform(ks[10], (L_, 2, S5_GROUPS), f32,
                                   minval=math.log(0.001), maxval=math.log(0.1))
    bscale = (2.0 * S5_GROUP_CH) ** -0.5
    s5_b_re = nrm(ks[11], (L_, 2, S5_GROUPS, S5_STATE, S5_GROUP_CH), bscale)
    s5_b_im = nrm(ks[12], (L_, 2, S5_GROUPS, S5_STATE, S5_GROUP_CH), bscale)
    cscale = (2.0 * S5_STATE) ** -0.5
    s5_c_re = nrm(ks[13], (L_, 2, S5_GROUPS, S5_GROUP_CH, S5_STATE), cscale)
    s5_c_im = nrm(ks[14], (L_, 2, S5_GROUPS, S5_GROUP_CH, S5_STATE), cscale)
    s5_d = nrm(ks[15], (L_, S5_GROUPS, S5_GROUP_CH), 1.0)
    s5_w_glu = nrm(ks[16], (L_, S5_WIDTH, S5_WIDTH), S5_WIDTH ** -0.5)
    s5_b_glu = nrm(ks[17], (L_, S5_WIDTH), 0.02)
    w_up_a = nrm(ks[18], (L_, GMLP_WIDTH, D_MODEL), GMLP_WIDTH ** -0.5)
    w_up_b = nrm(ks[19], (L_, S5_WIDTH, D_MODEL), S5_WIDTH ** -0.5)
    w_out = nrm(ks[20], (L_, D_MODEL, D_MODEL), D_MODEL ** -0.5)
    norm2_g = 1.0 + nrm(ks[21], (L_, D_MODEL), 0.02)
    w_router = nrm(ks[22], (L_, D_MODEL, N_EXPERTS), D_MODEL ** -0.5)
    w_gate = nrm(ks[23], (L_, N_EXPERTS, D_MODEL, EXPERT_FF), D_MODEL ** -0.5)
    w_up = nrm(ks[24], (L_, N_EXPERTS, D_MODEL, EXPERT_FF), D_MODEL ** -0.5)
    w_down = nrm(ks[25], (L_, N_EXPERTS, EXPERT_FF, D_MODEL), EXPERT_FF ** -0.5)
    final_g = 1.0 + nrm(ks[26], (D_MODEL,), 0.02)
    return {"x": x, "norm1_g": norm1_g, "w_in": w_in, "b_gate": b_gate,
            "gmlp_ln_g": gmlp_ln_g, "gmlp_ln_b": gmlp_ln_b, "gmlp_w_s": gmlp_w_s, "gmlp_b_s": gmlp_b_s,
            "s5_lam_re": s5_lam_re, "s5_lam_im": s5_lam_im, "s5_log_dt": s5_log_dt,
            "s5_b_re": s5_b_re, "s5_b_im": s5_b_im, "s5_c_re": s5_c_re, "s5_c_im": s5_c_im,
            "s5_d": s5_d, "s5_w_glu": s5_w_glu, "s5_b_glu": s5_b_glu,
            "w_up_a": w_up_a, "w_up_b": w_up_b, "w_out": w_out, "norm2_g": norm2_g,
            "w_router": w_router, "w_gate": w_gate, "w_up": w_up, "w_down": w_down,
            "final_g": final_g}


def reference(x, norm1_g, w_in, b_gate, gmlp_ln_g, gmlp_ln_b, gmlp_w_s, gmlp_b_s,
              s5_lam_re, s5_lam_im, s5_log_dt, s5_b_re, s5_b_im, s5_c_re, s5_c_im,
              s5_d, s5_w_glu, s5_b_glu, w_up_a, w_up_b, w_out, norm2_g,
              w_router, w_gate, w_up, w_down, final_g):
    o_s5 = 2 * GMLP_WIDTH
    o_gate = o_s5 + S5_WIDTH
    for l in range(DEPTH):
        h = rms_norm(x, norm1_g[l])
        p = h @ w_in[l]
        uv = jax.nn.gelu(p[..., :o_s5], approximate=False)
        s_in = p[..., o_s5:o_gate]
        gates = jax.nn.sigmoid(p[..., o_gate:] + b_gate[l])
        g_a, g_b = gates[..., :D_MODEL], gates[..., D_MODEL:]
        br_a = gmlp_spatial_gating(uv[..., :GMLP_WIDTH], uv[..., GMLP_WIDTH:],
                                   gmlp_ln_g[l], gmlp_ln_b[l], gmlp_w_s[l], gmlp_b_s[l])
        br_b = s5_mixer(s_in, s5_lam_re[l], s5_lam_im[l], s5_log_dt[l], s5_b_re[l], s5_b_im[l],
                        s5_c_re[l], s5_c_im[l], s5_d[l], s5_w_glu[l], s5_b_glu[l])
        merged = g_a * (br_a @ w_up_a[l]) + g_b * (br_b @ w_up_b[l])
        x = x + (merged @ w_out[l]).astype(x.dtype)
        h2 = rms_norm(x, norm2_g[l])
        x = x + expert_choice_ffn(h2, w_router[l], w_gate[l], w_up[l], w_down[l]).astype(x.dtype)
    return rms_norm(x, final_g)
```

```python
import contextlib
import math
import numpy as np
import concourse.bass as bass
import concourse.mybir as mybir
from concourse.bass_utils import run_bass_kernel_spmd

F32 = mybir.dt.float32
BF16 = mybir.dt.bfloat16
I32 = mybir.dt.int32
I16 = mybir.dt.int16
AF = mybir.ActivationFunctionType
ALU = mybir.AluOpType
AX = mybir.AxisListType

D = 1024
KD = 8
GW = 512
S5W = 256
PT = 3328
NE = 16
FF = 2048
EPS = 1e-6
TB = 512
GT = 512
SB_BASE = 16512
SB_END = 229376

ENGS = ("pe", "act", "dve", "pool", "sp")
NDMA_SEMS = 8


class Op:
    __slots__ = ("id", "eng", "fn", "deps", "dma", "signal", "seq", "sem_idx", "waits", "prewait")

    def __init__(self, id, eng, fn, dma):
        self.id = id
        self.eng = eng
        self.fn = fn
        self.dma = dma
        self.deps = set()
        self.signal = False
        self.seq = 0
        self.sem_idx = -1
        self.waits = []
        self.prewait = None


class Sched:
    def __init__(self, nc):
        self.nc = nc
        self.ops = []
        self.last_writer = {}
        self.readers = {}
        self.last_on_eng = {}

    def add(self, eng, fn, reads=(), writes=(), dma=False):
        op = Op(len(self.ops), eng, fn, dma)
        deps = op.deps
        for t in reads:
            w = self.last_writer.get(t)
            if w is not None:
                deps.add(w)
            if t.startswith("bank"):
                for r in self.readers.get(t, ()):
                    if self.ops[r].eng != eng:
                        deps.add(r)
        for t in writes:
            w = self.last_writer.get(t)
            if w is not None:
                deps.add(w)
            for r in self.readers.get(t, ()):
                deps.add(r)
        for t in reads:
            self.readers.setdefault(t, []).append(op.id)
        for t in writes:
            self.last_writer[t] = op.id
            self.readers[t] = []
        deps.discard(op.id)
        self.ops.append(op)
        self.last_on_eng[eng] = op.id
        return op

    def pe(self, fn, reads=(), writes=()):
        return self.add("pe", fn, reads, writes)

    def act(self, fn, reads=(), writes=()):
        return self.add("act", fn, reads, writes)

    def dve(self, fn, reads=(), writes=()):
        return self.add("dve", fn, reads, writes)

    def pool(self, fn, reads=(), writes=()):
        return self.add("pool", fn, reads, writes)

    def any(self, eng, fn, reads=(), writes=()):
        return self.add(eng, fn, reads, writes)

    def dma(self, eng, fn, reads=(), writes=()):
        return self.add(eng, fn, reads, writes, dma=True)

    def barrier(self):
        pend = set()
        for t, w in self.last_writer.items():
            pend.add(w)
        for t, rs in self.readers.items():
            pend.update(rs)
        pend.update(self.last_on_eng.values())
        for op in self.ops:
            if op.dma:
                pend.add(op.id)
        ids = []
        for e in ENGS:
            op = Op(len(self.ops), e, (lambda eng: eng.nop()), False)
            op.deps = set(pend)
            self.ops.append(op)
            self.last_on_eng[e] = op.id
            ids.append(op.id)
        self.last_writer = {}
        self.readers = {}
        self._dma_done_upto = len(self.ops)

    def emit(self, final_wait_eng="sp"):
        nc = self.nc
        ops = self.ops

        def skip(dop, op):
            return dop.eng == "pe" and op.eng == "pe" and not dop.dma and not op.dma

        for op in ops:
            for d in op.deps:
                dop = ops[d]
                if skip(dop, op):
                    continue
                dop.signal = True
        for op in ops:
            if op.dma:
                op.signal = True
        eng_cnt = {e: 0 for e in ENGS}
        dma_cnt = {e: [0] * NDMA_SEMS for e in ENGS}
        dma_rr = {e: 0 for e in ENGS}
        for op in ops:
            if op.dma:
                k = dma_rr[op.eng] % NDMA_SEMS
                dma_rr[op.eng] += 1
                op.sem_idx = k
                op.prewait = dma_cnt[op.eng][k]
                dma_cnt[op.eng][k] += 16
                op.seq = dma_cnt[op.eng][k]
            elif op.signal:
                eng_cnt[op.eng] += 1
                op.seq = eng_cnt[op.eng]
        waited = {e: {} for e in ENGS}
        for op in ops:
            w = waited[op.eng]
            need = {}
            for d in op.deps:
                dop = ops[d]
                if skip(dop, op):
                    continue
                key = ("d", dop.eng, dop.sem_idx) if dop.dma else ("e", dop.eng)
                if dop.seq > need.get(key, 0):
                    need[key] = dop.seq
            if op.dma and op.prewait:
                key = ("d", op.eng, op.sem_idx)
                if op.prewait > need.get(key, 0):
                    need[key] = op.prewait
            for key, v in need.items():
                if w.get(key, 0) >= v:
                    continue
                w[key] = v
                op.waits.append((key, v))
        self.stats = dict(n_ops=len(ops), eng_cnt=dict(eng_cnt),
                          per_eng={e: sum(1 for o in ops if o.eng == e) for e in ENGS})
        with contextlib.ExitStack() as st:
            esem = {e: st.enter_context(nc.semaphore("se_" + e)) for e in ENGS}
            dsem = {e: [st.enter_context(nc.semaphore("sd_%s%d" % (e, i))) for i in range(NDMA_SEMS)]
                    for e in ENGS if dma_rr[e] > 0}
            block = st.enter_context(nc.Block())

            def semof(key):
                if key[0] == "e":
                    return esem[key[1]]
                return dsem[key[1]][key[2]]

            def run_engine(ename, eng):
                for op in ops:
                    if op.eng != ename:
                        continue
                    for key, v in op.waits:
                        eng.wait_ge(semof(key), v)
                    ins = op.fn(eng)
                    if op.dma:
                        ins.then_inc(dsem[ename][op.sem_idx], 16)
                    elif op.signal:
                        ins.then_inc(esem[ename], 1)
                if ename == final_wait_eng:
                    for e2 in dsem:
                        for i in range(NDMA_SEMS):
                            if dma_cnt[e2][i] > 0:
                                eng.wait_ge(dsem[e2][i], dma_cnt[e2][i])
                    for e2 in ENGS:
                        if eng_cnt[e2] > 0:
                            eng.wait_ge(esem[e2], eng_cnt[e2])

            @block.tensor
            def _(eng):
                run_engine("pe", eng)

            @block.scalar
            def _(eng):
                run_engine("act", eng)

            @block.vector
            def _(eng):
                run_engine("dve", eng)

            @block.gpsimd
            def _(eng):
                run_engine("pool", eng)

            @block.sync
            def _(eng):
                run_engine("sp", eng)


class Arena:
    def __init__(self, nc):
        self.nc = nc
        self.off = SB_BASE
        self.n = 0
        self.peak = SB_BASE

    def alloc(self, name, shape, dt):
        esz = 4 if dt in (F32, I32) else 2
        nbytes = esz * int(np.prod(shape[1:]))
        off = (self.off + 31) // 32 * 32
        assert off + nbytes <= SB_END, ("SBUF overflow", name, off, nbytes)
        self.n += 1
        t = self.nc.alloc_sbuf_tensor_at("%s_%d" % (name, self.n), list(shape), dt, offset=off)
        self.off = off + nbytes
        self.peak = max(self.peak, self.off)
        return t

    def mark(self):
        return self.off

    def release(self, m):
        self.off = m


class Rot:
    def __init__(self, arena, name, n, shape, dt):
        self.t = [arena.alloc("%s%d" % (name, i), shape, dt) for i in range(n)]
        self.name = name
        self.i = -1
        self.n = n

    def next(self):
        self.i += 1
        k = self.i % self.n
        return self.t[k], "%s#%d" % (self.name, k)


import os
HTQ = os.environ.get('HTQ', 'act')
P1STOP = int(os.environ.get('P1STOP', '9'))
S5POOL = int(os.environ.get('S5POOL', '2'))
HTGB = int(os.environ.get('HTGB', '1'))
VAR = os.environ.get('VAR', '')
TWO_PI_HI = 6.28125
TWO_PI_LO = 2.0 * math.pi - 6.28125
PI_CLAMP = 3.1415925


def build(NS, L, upto=99, dbg=False, cap=None):
    nc = bass.Bass("TRN2", target_bir_lowering=False)
    NG = L // GT
    NB = L // TB
    NT = L // 128
    CAP = cap if cap is not None else 2 * L // NE
    NQ = CAP // 128
    NTOK = NS * L

    def din(name, shape, dt=F32):
        return nc.dram_tensor(name, list(shape), dt, kind="ExternalInput").ap()

    def dscr(name, shape, dt):
        return nc.dram_tensor(name, list(shape), dt, kind="Internal").ap()

    x_d = din("x", [NS, L, D])
    g1_d = din("g1", [128, KD])
    win_d = din("w_in", [D, PT])
    bgate_d = din("b_gate", [128, 16])
    lng_d = din("ln_g", [128, GW])
    lnb_d = din("ln_b", [128, GW])
    wsT_d = din("wsT", [128, 8, 128])
    bstab_d = din("bstab", [128, GW])
    lamre_c_d = din("lamre_c", [128, 16])
    lamim_c_d = din("lamim_c", [128, 16])
    logdt_c_d = din("logdt_c", [128, 16])
    lamre_r_d = din("lamre_r", [128, 2048])
    lamim_r_d = din("lamim_r", [128, 2048])
    logdt_r_d = din("logdt_r", [128, 2048])
    braw_re_d = din("braw_re", [128, 2048])
    braw_im_d = din("braw_im", [128, 2048])
    craw_re_d = din("craw_re", [128, 2048])
    craw_im_d = din("craw_im", [128, 2048])
    dskip_d = din("dskip", [128, 2])
    wglu_d = din("w_glu", [S5W, S5W])
    bglu_d = din("b_glu", [128, 2])
    wupa_d = din("w_up_a", [GW, D])
    wupb_d = din("w_up_b", [S5W, D])
    wout_d = din("w_out", [D, D])
    g2_d = din("g2", [128, KD])
    wr_d = din("w_router", [D, NE])
    wg_d = din("w_gate", [NE, D, FF])
    wu_d = din("w_up", [NE, D, FF])
    wd_d = din("w_down", [NE, FF, D])
    gf_d = din("gf", [128, D])
    out_d = nc.dram_tensor("out", [NTOK, D], F32, kind="ExternalOutput").ap()

    hT_d = dscr("hT_scr", [NS, KD, 128, L], BF16)
    acc_d = dscr("acc_scr", [NTOK, D], F32)
    h2_d = dscr("h2_scr", [NTOK, D], BF16)
    dbg_outs = {}

    def dout(name, shape, dt=F32):
        t = nc.dram_tensor(name, list(shape), dt, kind="ExternalOutput").ap()
        dbg_outs[name] = t
        return t

    S = Sched(nc)
    A = Arena(nc)
    banks = [nc.alloc_psum_tensor("bank%d" % i, [128, 512], F32) for i in range(8)]

    def bankbf(i):
        return banks[i][:].bitcast(BF16)

    ident_bf = A.alloc("ident_bf", [128, 128], BF16)
    ident_f = A.alloc("ident_f", [128, 128], F32)
    halfpi = A.alloc("halfpi", [128, 1], F32)
    S.pool(lambda e: e.memset(ident_f[:], 1.0), writes=["ident_f"])
    S.pool(lambda e: e.affine_select(out=ident_f[:], in_=ident_f[:], pattern=[[-1, 128]], compare_op=ALU.is_equal,
                                    fill=0.0, base=0, channel_multiplier=1), reads=["ident_f"], writes=["ident_f"])
    S.dve(lambda e: e.tensor_copy(out=ident_bf[:], in_=ident_f[:]), reads=["ident_f"], writes=["ident_bf"])
    S.pool(lambda e: e.memset(halfpi[:], math.pi / 2.0), writes=["halfpi"])
    idx_i = A.alloc("idx_i", [128, 64 * NQ], I32)
    m0 = A.mark()
    brbT = [A.alloc("brbT%d" % s, [128, 2, L], BF16) for s in range(NS)]

    mA = A.mark()
    wins5 = A.alloc("wins5", [128, KD, S5W], BF16)
    g1 = A.alloc("g1", [128, KD], F32)
    bbT_re = A.alloc("bbT_re", [128, 16, 128], BF16)
    bbT_im = A.alloc("bbT_im", [128, 16, 128], BF16)
    cT_re = A.alloc("cT_re", [128, 16, 128], BF16)
    cT_imn = A.alloc("cT_imn", [128, 16, 128], BF16)
    cosT = A.alloc("cosT", [128, 16, TB], F32)
    sinT = A.alloc("sinT", [128, 16, TB], F32)
    rcol = A.alloc("rcol", [128, 16], F32)
    pblk_r = A.alloc("pblk_r", [128, 16], F32)
    pblk_i = A.alloc("pblk_i", [128, 16], F32)
    pblk_in = A.alloc("pblk_in", [128, 16], F32)
    dskip = A.alloc("dskip", [128, 2], F32)
    wglu = A.alloc("wglu", [128, 2, S5W], BF16)
    bglu = A.alloc("bglu", [128, 2], F32)

    S.dma("sp", lambda e: e.dma_start(out=g1[:], in_=g1_d[:, :]), writes=["g1"])
    S.dma("sp", lambda e: e.dma_start(out=dskip[:], in_=dskip_d[:, :]), writes=["dskip"])
    S.dma("sp", lambda e: e.dma_start(out=bglu[:], in_=bglu_d[:, :]), writes=["bglu"])

    mS = A.mark()
    stg = A.alloc("stg_s5w", [128, KD, S5W], F32)
    S.dma("sp", lambda e: e.dma_start(out=stg[:], in_=win_d[:, 2 * GW:2 * GW + S5W].rearrange("(k p) c -> p k c", p=128)),
          writes=["stg"])
    for k in range(KD):
        S.dve(lambda e, k=k: e.tensor_scalar(out=wins5[:, k, :], in0=stg[:, k, :], scalar1=g1[:, k:k + 1], scalar2=None,
                                             op0=ALU.mult), reads=["stg", "g1"], writes=["wins5"])
    stg2 = A.alloc("stg_glu", [128, 2, S5W], F32)
    S.dma("sp", lambda e: e.dma_start(out=stg2[:], in_=wglu_d[:, :].rearrange("(k p) c -> p k c", p=128)), writes=["stg2"])
    S.dve(lambda e: e.tensor_copy(out=wglu[:], in_=stg2[:]), reads=["stg2"], writes=["wglu"])

    def s5_scalars(tag, F, lamre_d, lamim_d, logdt_d):
        T = {}

        def al(n):
            T[n] = A.alloc(tag + n, [128, F], F32)
            return T[n]

        lamre = al("lamre"); lamim = al("lamim"); logdt = al("logdt")
        S.dma("sp", lambda e: e.dma_start(out=lamre[:], in_=lamre_d[:, :]), writes=[tag + "lamre"])
        S.dma("sp", lambda e: e.dma_start(out=lamim[:], in_=lamim_d[:, :]), writes=[tag + "lamim"])
        S.dma("sp", lambda e: e.dma_start(out=logdt[:], in_=logdt_d[:, :]), writes=[tag + "logdt"])
        dt = al("dt"); r = al("r"); th = al("th"); y = al("y"); kk = al("kk")
        s1 = al("s1"); c1 = al("c1"); t1 = al("t1"); t2 = al("t2"); cre = al("cre"); cim = al("cim")
        tk = lambda n: tag + n

        def dv(fn, reads, writes):
            S.dve(fn, reads=[tk(n) for n in reads], writes=[tk(n) for n in writes])

        def ac(fn, reads, writes):
            S.act(fn, reads=[tk(n) for n in reads], writes=[tk(n) for n in writes])

        def dve_exp(out_t, on, in_t, inn, offset, deg):
            dv(lambda e: e.tensor_scalar(out=t2[:], in0=in_t[:], scalar1=float(offset), scalar2=None, op0=ALU.add), [inn], ["t2"])
            dv(lambda e: e.tensor_scalar(out=out_t[:], in0=t2[:], scalar1=1.0 / math.factorial(deg), scalar2=None, op0=ALU.mult),
               ["t2"], [on])
            for kq in range(deg - 1, 0, -1):
                dv(lambda e, kq=kq: e.scalar_tensor_tensor(out=out_t[:], in0=out_t[:], scalar=1.0 / math.factorial(kq), in1=t2[:],
                                                           op0=ALU.add, op1=ALU.mult), [on, "t2"], [on])
            dv(lambda e: e.tensor_scalar(out=out_t[:], in0=out_t[:], scalar1=1.0, scalar2=math.exp(-offset), op0=ALU.add,
                                         op1=ALU.mult), [on], [on])
        dve_exp(dt, "dt", logdt, "logdt", 6.9375, 27)
        dv(lambda e: e.tensor_tensor(out=t1[:], in0=lamre[:], in1=dt[:], op=ALU.mult), ["lamre", "dt"], ["t1"])
        dv(lambda e: e.tensor_scalar(out=t1[:], in0=t1[:], scalar1=-1.0, scalar2=None, op0=ALU.mult), ["t1"], ["t1"])
        dve_exp(r, "r", t1, "t1", 0.0, 8)
        dv(lambda e: e.reciprocal(out=r[:], in_=r[:]), ["r"], ["r"])
        dv(lambda e: e.tensor_tensor(out=th[:], in0=lamim[:], in1=dt[:], op=ALU.mult), ["lamim", "dt"], ["th"])
        dv(lambda e: e.tensor_scalar(out=y[:], in0=th[:], scalar1=1.0 / (2.0 * math.pi), scalar2=None, op0=ALU.mult),
           ["th"], ["y"])
        dv(lambda e: e.tensor_scalar(out=kk[:], in0=y[:], scalar1=0.5, scalar2=None, op0=ALU.is_gt), ["y"], ["kk"])
        for m in range(1, 8):
            dv(lambda e, m=m: e.scalar_tensor_tensor(out=kk[:], in0=y[:], scalar=m + 0.5, in1=kk[:], op0=ALU.is_gt,
                                                     op1=ALU.add), ["y", "kk"], ["kk"])
        dv(lambda e: e.scalar_tensor_tensor(out=th[:], in0=kk[:], scalar=-TWO_PI_HI, in1=th[:], op0=ALU.mult, op1=ALU.add),
           ["kk", "th"], ["th"])
        dv(lambda e: e.scalar_tensor_tensor(out=th[:], in0=kk[:], scalar=-TWO_PI_LO, in1=th[:], op0=ALU.mult, op1=ALU.add),
           ["kk", "th"], ["th"])
        dv(lambda e: e.tensor_scalar(out=th[:], in0=th[:], scalar1=PI_CLAMP, scalar2=-PI_CLAMP, op0=ALU.min, op1=ALU.max),
           ["th"], ["th"])
        ac(lambda e: e.activation(out=s1[:], in_=th[:], func=AF.Sin), ["th"], ["s1"])
        dv(lambda e: e.scalar_tensor_tensor(out=t2[:], in0=th[:], scalar=-1.0, in1=th[:], op0=ALU.mult, op1=ALU.max), ["th"], ["t2"])
        S.act(lambda e: e.activation(out=c1[:], in_=t2[:], func=AF.Sin, bias=halfpi[:, 0:1], scale=-1.0),
              reads=[tk("t2"), "halfpi"], writes=[tk("c1")])
        lbr = y; lbi = kk
        dv(lambda e: e.tensor_tensor(out=lbr[:], in0=r[:], in1=c1[:], op=ALU.mult), ["r", "c1"], ["y"])
        dv(lambda e: e.tensor_tensor(out=lbi[:], in0=r[:], in1=s1[:], op=ALU.mult), ["r", "s1"], ["kk"])
        dv(lambda e: e.tensor_scalar(out=lbr[:], in0=lbr[:], scalar1=-1.0, scalar2=None, op0=ALU.add), ["y"], ["y"])
        dv(lambda e: e.tensor_tensor(out=t1[:], in0=lamre[:], in1=lamre[:], op=ALU.mult), ["lamre"], ["t1"])
        dv(lambda e: e.tensor_tensor(out=t2[:], in0=lamim[:], in1=lamim[:], op=ALU.mult), ["lamim"], ["t2"])
        dv(lambda e: e.tensor_tensor(out=t1[:], in0=t1[:], in1=t2[:], op=ALU.add), ["t1", "t2"], ["t1"])
        dv(lambda e: e.reciprocal(out=t1[:], in_=t1[:]), ["t1"], ["t1"])
        dv(lambda e: e.tensor_tensor(out=cre[:], in0=lbr[:], in1=lamre[:], op=ALU.mult), ["y", "lamre"], ["cre"])
        dv(lambda e: e.tensor_tensor(out=t2[:], in0=lbi[:], in1=lamim[:], op=ALU.mult), ["kk", "lamim"], ["t2"])
        dv(lambda e: e.tensor_tensor(out=cre[:], in0=cre[:], in1=t2[:], op=ALU.add), ["cre", "t2"], ["cre"])
        dv(lambda e: e.tensor_tensor(out=cre[:], in0=cre[:], in1=t1[:], op=ALU.mult), ["cre", "t1"], ["cre"])
        dv(lambda e: e.tensor_tensor(out=cim[:], in0=lbi[:], in1=lamre[:], op=ALU.mult), ["kk", "lamre"], ["cim"])
        dv(lambda e: e.tensor_tensor(out=t2[:], in0=lbr[:], in1=lamim[:], op=ALU.mult), ["y", "lamim"], ["t2"])
        dv(lambda e: e.tensor_tensor(out=cim[:], in0=cim[:], in1=t2[:], op=ALU.subtract), ["cim", "t2"], ["cim"])
        dv(lambda e: e.tensor_tensor(out=cim[:], in0=cim[:], in1=t1[:], op=ALU.mult), ["cim", "t1"], ["cim"])
        return dict(r=r, c1=c1, s1=s1, cre=cre, cim=cim), tk

    colT, ctk = s5_scalars("c_", 16, lamre_c_d, lamim_c_d, logdt_c_d)
    S.dve(lambda e: e.tensor_copy(out=rcol[:], in_=colT["r"][:]), reads=[ctk("r")], writes=["rcol"])
    pr = A.alloc("pr", [128, 16], F32); pi_ = A.alloc("pi", [128, 16], F32)
    pt1 = A.alloc("pt1", [128, 16], F32); pt2 = A.alloc("pt2", [128, 16], F32)
    S.dve(lambda e: e.tensor_copy(out=pr[:], in_=colT["c1"][:]), reads=[ctk("c1")], writes=["pr"])
    S.dve(lambda e: e.tensor_copy(out=pi_[:], in_=colT["s1"][:]), reads=[ctk("s1")], writes=["pi"])
    S.pool(lambda e: e.memset(cosT[:, :, 0:1], 1.0), writes=["cosT"])
    S.pool(lambda e: e.memset(sinT[:, :, 0:1], 0.0), writes=["sinT"])
    tq = [A.alloc("tq%d" % i, [128, 16, TB // 2], F32) for i in range(4)]
    k = 1
    while k < TB:
        def bc(t, k=k):
            return t[:, :].unsqueeze(2).to_broadcast([128, 16, k])
        Ck = cosT[:, :, 0:k]; Sk = sinT[:, :, 0:k]
        S.dve(lambda e, k=k, Ck=Ck: e.tensor_tensor(out=tq[0][:, :, 0:k], in0=Ck, in1=bc(pr, k), op=ALU.mult),
              reads=["cosT", "pr"], writes=["tq0"])
        S.dve(lambda e, k=k, Sk=Sk: e.tensor_tensor(out=tq[1][:, :, 0:k], in0=Sk, in1=bc(pi_, k), op=ALU.mult),
              reads=["sinT", "pi"], writes=["tq1"])
        S.dve(lambda e, k=k, Ck=Ck: e.tensor_tensor(out=tq[2][:, :, 0:k], in0=Ck, in1=bc(pi_, k), op=ALU.mult),
              reads=["cosT", "pi"], writes=["tq2"])
        S.dve(lambda e, k=k, Sk=Sk: e.tensor_tensor(out=tq[3][:, :, 0:k], in0=Sk, in1=bc(pr, k), op=ALU.mult),
              reads=["sinT", "pr"], writes=["tq3"])
        S.dve(lambda e, k=k: e.tensor_tensor(out=cosT[:, :, k:2 * k], in0=tq[0][:, :, 0:k], in1=tq[1][:, :, 0:k],
                                             op=ALU.subtract), reads=["tq0", "tq1"], writes=["cosT"])
        S.dve(lambda e, k=k: e.tensor_tensor(out=sinT[:, :, k:2 * k], in0=tq[2][:, :, 0:k], in1=tq[3][:, :, 0:k],
                                             op=ALU.add), reads=["tq2", "tq3"], writes=["sinT"])
        S.dve(lambda e: e.tensor_tensor(out=pt1[:], in0=pr[:], in1=pr[:], op=ALU.mult), reads=["pr"], writes=["pt1"])
        S.dve(lambda e: e.tensor_tensor(out=pt2[:], in0=pi_[:], in1=pi_[:], op=ALU.mult), reads=["pi"], writes=["pt2"])
        S.dve(lambda e: e.tensor_tensor(out=pt2[:], in0=pt1[:], in1=pt2[:], op=ALU.subtract), reads=["pt1", "pt2"],
              writes=["pt2"])
        S.dve(lambda e: e.tensor_tensor(out=pt1[:], in0=pr[:], in1=pi_[:], op=ALU.mult), reads=["pr", "pi"], writes=["pt1"])
        S.dve(lambda e: e.tensor_copy(out=pr[:], in_=pt2[:]), reads=["pt2"], writes=["pr"])
        S.dve(lambda e: e.tensor_scalar(out=pi_[:], in0=pt1[:], scalar1=2.0, scalar2=None, op0=ALU.mult), reads=["pt1"],
              writes=["pi"])
        k *= 2
    S.dve(lambda e: e.tensor_copy(out=pblk_r[:], in_=pr[:]), reads=["pr"], writes=["pblk"])
    S.dve(lambda e: e.tensor_copy(out=pblk_i[:], in_=pi_[:]), reads=["pi"], writes=["pblk"])
    S.dve(lambda e: e.tensor_scalar(out=pblk_in[:], in0=pi_[:], scalar1=-1.0, scalar2=None, op0=ALU.mult), reads=["pi"],
          writes=["pblk"])
    S.barrier()
    A.release(mS)
    for dd_ in range(2):
        mR = A.mark()
        hs = slice(dd_ * 1024, (dd_ + 1) * 1024)
        rowT, rtk = s5_scalars("r%d_" % dd_, 1024, lamre_r_d[:, hs], lamim_r_d[:, hs], logdt_r_d[:, hs])
        braw_re = rowT["r"]; braw_im = rowT["c1"]
        S.dma("sp", lambda e, braw_re=braw_re, hs=hs: e.dma_start(out=braw_re[:], in_=braw_re_d[:, hs]),
              reads=[rtk("cre"), rtk("cim")], writes=[rtk("r")])
        S.dma("sp", lambda e, braw_im=braw_im, hs=hs: e.dma_start(out=braw_im[:], in_=braw_im_d[:, hs]),
              reads=[rtk("cre"), rtk("cim")], writes=[rtk("c1")])
        u1 = rowT["s1"]
        cre = rowT["cre"]; cim = rowT["cim"]
        bbT_re_f = bbT_re[:, dd_ * 8:(dd_ + 1) * 8, :].rearrange("p a b -> p (a b)")
        bbT_im_f = bbT_im[:, dd_ * 8:(dd_ + 1) * 8, :].rearrange("p a b -> p (a b)")
        tA = A.alloc("rowtA", [128, 1024], F32)
        tAk = "rowtA%d" % dd_
        S.dve(lambda e, u1=u1, cim=cim, braw_im=braw_im: e.tensor_tensor(out=u1[:], in0=cim[:], in1=braw_im[:], op=ALU.mult),
              reads=[rtk("cim"), rtk("c1")], writes=[rtk("s1")])
        S.dve(lambda e, tA=tA, cre=cre, braw_re=braw_re: e.tensor_tensor(out=tA[:], in0=cre[:], in1=braw_re[:], op=ALU.mult),
              reads=[rtk("cre"), rtk("r")], writes=[tAk])
        S.dve(lambda e, tA=tA, u1=u1, bbT_re_f=bbT_re_f: e.tensor_tensor(out=bbT_re_f, in0=tA[:], in1=u1[:], op=ALU.subtract),
              reads=[tAk, rtk("s1")], writes=["bbT_re"])
        S.dve(lambda e, u1=u1, cim=cim, braw_re=braw_re: e.tensor_tensor(out=u1[:], in0=cim[:], in1=braw_re[:], op=ALU.mult),
              reads=[rtk("cim"), rtk("r"), "bbT_re"], writes=[rtk("s1")])
        S.dve(lambda e, tA=tA, cre=cre, braw_im=braw_im: e.tensor_tensor(out=tA[:], in0=cre[:], in1=braw_im[:], op=ALU.mult),
              reads=[rtk("cre"), rtk("c1"), "bbT_re"], writes=[tAk])
        S.dve(lambda e, tA=tA, u1=u1, bbT_im_f=bbT_im_f: e.tensor_tensor(out=bbT_im_f, in0=tA[:], in1=u1[:], op=ALU.add),
              reads=[tAk, rtk("s1")], writes=["bbT_im"])
        S.barrier()
        A.release(mR)
    cst = A.alloc("cst", [128, 2048], F32)
    S.dma("sp", lambda e: e.dma_start(out=cst[:], in_=craw_re_d[:, :]), writes=["cst"])
    S.dve(lambda e: e.tensor_copy(out=cT_re[:].rearrange("p a b -> p (a b)"), in_=cst[:]), reads=["cst"], writes=["cT_re"])
    S.dma("sp", lambda e: e.dma_start(out=cst[:], in_=craw_im_d[:, :]), reads=["cT_re"], writes=["cst"])
    S.dve(lambda e: e.tensor_scalar(out=cT_imn[:].rearrange("p a b -> p (a b)"), in0=cst[:], scalar1=-1.0, scalar2=None,
                                    op0=ALU.mult), reads=["cst"], writes=["cT_imn"])
    S.barrier()
    A.release(mS)
    sin_bf = A.alloc("sin_bf", [128, 2, L], BF16)
    yacc = A.alloc("yacc", [128, 2, L], F32)

    epsb = A.alloc("epsb", [128, 1], F32)
    S.pool(lambda e: e.memset(epsb[:], EPS), writes=["epsb"])
    mW0 = A.mark()
    xt1_r = Rot(A, "xt1", 4, [128, D], F32)
    xn_r = Rot(A, "xn", 1, [128, 4, D], BF16)
    hT_r = Rot(A, "hTg", 1, [128, KD, GT], BF16)
    junk = A.alloc("junk", [128, D], BF16)
    ss_r = Rot(A, "ss", 2, [128, 4], F32)
    rs_r = Rot(A, "rs", 2, [128, 4], F32)
    mW = A.mark()

    def phase1(s):
        for gi in range(NG):
            xn, xnk = xn_r.next()
            hTg, hTk = hT_r.next()
            ss, ssk = ss_r.next()
            rs, rsk = rs_r.next()
            for i in range(4):
                xt1, xt1k = xt1_r.next()
                S.dma("sp", lambda e, xt1=xt1, gi=gi, i=i: e.dma_start(
                    out=xt1[:], in_=x_d[s, gi * GT + i * 128:gi * GT + (i + 1) * 128, :]), writes=[xt1k])
                S.act(lambda e, xt1=xt1, xn=xn, ss=ss, i=i: e.activation(out=xn[:, i, :], in_=xt1[:], func=AF.Square,
                                                                         accum_out=ss[:, i:i + 1]),
                      reads=[xt1k], writes=[ssk + str(i), xnk + str(i)])
                S.act(lambda e, ss=ss, rs=rs, i=i: e.activation(out=rs[:, i:i + 1], in_=ss[:, i:i + 1], func=AF.Sqrt, bias=epsb[:, 0:1],
                                                               scale=1.0 / D),
                      reads=[ssk + str(i), "epsb"], writes=[rsk + str(i)])
                S.dve(lambda e, rs=rs, i=i: e.reciprocal(out=rs[:, i:i + 1], in_=rs[:, i:i + 1]), reads=[rsk + str(i)], writes=[rsk + str(i)])
                S.dve(lambda e, xt1=xt1, xn=xn, rs=rs, i=i: e.tensor_scalar(out=xn[:, i, :], in0=xt1[:],
                                                                            scalar1=rs[:, i:i + 1], scalar2=None, op0=ALU.mult),
                      reads=[xt1k, rsk + str(i)], writes=[xnk + str(i)])
            if P1STOP < 2:
                continue
            for i in range(4):
                pb = (gi * 4 + i) % 2
                for k in range(KD):
                    S.pe(lambda e, xn=xn, i=i, k=k, pb=pb: e.transpose(out=bankbf(pb)[:, k * 128:(k + 1) * 128],
                                                                        in_=xn[:, i, k * 128:(k + 1) * 128], identity=ident_bf[:]),
                         reads=[xnk + str(i), "ident_bf"], writes=["bank%d" % pb])
                eng = "act" if i % 2 == 0 else "dve"
                if eng == "act":
                    S.act(lambda e, hTg=hTg, i=i, pb=pb: e.activation(
                        out=hTg[:, :, i * 128:(i + 1) * 128], in_=bankbf(pb).rearrange("p (k t) -> p k t", k=KD), func=AF.Copy),
                        reads=["bank%d" % pb], writes=[hTk + str(i)])
                else:
                    S.dve(lambda e, hTg=hTg, i=i, pb=pb: e.tensor_copy(
                        out=hTg[:, :, i * 128:(i + 1) * 128], in_=bankbf(pb).rearrange("p (k t) -> p k t", k=KD)),
                        reads=["bank%d" % pb], writes=[hTk + str(i)])
            hTall = [hTk + str(i) for i in range(4)]
            if P1STOP < 3:
                continue
            for kt in range(2):
                for k in range(KD):
                    S.pe(lambda e, hTg=hTg, kt=kt, k=k: e.matmul(banks[2 + kt][:], lhsT=wins5[:, k, kt * 128:(kt + 1) * 128],
                                                                   rhs=hTg[:, k, :], start=(k == 0), stop=(k == KD - 1)),
                         reads=hTall + ["wins5"], writes=["bank%d" % (2 + kt)])
                if 'noact' in VAR:
                    continue
                if 'onlydve' not in VAR:
                    S.act(lambda e, kt=kt, gi=gi: e.activation(out=sin_bf[:, kt, gi * GT:(gi + 1) * GT], in_=banks[2 + kt][:],
                                                               func=AF.Copy), reads=["bank%d" % (2 + kt)], writes=["sin_bf", "ser%d" % kt])
                if 'nodve' in VAR:
                    continue
                S.dve(lambda e, kt=kt, gi=gi: e.tensor_scalar(out=yacc[:, kt, gi * GT:(gi + 1) * GT], in0=banks[2 + kt][:],
                                                              scalar1=dskip[:, kt:kt + 1], scalar2=None, op0=ALU.mult),
                      reads=["bank%d" % (2 + kt), "dskip"] + (["ser%d" % kt] if 'ser' in VAR else []), writes=["yacc"])
            if P1STOP < 4:
                continue
            S.dma(HTQ, lambda e, hTg=hTg, gi=gi: e.dma_start(
                out=hT_d[s, :, :, gi * GT:(gi + 1) * GT].rearrange("k p t -> p k t"), in_=hTg[:]),
                reads=hTall, writes=["hT_d%d_%d" % (s, gi)])

    A.release(mW0)
    NR = 3
    bu_r = Rot(A, "bu", NR, [128, 2, TB], F32)
    mm_r = Rot(A, "mm", 2, [128, 2, TB], F32)
    xt_r = Rot(A, "xt", NR, [128, 2, TB], F32)
    xb_r = Rot(A, "xb", 2, [128, 4, TB], BF16)
    init = [A.alloc("init%d" % i, [128, 16, 2], F32) for i in range(2)]
    ctmp = A.alloc("ctmp", [128, 16, 2], F32)
    sig_t = [mm_r.t[0][:, 0, 0:GT], mm_r.t[0][:, 1, 0:GT]]
    yg = sin_bf

    def s5(s):
        ulist = []
        for bi in range(NB):
            for d in range(2):
                for j in range(8):
                    ulist.append((bi, d, j, len(ulist)))
        ub = {}

        def upar(u):
            bi, d, j, uc = u
            b = bi if d == 0 else NB - 1 - bi
            return bi, d, j, uc, slice(b * TB, (b + 1) * TB), 6 + d, d * 8 + j, j // 4, (uc % NR) * 2

        def stage1(u):
            bi, d, j, uc, tsl, ybank, dj, kt, bk = upar(u)
            bu, buk = bu_r.next()
            S.pe(lambda e, dj=dj, kt=kt, tsl=tsl, bk=bk: e.matmul(banks[bk][:, 0:TB], lhsT=bbT_re[:, dj, :],
                                                                  rhs=sin_bf[:, kt, tsl], start=True, stop=True),
                 reads=["sin_bf", "bbT_re"], writes=["bank%d" % bk])
            S.pe(lambda e, dj=dj, kt=kt, tsl=tsl, bk=bk: e.matmul(banks[bk + 1][:, 0:TB], lhsT=bbT_im[:, dj, :],
                                                                  rhs=sin_bf[:, kt, tsl], start=True, stop=True),
                 reads=["sin_bf", "bbT_im"], writes=["bank%d" % (bk + 1)])

            def rv(ap, d=d):
                return ap if d == 0 else ap[:, ::-1]
            S.act(lambda e, bu=bu, bk=bk, rv=rv: e.activation(out=bu[:, 0, :], in_=rv(banks[bk][:, 0:TB]), func=AF.Copy),
                  reads=["bank%d" % bk], writes=[buk + "r"])
            S.act(lambda e, bu=bu, bk=bk, rv=rv: e.activation(out=bu[:, 1, :], in_=rv(banks[bk + 1][:, 0:TB]), func=AF.Copy),
                  reads=["bank%d" % (bk + 1)], writes=[buk + "i"])
            ub[uc] = (bu, buk, rv)

        def stage2(u):
            bi, d, j, uc, tsl, ybank, dj, kt, bk = upar(u)
            bu, buk, rv = ub.pop(uc)
            mm, mmk = mm_r.next(); xt, xtk = xt_r.next(); xb, xbk = xb_r.next()
            Cc = cosT[:, dj, :]; Sn = sinT[:, dj, :]
            mods = [
                (lambda e: e.tensor_tensor(out=mm[:, 0, :], in0=bu[:, 0, :], in1=Cc, op=ALU.mult), [buk + "r"], [mmk + "0"]),
                (lambda e: e.tensor_tensor(out=mm[:, 1, :], in0=bu[:, 1, :], in1=Sn, op=ALU.mult), [buk + "i"], [mmk + "1"]),
                (lambda e: e.tensor_tensor(out=mm[:, 0, :], in0=mm[:, 0, :], in1=mm[:, 1, :], op=ALU.add), [mmk + "0", mmk + "1"], [mmk + "0"]),
                (lambda e: e.tensor_tensor(out=mm[:, 1, :], in0=bu[:, 1, :], in1=Cc, op=ALU.mult), [buk + "i", mmk + "1"], [mmk + "1"]),
                (lambda e: e.tensor_tensor(out=bu[:, 0, :], in0=bu[:, 0, :], in1=Sn, op=ALU.mult), [buk + "r"], [buk + "r"]),
                (lambda e: e.tensor_tensor(out=mm[:, 1, :], in0=mm[:, 1, :], in1=bu[:, 0, :], op=ALU.subtract), [mmk + "1", buk + "r"], [mmk + "1"]),
            ]
            for oi, (fn_, rd_, wr_) in enumerate(mods):
                S.any("pool" if oi < S5POOL else "dve", fn_, reads=rd_, writes=wr_)
            rb = rcol[:, dj:dj + 1].to_broadcast([128, TB])
            ini = init[bi % 2]
            inik = "init%d_%d" % (bi % 2, dj)
            if bi == 0:
                i_re = 0.0; i_im = 0.0; ird = []
            else:
                i_re = ini[:, dj, 0:1]; i_im = ini[:, dj, 1:2]; ird = [inik]
            S.dve(lambda e, xt=xt, mm=mm, rb=rb, i_re=i_re: e.tensor_tensor_scan(
                out=xt[:, 0, :], data0=rb, data1=mm[:, 0, :], initial=i_re, op0=ALU.mult, op1=ALU.add),
                reads=[mmk + "0"] + ird, writes=[xtk + "r"])
            S.dve(lambda e, xt=xt, mm=mm, rb=rb, i_im=i_im: e.tensor_tensor_scan(
                out=xt[:, 1, :], data0=rb, data1=mm[:, 1, :], initial=i_im, op0=ALU.mult, op1=ALU.add),
                reads=[mmk + "1"] + ird, writes=[xtk + "i"])
            if bi < NB - 1:
                nin = init[(bi + 1) % 2]
                nk = "init%d_%d" % ((bi + 1) % 2, dj)
                ck = "ctmp%d" % dj
                S.dve(lambda e, xt=xt, dj=dj: e.tensor_scalar(out=ctmp[:, dj, 0:1], in0=xt[:, 0, TB - 1:TB],
                                                              scalar1=pblk_r[:, dj:dj + 1], scalar2=None, op0=ALU.mult),
                      reads=[xtk + "r"], writes=[ck + "a"])
                S.dve(lambda e, xt=xt, dj=dj: e.tensor_scalar(out=ctmp[:, dj, 1:2], in0=xt[:, 1, TB - 1:TB],
                                                              scalar1=pblk_r[:, dj:dj + 1], scalar2=None, op0=ALU.mult),
                      reads=[xtk + "i"], writes=[ck + "b"])
                S.dve(lambda e, xt=xt, dj=dj, nin=nin: e.scalar_tensor_tensor(
                    out=nin[:, dj, 0:1], in0=xt[:, 1, TB - 1:TB], scalar=pblk_in[:, dj:dj + 1], in1=ctmp[:, dj, 0:1],
                    op0=ALU.mult, op1=ALU.add), reads=[xtk + "i", ck + "a"], writes=[nk])
                S.dve(lambda e, xt=xt, dj=dj, nin=nin: e.scalar_tensor_tensor(
                    out=nin[:, dj, 1:2], in0=xt[:, 0, TB - 1:TB], scalar=pblk_i[:, dj:dj + 1], in1=ctmp[:, dj, 1:2],
                    op0=ALU.mult, op1=ALU.add), reads=[xtk + "r", ck + "b"], writes=[nk])
            S.dve(lambda e: e.tensor_tensor(out=rv(xb[:, 0, :]), in0=xt[:, 0, :], in1=Cc, op=ALU.mult), reads=[xtk + "r"], writes=[xbk + "0"])
            S.dve(lambda e: e.scalar_tensor_tensor(out=rv(xb[:, 1, :]), in0=xt[:, 1, :], scalar=-1.0, in1=Sn, op0=ALU.mult, op1=ALU.mult),
                  reads=[xtk + "i"], writes=[xbk + "1"])
            S.dve(lambda e: e.tensor_tensor(out=rv(xb[:, 2, :]), in0=xt[:, 0, :], in1=Sn, op=ALU.mult), reads=[xtk + "r"], writes=[xbk + "2"])
            S.dve(lambda e: e.tensor_tensor(out=rv(xb[:, 3, :]), in0=xt[:, 1, :], in1=Cc, op=ALU.mult), reads=[xtk + "i"], writes=[xbk + "3"])
            for pi_x, lt in enumerate((cT_re, cT_re, cT_imn, cT_imn)):
                S.pe(lambda e, pi_x=pi_x, lt=lt: e.matmul(banks[ybank][:, 0:TB], lhsT=lt[:, dj, :], rhs=xb[:, pi_x, :],
                                                          start=(j % 4 == 0 and pi_x == 0), stop=(j % 4 == 3 and pi_x == 3)),
                     reads=[xbk + str(pi_x), "cT_re", "cT_imn"], writes=["bank%d" % ybank])
            if j % 4 == 3:
                S.dve(lambda e, kt=kt, tsl=tsl, ybank=ybank: e.tensor_tensor(out=yacc[:, kt, tsl], in0=yacc[:, kt, tsl],
                                                                             in1=banks[ybank][:, 0:TB], op=ALU.add),
                      reads=["bank%d" % ybank, "yacc"], writes=["yacc"])

        for u in ulist[:2]:
            stage1(u)
        for i_, u in enumerate(ulist):
            if i_ + 2 < len(ulist):
                stage1(ulist[i_ + 2])
            stage2(u)
        S.barrier()
        sgi = 0
        for gi in range(NG):
            gsl = slice(gi * GT, (gi + 1) * GT)
            for kt in range(2):
                S.act(lambda e, kt=kt, gsl=gsl: e.activation(out=yg[:, kt, gsl], in_=yacc[:, kt, gsl], func=AF.Gelu),
                      reads=["yacc"], writes=["yg%d" % gi, "sin_bf"])
            for m in range(2):
                pb = m
                for kt in range(2):
                    S.pe(lambda e, m=m, kt=kt, gsl=gsl, pb=pb: e.matmul(banks[pb][:], lhsT=wglu[:, kt, m * 128:(m + 1) * 128],
                                                                        rhs=yg[:, kt, gsl], start=(kt == 0), stop=(kt == 1)),
                         reads=["yg%d" % gi, "wglu"], writes=["bank%d" % pb])
                sg = sig_t[sgi % 2]; sgk = "sigt%d" % (sgi % 2)
                sgi += 1
                S.act(lambda e, sg=sg, m=m, pb=pb: e.activation(out=sg, in_=banks[pb][:], func=AF.Sigmoid, bias=bglu[:, m:m + 1]),
                      reads=["bank%d" % pb, "bglu"], writes=[sgk])
                S.dve(lambda e, sg=sg, m=m, gsl=gsl: e.tensor_tensor(out=brbT[s][:, m, gsl], in0=yg[:, m, gsl], in1=sg, op=ALU.mult),
                      reads=[sgk, "yg%d" % gi], writes=["brbT%d" % s])

    for s in range(NS):
        if upto >= 1:
            phase1(s)
        S.barrier()
        if upto >= 2:
            s5(s)
        S.barrier()
    if dbg:
        d_sin = dout("dbg_yacc", [NS, 2, 128, L])
        d_brb = dout("dbg_brb", [NS, 2, 128, L], BF16)
        S.dma("sp", lambda e: e.dma_start(out=d_sin[NS - 1].rearrange("k p t -> p k t"), in_=yacc[:]), reads=["yacc"], writes=["dbg1"])
        for s in range(NS):
            S.dma("sp", lambda e, s=s: e.dma_start(out=d_brb[s].rearrange("k p t -> p k t"), in_=brbT[s][:]),
                  reads=["brbT%d" % s], writes=["dbg2%d" % s])
        d_tab = dout("dbg_cos", [128, 16, TB])
        S.dma("sp", lambda e: e.dma_start(out=d_tab[:, :, :], in_=cosT[:]), reads=["cosT"], writes=["dbg3"])
        d_bb = dout("dbg_bb", [128, 16, 128], BF16)
        S.dma("sp", lambda e: e.dma_start(out=d_bb[:, :, :], in_=bbT_re[:]), reads=["bbT_re"], writes=["dbg4"])
    S.barrier()
    A.release(mA)
    if upto <= 2:
        zt = A.alloc("zt", [128, D], F32)
        S.pool(lambda e: e.memset(zt[:], 0.0), writes=["zt"])
        for t in range(NTOK // 128):
            S.dma("sp", lambda e, t=t: e.dma_start(out=out_d[t * 128:(t + 1) * 128, :], in_=zt[:]), reads=["zt"], writes=["o%d" % t])
        S.emit()
        return nc, S, A

    HX = D + 32
    h2x_d = dscr("h2x_scr", [NTOK, D], BF16)
    pr_d = dscr("pr_scr", [NTOK, NE], F32)
    scoresT = A.alloc("scoresT", [64, L], F32)
    mB = A.mark()
    win_bf = A.alloc("win_bf", [128, KD, PT], BF16)
    wupa = A.alloc("wupa", [128, 4, D], BF16)
    wupb = A.alloc("wupb", [128, 2, D], BF16)
    wout = A.alloc("wout", [128, KD, D], BF16)
    wsT = A.alloc("wsT", [128, 8, 128], BF16)
    wr = A.alloc("wr", [128, KD, NE], BF16)
    lng = A.alloc("lng", [128, GW], F32)
    lnb = A.alloc("lnb", [128, GW], F32)
    bstab = A.alloc("bstab", [128, GW], F32)
    bgate = A.alloc("bgate", [128, 16], F32)
    g2 = A.alloc("g2", [128, KD], F32)
    g1b = A.alloc("g1b", [128, KD], F32)
    epsb2 = A.alloc("epsb2", [128, 1], F32)
    S.pool(lambda e: e.memset(epsb2[:], EPS), writes=["epsb2"])
    S.pool(lambda e: e.memset(scoresT[:], 0.0), writes=["scoresT"])
    for (t_, d_, nm) in ((lng, lng_d, "lng"), (lnb, lnb_d, "lnb"), (bstab, bstab_d, "bstab"), (bgate, bgate_d, "bgate"),
                         (g2, g2_d, "g2"), (g1b, g1_d, "g1b")):
        S.dma("sp", lambda e, t_=t_, d_=d_: e.dma_start(out=t_[:], in_=d_[:, :]), writes=[nm])
    mBs = A.mark()
    stgB = Rot(A, "stgB", 2, [128, PT], F32)
    for k in range(KD):
        st_, stk = stgB.next()
        S.dma("sp", lambda e, st_=st_, k=k: e.dma_start(out=st_[:], in_=win_d[k * 128:(k + 1) * 128, :]), writes=[stk])
        S.dve(lambda e, st_=st_, k=k: e.tensor_scalar(out=win_bf[:, k, :], in0=st_[:], scalar1=g1b[:, k:k + 1], scalar2=None,
                                                      op0=ALU.mult), reads=[stk, "g1b"], writes=["win_bf"])
    def load_cast(dst2d, src2d, ncol, nm, eng):
        st_, stk = stgB.next()
        S.dma("sp", lambda e: e.dma_start(out=st_[:, 0:ncol], in_=src2d), writes=[stk])
        if eng == "act":
            S.act(lambda e: e.activation(out=dst2d, in_=st_[:, 0:ncol], func=AF.Copy), reads=[stk], writes=[nm])
        else:
            S.dve(lambda e: e.tensor_copy(out=dst2d, in_=st_[:, 0:ncol]), reads=[stk], writes=[nm])
    for k in range(4):
        load_cast(wupa[:, k, :], wupa_d[k * 128:(k + 1) * 128, :], D, "wupa", "act")
    for k in range(2):
        load_cast(wupb[:, k, :], wupb_d[k * 128:(k + 1) * 128, :], D, "wupb", "dve")
    for k in range(KD):
        load_cast(wout[:, k, :], wout_d[k * 128:(k + 1) * 128, :], D, "wout", "act" if k % 2 else "dve")
    load_cast(wsT[:].rearrange("p a b -> p (a b)"), wsT_d[:, :, :].rearrange("p a b -> p (a b)"), 1024, "wsT", "dve")
    for k in range(KD):
        load_cast(wr[:, k, :], wr_d[k * 128:(k + 1) * 128, :], NE, "wr", "dve")
    S.barrier()
    A.release(mBs)

    hTg_r = Rot(A, "hTgB", HTGB, [128, KD, GT], BF16)
    xt_r2 = Rot(A, "xtB", 1, [128, D], F32)
    u_r = Rot(A, "u_sb", 3, [128, GW], F32)
    v_r = Rot(A, "v_sb", 3, [128, GW], F32)
    vn_r = Rot(A, "vn", 3, [128, GW], BF16)
    bra_r = Rot(A, "bra", 2, [128, GW], BF16)
    braT_r = Rot(A, "braT", 1, [128, 4, GT], BF16)
    sg_r = Rot(A, "sgB", 4, [128, GT], F32)
    mrg_r = Rot(A, "mrg", 1, [128, KD, GT], BF16)
    x1_r = Rot(A, "x1", 2, [128, D], F32)
    hn_r = Rot(A, "hn2", 3, [128, D], BF16)
    pr_r = Rot(A, "prB", 4, [128, NE], F32)
    h2T_r = Rot(A, "h2T", 1, [128, KD, 128], BF16)
    st_r = Rot(A, "stB", 8, [128, 8], F32)
    ex_r = Rot(A, "exB", 2, [128, NE], F32)
    pw_r = [[A.alloc("pw%d_%d" % (s, i), [128, 32 * NS], F32) for i in range(2)] for s in range(NS)]
    for s in range(NS):
        for i in range(2):
            S.pool(lambda e, s=s, i=i: e.memset(pw_r[s][i][:], 0.0), writes=["pw%d_%d" % (s, i)])

    hT_next = {}

    def phaseB(s):
        for gi in range(NG):
            phaseB_group(s, gi)

    def phaseB_group(s, gi):
        if True:
            def load_hT(s_, gi_):
                t_, k_ = hTg_r.next()
                S.dma("sp", lambda e: e.dma_start(
                    out=t_[:], in_=hT_d[s_, :, :, gi_ * GT:(gi_ + 1) * GT].rearrange("k p t -> p k t")),
                    reads=["hT_d%d_%d" % (s_, gi_)], writes=[k_])
                return t_, k_
            if (s, gi) in hT_next:
                hTg, hTk = hT_next.pop((s, gi))
            else:
                hTg, hTk = load_hT(s, gi)
            braT, braTk = braT_r.next()
            mrg, mrgk = mrg_r.next()
            tb = {}

            def gm1(i):
                tsl = slice(i * 128, (i + 1) * 128)
                u_sb, uk = u_r.next(); v_sb, vk = v_r.next(); vn, vnk = vn_r.next(); stt, stk = st_r.next()
                vh, vhk = v_sb, vk
                for (bk_, c0) in ((0, 0), (1, GW)):
                    for k in range(KD):
                        S.pe(lambda e, k=k, bk_=bk_, c0=c0: e.matmul(
                            banks[bk_][:], lhsT=hTg[:, k, tsl], rhs=win_bf[:, k, c0:c0 + GW], start=(k == 0), stop=(k == KD - 1)),
                            reads=[hTk, "win_bf"], writes=["bank%d" % bk_])
                S.act(lambda e: e.activation(out=u_sb[:], in_=banks[0][:], func=AF.Gelu), reads=["bank0"], writes=[uk])
                S.act(lambda e: e.activation(out=v_sb[:], in_=banks[1][:], func=AF.Gelu, accum_out=stt[:, 0:1]),
                      reads=["bank1"], writes=[vk, stk + "a"])
                S.dve(lambda e: e.tensor_scalar(out=stt[:, 1:2], in0=stt[:, 0:1], scalar1=-1.0 / GW, scalar2=None, op0=ALU.mult),
                      reads=[stk + "a"], writes=[stk + "b"])
                S.act(lambda e: e.activation(out=vn[:], in_=v_sb[:], func=AF.Square, bias=stt[:, 1:2],
                                             accum_out=stt[:, 2:3]), reads=[vk, stk + "b"], writes=[stk + "c", vnk])
                S.act(lambda e: e.activation(out=stt[:, 3:4], in_=stt[:, 2:3], func=AF.Sqrt, bias=epsb2[:, 0:1], scale=1.0 / GW),
                      reads=[stk + "c", "epsb2"], writes=[stk + "d"])
                S.dve(lambda e: e.reciprocal(out=stt[:, 4:5], in_=stt[:, 3:4]), reads=[stk + "d"], writes=[stk + "e"])
                S.dve(lambda e: e.tensor_scalar(out=vh[:], in0=v_sb[:], scalar1=stt[:, 1:2], scalar2=stt[:, 4:5],
                                                op0=ALU.add, op1=ALU.mult),
                      reads=[vk, stk + "b", stk + "e"], writes=[vhk])
                S.pool(lambda e: e.tensor_tensor(out=vh[:], in0=vh[:], in1=lng[:], op=ALU.mult), reads=[vhk, "lng"], writes=[vhk])
                S.dve(lambda e: e.tensor_tensor(out=vn[:], in0=vh[:], in1=lnb[:], op=ALU.add), reads=[vhk, "lnb"], writes=[vnk])
                tb[("g", i)] = (tsl, u_sb, uk, v_sb, vk, vn, vnk)

            def gm2(i):
                tsl, u_sb, uk, v_sb, vk, vn, vnk = tb.pop(("g", i))
                zb, zbk = v_sb, vk
                bra, brak = bra_r.next()
                for g in range(8):
                    S.pe(lambda e, g=g: e.matmul(banks[2][:, g * 64:(g + 1) * 64], lhsT=wsT[:, g, :], rhs=vn[:, g * 64:(g + 1) * 64],
                                                 start=True, stop=True), reads=[vnk, "wsT"], writes=["bank2"])
                S.dve(lambda e: e.tensor_tensor(out=zb[:], in0=banks[2][:], in1=bstab[:], op=ALU.add), reads=["bank2", "bstab"],
                      writes=[zbk])
                S.dve(lambda e: e.tensor_tensor(out=bra[:], in0=zb[:], in1=u_sb[:], op=ALU.mult),
                      reads=[zbk, uk], writes=[brak])
                tb[("t", i)] = (tsl, bra, brak)

            def gm3(i):
                tsl, bra, brak = tb.pop(("t", i))
                for k in range(4):
                    S.pe(lambda e, k=k: e.transpose(out=bankbf(3)[:, k * 128:(k + 1) * 128], in_=bra[:, k * 128:(k + 1) * 128],
                                                    identity=ident_bf[:]), reads=[brak, "ident_bf"], writes=["bank3"])
                S.act(lambda e: e.activation(out=braT[:, :, tsl], in_=bankbf(3)[:, 0:512].rearrange("p (k t) -> p k t", k=4),
                                             func=AF.Copy), reads=["bank3"], writes=[braTk + str(i)])

            gm1(0); gm1(1); gm1(2); gm2(0); gm2(1); gm1(3); gm3(0); gm3(1); gm2(2); gm3(2); gm2(3); gm3(3)
            braTall = [braTk + str(i) for i in range(4)]
            gsl = slice(gi * GT, (gi + 1) * GT)
            for m in range(KD):
                fs = slice(m * 128, (m + 1) * 128)
                bA, bGa, bB, bGb = (4, 5, 6, 7) if m % 2 == 0 else (0, 1, 2, 3)
                for k in range(4):
                    S.pe(lambda e, k=k, fs=fs, bA=bA: e.matmul(banks[bA][:], lhsT=wupa[:, k, fs], rhs=braT[:, k, :], start=(k == 0), stop=(k == 3)),
                         reads=braTall + ["wupa"], writes=["bank%d" % bA])
                for k in range(KD):
                    S.pe(lambda e, k=k, m=m, bGa=bGa: e.matmul(banks[bGa][:], lhsT=win_bf[:, k, 1280 + m * 128:1280 + (m + 1) * 128], rhs=hTg[:, k, :],
                                                               start=(k == 0), stop=(k == KD - 1)), reads=[hTk, "win_bf"], writes=["bank%d" % bGa])
                for k in range(2):
                    S.pe(lambda e, k=k, fs=fs, bB=bB: e.matmul(banks[bB][:], lhsT=wupb[:, k, fs], rhs=brbT[s][:, k, gsl], start=(k == 0), stop=(k == 1)),
                         reads=["wupb"], writes=["bank%d" % bB])
                for k in range(KD):
                    S.pe(lambda e, k=k, m=m, bGb=bGb: e.matmul(banks[bGb][:], lhsT=win_bf[:, k, 2304 + m * 128:2304 + (m + 1) * 128], rhs=hTg[:, k, :],
                                                               start=(k == 0), stop=(k == KD - 1)), reads=[hTk, "win_bf"], writes=["bank%d" % bGb])
                sga, sgak = sg_r.next(); sgb, sgbk = sg_r.next()
                S.act(lambda e, sga=sga, m=m, bGa=bGa: e.activation(out=sga[:], in_=banks[bGa][:], func=AF.Sigmoid, bias=bgate[:, m:m + 1]),
                      reads=["bank%d" % bGa, "bgate"], writes=[sgak])
                S.act(lambda e, sgb=sgb, m=m, bGb=bGb: e.activation(out=sgb[:], in_=banks[bGb][:], func=AF.Sigmoid, bias=bgate[:, 8 + m:9 + m]),
                      reads=["bank%d" % bGb, "bgate"], writes=[sgbk])
                S.dve(lambda e, sga=sga, bA=bA: e.tensor_tensor(out=sga[:], in0=sga[:], in1=banks[bA][:], op=ALU.mult), reads=[sgak, "bank%d" % bA], writes=[sgak])
                S.dve(lambda e, sgb=sgb, bB=bB: e.tensor_tensor(out=sgb[:], in0=sgb[:], in1=banks[bB][:], op=ALU.mult), reads=[sgbk, "bank%d" % bB], writes=[sgbk])
                S.pool(lambda e, sga=sga, sgb=sgb, m=m: e.tensor_tensor(out=mrg[:, m, :], in0=sga[:], in1=sgb[:], op=ALU.add),
                       reads=[sgak, sgbk], writes=[mrgk + str(m)])
            mrgall = [mrgk + str(m) for m in range(KD)]
            nxt = (s, gi + 1) if gi + 1 < NG else ((s + 1, 0) if s + 1 < NS else None)
            if nxt is not None:
                hT_next[nxt] = load_hT(*nxt)

            def wa(i):
                tsl = slice(i * 128, (i + 1) * 128)
                tok0 = s * L + gi * GT + i * 128
                xt, xtk = xt_r2.next(); x1, x1k = x1_r.next(); hn, hnk = hn_r.next(); stt, stk = st_r.next()
                S.dma("sp", lambda e: e.dma_start(out=xt[:], in_=x_d[s, gi * GT + i * 128:gi * GT + (i + 1) * 128, :]), writes=[xtk])
                for hh in range(2):
                    for m in range(KD):
                        S.pe(lambda e, m=m, hh=hh: e.matmul(banks[hh][:], lhsT=mrg[:, m, tsl], rhs=wout[:, m, hh * 512:(hh + 1) * 512],
                                                            start=(m == 0), stop=(m == KD - 1)),
                             reads=mrgall + ["wout"], writes=["bank%d" % hh])
                    S.dve(lambda e, hh=hh: e.tensor_tensor(out=x1[:, hh * 512:(hh + 1) * 512], in0=xt[:, hh * 512:(hh + 1) * 512],
                                                           in1=banks[hh][:], op=ALU.add),
                          reads=[xtk, "bank%d" % hh], writes=[x1k + str(hh)])
                x1all = [x1k + "0", x1k + "1"]
                S.dma("act", lambda e: e.dma_start(out=acc_d[tok0:tok0 + 128, :], in_=x1[:]), reads=x1all,
                      writes=["acc_d%d" % (tok0 // 128)])
                S.act(lambda e: e.activation(out=hn[:, 0:D], in_=x1[:], func=AF.Square, accum_out=stt[:, 0:1]),
                      reads=x1all, writes=[stk + "a", hnk + "h"])
                S.act(lambda e: e.activation(out=stt[:, 1:2], in_=stt[:, 0:1], func=AF.Sqrt, bias=epsb2[:, 0:1], scale=1.0 / D),
                      reads=[stk + "a", "epsb2"], writes=[stk + "b"])
                S.dve(lambda e: e.reciprocal(out=stt[:, 2:3], in_=stt[:, 1:2]), reads=[stk + "b"], writes=[stk + "c"])
                S.dve(lambda e: e.tensor_scalar(out=hn[:, 0:D], in0=x1[:], scalar1=stt[:, 2:3], scalar2=None, op0=ALU.mult),
                      reads=x1all + [stk + "c"], writes=[hnk + "h"])
                S.dma("act", lambda e: e.dma_start(out=h2x_d[tok0:tok0 + 128, :], in_=hn[:, 0:D]), reads=[hnk + "h"],
                      writes=["h2x_dh%d" % (tok0 // 128)])
                tb[("w", i)] = (tok0, hn, hnk, stt, stk)

            def wb(i):
                tok0, hn, hnk, stt, stk = tb[("w", i)]
                h2T, h2Tk = h2T_r.next(); ex, exk = ex_r.next()
                for k in range(KD):
                    S.pe(lambda e, k=k: e.transpose(out=bankbf(2)[:, k * 128:(k + 1) * 128], in_=hn[:, k * 128:(k + 1) * 128],
                                                    identity=ident_bf[:]), reads=[hnk + "h", "ident_bf"], writes=["bank2"])
                S.dve(lambda e: e.tensor_tensor(out=h2T[:], in0=bankbf(2).rearrange("p (k t) -> p k t", k=KD),
                                                in1=g2[:, :].unsqueeze(2).to_broadcast([128, KD, 128]), op=ALU.mult),
                      reads=["bank2", "g2"], writes=[h2Tk])
                tb[("l", i)] = (h2T, h2Tk, ex, exk)

            def wl(i):
                tok0, hn, hnk, stt, stk = tb[("w", i)]
                h2T, h2Tk, ex, exk = tb.pop(("l", i))
                for k in range(KD):
                    S.pe(lambda e, k=k: e.matmul(banks[3][:, 0:NE], lhsT=h2T[:, k, :], rhs=wr[:, k, :], start=(k == 0), stop=(k == KD - 1)),
                         reads=[h2Tk, "wr"], writes=["bank3"])
                S.dve(lambda e: e.tensor_reduce(out=stt[:, 3:4], in_=banks[3][:, 0:NE], axis=AX.X, op=ALU.max), reads=["bank3"],
                      writes=[stk + "d"])
                S.dve(lambda e: e.tensor_scalar(out=stt[:, 4:5], in0=stt[:, 3:4], scalar1=-1.0, scalar2=None, op0=ALU.mult),
                      reads=[stk + "d"], writes=[stk + "e"])
                S.act(lambda e: e.activation(out=ex[:], in_=banks[3][:, 0:NE], func=AF.Exp, bias=stt[:, 4:5], accum_out=stt[:, 5:6]),
                      reads=["bank3", stk + "e"], writes=[exk, stk + "f"])
                S.dve(lambda e: e.reciprocal(out=stt[:, 6:7], in_=stt[:, 5:6]), reads=[stk + "f"], writes=[stk + "g"])
                prt, prk = pr_r.next()
                pview = prt[:, :]
                S.dve(lambda e: e.tensor_scalar(out=pview, in0=ex[:], scalar1=stt[:, 6:7], scalar2=None, op0=ALU.mult),
                      reads=[exk, stk + "g"], writes=[prk])
                S.dma("act", lambda e: e.dma_start(out=pr_d[tok0:tok0 + 128, :], in_=prt[:]), reads=[prk],
                      writes=["pr_d%d" % (tok0 // 128)])
                pwt = pw_r[s][i % 2]; pwk = "pw%d_%d" % (s, i % 2)
                S.dve(lambda e: e.tensor_copy(out=pwt[:, 32 * s:32 * s + NE], in_=pview), reads=[prk], writes=[pwk])
                tb[("p", i)] = (pwt, pwk)

            def wc(i):
                nt = gi * 4 + i
                pwt, pwk = tb.pop(("p", i))
                S.pe(lambda e: e.transpose(out=banks[3][0:32 * NS, 128:256], in_=pwt[:], identity=ident_f[:]), reads=[pwk, "ident_f"],
                     writes=["bank3"])
                S.act(lambda e: e.activation(out=scoresT[32 * s:32 * s + NE, nt * 128:(nt + 1) * 128],
                                             in_=banks[3][32 * s:32 * s + NE, 128:256], func=AF.Copy), reads=["bank3"], writes=["scoresT"])

            wa(0); wa(1); wa(2); wb(0); wa(3); wl(0); wb(1); wc(0); wl(1); wb(2); wc(1); wl(2); wb(3); wc(2); wl(3); wc(3)
            if dbg and s == 0 and gi == 0:
                d_bra = dout("dbg_braT", [4, 128, GT], BF16)
                d_mrg = dout("dbg_mrg", [KD, 128, GT], BF16)
                S.dma("sp", lambda e, braT=braT: e.dma_start(out=d_bra.rearrange("k p t -> p k t"), in_=braT[:]), reads=braTall, writes=["dbgb1"])
                S.dma("sp", lambda e, mrg=mrg: e.dma_start(out=d_mrg.rearrange("k p t -> p k t"), in_=mrg[:]), reads=mrgall, writes=["dbgb2"])

    for s in range(NS):
        phaseB(s)
    S.barrier()
    A.release(mB)
    if dbg:
        d_sc = dout("dbg_scores", [64, L])
        S.dma("sp", lambda e: e.dma_start(out=d_sc[:, :], in_=scoresT[:]), reads=["scoresT"], writes=["dbg5"])
        S.barrier()
    if upto <= 3:
        xo_r = Rot(A, "xo", 2, [128, D], F32)
        for t in range(NTOK // 128):
            xo, xok = xo_r.next()
            S.dma("sp", lambda e, t=t, xo=xo: e.dma_start(out=xo[:], in_=acc_d[t * 128:(t + 1) * 128, :]), writes=[xok])
            S.dma("sp", lambda e, t=t, xo=xo: e.dma_start(out=out_d[t * 128:(t + 1) * 128, :], in_=xo[:]), reads=[xok], writes=["o%d" % t])
        S.emit()
        return nc, S, A

    NR_ = 64
    mC = A.mark()
    lo = A.alloc("lo", [NR_, 1], F32); hi = A.alloc("hi", [NR_, 1], F32); mid = A.alloc("mid", [NR_, 1], F32)
    cnt = A.alloc("cnt", [NR_, 1], F32); flg = A.alloc("flg", [NR_, 1], F32); dlt = A.alloc("dlt", [NR_, 1], F32)
    onec = A.alloc("onec", [NR_, 1], F32)
    junkC = A.alloc("junkC", [NR_, L], F32)
    csum = A.alloc("csum", [NR_, L], F32)
    csT = A.alloc("csT", [128, NT, NR_], F32)
    iota_c = A.alloc("iota_c", [128, CAP], I16)
    ones_bf = A.alloc("ones_bf", [128, 1], BF16)
    idx_f = A.alloc("idx_f", [128, NR_ * NQ], F32)
    le_r = Rot(A, "LE", 4, [128, CAP], BF16)
    S.pool(lambda e: e.memset(lo[:], 0.0), writes=["lo"])
    S.pool(lambda e: e.memset(hi[:], 1.0), writes=["hi"])
    S.pool(lambda e: e.memset(onec[:], 1.0), writes=["onec"])
    S.pool(lambda e: e.memset(ones_bf[:], 1.0), writes=["ones_bf"])
    S.pool(lambda e: e.iota(iota_c[:], pattern=[[1, CAP]], base=0, channel_multiplier=0, allow_small_or_imprecise_dtypes=True),
           writes=["iota_c"])
    for it in range(30):
        S.dve(lambda e: e.tensor_tensor(out=mid[:], in0=lo[:], in1=hi[:], op=ALU.add), reads=["lo", "hi"], writes=["mid"])
        S.dve(lambda e: e.tensor_scalar(out=mid[:], in0=mid[:], scalar1=0.5, scalar2=None, op0=ALU.mult), reads=["mid"], writes=["mid"])
        S.dve(lambda e: e.tensor_scalar(out=junkC[:], in0=scoresT[:], scalar1=mid[:, 0:1], scalar2=None, op0=ALU.is_gt, op1=ALU.add,
                                        accum_out=cnt[:, 0:1]), reads=["scoresT", "mid"], writes=["cnt", "junkC"])
        S.dve(lambda e: e.tensor_scalar(out=flg[:], in0=cnt[:], scalar1=float(CAP), scalar2=None, op0=ALU.is_ge), reads=["cnt"], writes=["flg"])
        S.dve(lambda e: e.tensor_tensor(out=dlt[:], in0=mid[:], in1=lo[:], op=ALU.subtract), reads=["mid", "lo"], writes=["dlt"])
        S.dve(lambda e: e.scalar_tensor_tensor(out=lo[:], in0=dlt[:], scalar=flg[:, 0:1], in1=lo[:], op0=ALU.mult, op1=ALU.add),
              reads=["dlt", "flg", "lo"], writes=["lo"])
        S.dve(lambda e: e.tensor_tensor(out=dlt[:], in0=hi[:], in1=mid[:], op=ALU.subtract), reads=["mid", "hi", "lo"], writes=["dlt"])
        S.dve(lambda e: e.scalar_tensor_tensor(out=hi[:], in0=dlt[:], scalar=flg[:, 0:1], in1=mid[:], op0=ALU.mult, op1=ALU.add),
              reads=["dlt", "flg", "mid"], writes=["hi"])
    S.dve(lambda e: e.tensor_scalar(out=junkC[:], in0=scoresT[:], scalar1=lo[:, 0:1], scalar2=None, op0=ALU.is_gt), reads=["scoresT", "lo"],
          writes=["junkC"])
    S.dve(lambda e: e.tensor_tensor_scan(out=csum[:], data0=onec[:, 0:1].to_broadcast([NR_, L]), data1=junkC[:], initial=0.0,
                                         op0=ALU.mult, op1=ALU.add), reads=["junkC", "onec"], writes=["csum"])
    for n0 in range(0, NT, 8):
        nn = min(8, NT - n0)
        pb = (n0 // 8) % 2
        for n in range(n0, n0 + nn):
            S.pe(lambda e, n=n, n0=n0, pb=pb: e.transpose(out=banks[pb][:, (n - n0) * NR_:(n - n0 + 1) * NR_], in_=csum[0:NR_, n * 128:(n + 1) * 128],
                                                          identity=ident_f[0:NR_, 0:NR_]), reads=["csum", "ident_f"], writes=["bank%d" % pb])
        S.act(lambda e, n0=n0, nn=nn, pb=pb: e.activation(out=csT[:, n0:n0 + nn, :], in_=banks[pb][:, 0:nn * NR_].rearrange("p (a b) -> p a b", b=NR_),
                                                          func=AF.Copy), reads=["bank%d" % pb], writes=["csT"])
    NRL = NS * NE
    assert NRL <= 32 and CAP <= 512
    onehot = A.alloc("onehot", [128, 32, 32], BF16)
    idxrow = A.alloc("idxrow", [32, CAP], F32)
    S.pool(lambda e: e.memset(onehot[:], 0.0), writes=["onehot"])
    for rl in range(NRL):
        S.pool(lambda e, rl=rl: e.memset(onehot[:, rl, rl:rl + 1], 1.0), reads=["onehot"], writes=["onehot"])
    S.dve(lambda e: e.memset(idx_f[:], 0.0), writes=["idx_f"])
    for rl in range(NRL):
        s_ = rl // NE; e_ = rl % NE
        r = 32 * s_ + e_
        for n in range(NT):
            LE, lek = le_r.next()
            S.dve(lambda e, LE=LE, n=n, r=r: e.tensor_scalar(out=LE[:], in0=iota_c[:], scalar1=csT[:, n, r:r + 1], scalar2=None, op0=ALU.is_ge),
                  reads=["iota_c", "csT"], writes=[lek])
            S.pe(lambda e, LE=LE, rl=rl, n=n: e.matmul(banks[2][0:32, 0:CAP], lhsT=onehot[:, rl, :], rhs=LE[:, 0:CAP],
                                                       start=(rl == 0 and n == 0), stop=(rl == NRL - 1 and n == NT - 1)),
                 reads=[lek, "onehot"], writes=["bank2"])
    S.act(lambda e: e.activation(out=idxrow[:], in_=banks[2][0:32, 0:CAP], func=AF.Copy), reads=["bank2"], writes=["idxrow"])
    for q in range(NQ):
        S.pe(lambda e, q=q: e.transpose(out=banks[3][:, q * 32:(q + 1) * 32], in_=idxrow[0:32, q * 128:(q + 1) * 128],
                                        identity=ident_f[0:32, 0:32]), reads=["idxrow", "ident_f"], writes=["bank3"])
    idx_f3 = idx_f[:].rearrange("p (r q) -> p r q", q=NQ)
    for s in range(NS):
        for q in range(NQ):
            S.dve(lambda e, s=s, q=q: e.tensor_scalar(out=idx_f3[:, 32 * s:32 * s + NE, q],
                                                     in0=banks[3][:, q * 32 + NE * s:q * 32 + NE * s + NE],
                                                     scalar1=float(s * L), scalar2=None, op0=ALU.add),
                  reads=["bank3", "idx_f"], writes=["idx_f"])
    S.dve(lambda e: e.tensor_copy(out=idx_i[:], in_=idx_f[:]), reads=["idx_f"], writes=["idx_i"])
    if dbg:
        d_idx = dout("dbg_idx", [128, NR_ * NQ], I32)
        S.dma("sp", lambda e: e.dma_start(out=d_idx[:, :], in_=idx_i[:]), reads=["idx_i"], writes=["dbg6"])
    S.barrier()
    A.release(m0)

    mD = A.mark()
    g2d = A.alloc("g2d", [128, KD], F32)
    S.dma("sp", lambda e: e.dma_start(out=g2d[:], in_=g2_d[:, :]), writes=["g2d"])
    stg_r = Rot(A, "stgD", 3, [128, KD, 512], F32)
    wg_r = Rot(A, "wg_bf", 2, [128, KD, 512], BF16)
    wu_r = Rot(A, "wu_bf", 2, [128, KD, 512], BF16)
    wd_bf = A.alloc("wd_bf", [128, 16, D], BF16)
    xsT = [A.alloc("xsT%d" % s, [128, KD, CAP], BF16) for s in range(NS)]
    actT = [A.alloc("actT%d" % s, [128, 16, CAP], BF16) for s in range(NS)]
    xsg_r = Rot(A, "xsg", NS * NQ, [128, D], BF16)
    pg_r = Rot(A, "pg", NS * NQ, [128, NE], F32)
    ysb_r = Rot(A, "ysb", 3, [128, D], F32)
    sil_r = Rot(A, "sil", 2, [128, CAP], F32)
    gts2 = [A.alloc("gts%d" % i, [128, NS * NQ], F32) for i in range(2)]
    cast_i = [0]

    def cast(dst, src, reads, writes):
        k = cast_i[0] % 3
        cast_i[0] += 1
        if k == 0:
            S.act(lambda e: e.activation(out=dst, in_=src, func=AF.Copy), reads=reads, writes=writes)
        elif k == 1:
            S.dve(lambda e: e.tensor_copy(out=dst, in_=src), reads=reads, writes=writes)
        else:
            S.pool(lambda e: e.tensor_copy(out=dst, in_=src), reads=reads, writes=writes)

    units = []
    for e_ in range(NE):
        for pc in range(4):
            units.append((e_, "gu", pc))
        units.append((e_, "d", 0))
    ubuf = {}

    def load_unit(u):
        e_, kind, pc = u
        if kind == "gu":
            wg, wgk = wg_r.next(); wu, wuk = wu_r.next()
            ubuf[u] = (wg, wgk, wu, wuk)
            for (w_, wk_, src_d) in ((wg, wgk, wg_d), (wu, wuk, wu_d)):
                st_, stk = stg_r.next()
                S.dma("sp", lambda e, st_=st_, src_d=src_d: e.dma_start(
                    out=st_[:], in_=src_d[e_, :, pc * 512:(pc + 1) * 512].rearrange("(k p) f -> p k f", p=128)), writes=[stk])
                cast(w_[:], st_[:], [stk], [wk_])
        else:
            for fh in range(2):
                for hh in range(2):
                    st_, stk = stg_r.next()
                    S.dma("sp", lambda e, st_=st_, fh=fh, hh=hh: e.dma_start(
                        out=st_[:], in_=wd_d[e_, fh * 1024:(fh + 1) * 1024, hh * 512:(hh + 1) * 512].rearrange("(k p) c -> p k c", p=128)),
                        writes=[stk])
                    cast(wd_bf[:, fh * 8:(fh + 1) * 8, hh * 512:(hh + 1) * 512], st_[:], [stk], ["wd_bf"])

    gbuf = {}

    def gather_dma(e_):
        gts = gts2[e_ % 2]
        for s in range(NS):
            r = 32 * s + e_
            for q in range(NQ):
                col = r * NQ + q
                xsg, xsgk = xsg_r.next()
                gbuf[(e_, s, q)] = (xsg, xsgk)
                S.dma("pool", lambda e, xsg=xsg, col=col: e.indirect_dma_start(
                    out=xsg[:], out_offset=None, in_=h2x_d[:, :], in_offset=bass.IndirectOffsetOnAxis(ap=idx_i[:, col:col + 1], axis=0)),
                    reads=["idx_i"], writes=[xsgk])
                pg, pgk = pg_r.next()
                S.dma("pool", lambda e, pg=pg, col=col: e.indirect_dma_start(
                    out=pg[:], out_offset=None, in_=pr_d[:, :], in_offset=bass.IndirectOffsetOnAxis(ap=idx_i[:, col:col + 1], axis=0)),
                    reads=["idx_i"], writes=[pgk])
                S.dve(lambda e, pg=pg, s=s, q=q, gts=gts: e.tensor_copy(out=gts[:, s * NQ + q:s * NQ + q + 1], in_=pg[:, e_:e_ + 1]),
                      reads=[pgk], writes=["gts%d_%d_%d" % (e_ % 2, s, q)])

    def gather_tr(e_):
        for s in range(NS):
            for q in range(NQ):
                xsg, xsgk = gbuf.pop((e_, s, q))
                pb = 6 + (q % 2)
                for k in range(KD):
                    S.pe(lambda e, xsg=xsg, k=k, pb=pb: e.transpose(out=bankbf(pb)[:, k * 128:(k + 1) * 128], in_=xsg[:, k * 128:(k + 1) * 128],
                                                                   identity=ident_bf[:]), reads=[xsgk, "ident_bf"], writes=["bank%d" % pb])
                S.dve(lambda e, s=s, q=q, pb=pb: e.tensor_tensor(out=xsT[s][:, :, q * 128:(q + 1) * 128],
                                                                 in0=bankbf(pb).rearrange("p (k t) -> p k t", k=KD),
                                                                 in1=g2d[:, :].unsqueeze(2).to_broadcast([128, KD, 128]), op=ALU.mult),
                      reads=["bank%d" % pb, "g2d"], writes=["xsT%d" % s])

    gu_i = [0]

    def compute_unit(u):
        e_, kind, pc = u
        if kind == "gu":
            wg, wgk, wu, wuk = ubuf[u]
            for s in range(NS):
                for ft in range(4):
                    fs = slice(ft * 128, (ft + 1) * 128)
                    ba = gu_i[0] % 2; bu_ = 2 + gu_i[0] % 2
                    gu_i[0] += 1
                    for k in range(KD):
                        S.pe(lambda e, wg=wg, k=k, fs=fs, s=s, ba=ba: e.matmul(banks[ba][:, 0:CAP], lhsT=wg[:, k, fs], rhs=xsT[s][:, k, :],
                                                                                start=(k == 0), stop=(k == KD - 1)),
                             reads=[wgk, "xsT%d" % s], writes=["bank%d" % ba])
                    for k in range(KD):
                        S.pe(lambda e, wu=wu, k=k, fs=fs, s=s, bu_=bu_: e.matmul(banks[bu_][:, 0:CAP], lhsT=wu[:, k, fs], rhs=xsT[s][:, k, :],
                                                                                  start=(k == 0), stop=(k == KD - 1)),
                             reads=[wuk, "xsT%d" % s], writes=["bank%d" % bu_])
                    sil, silk = sil_r.next()
                    S.act(lambda e, sil=sil, ba=ba: e.activation(out=sil[:], in_=banks[ba][:, 0:CAP], func=AF.Silu), reads=["bank%d" % ba], writes=[silk])
                    fc = pc * 4 + ft
                    S.dve(lambda e, sil=sil, bu_=bu_, s=s, fc=fc: e.tensor_tensor(out=actT[s][:, fc, :], in0=sil[:], in1=banks[bu_][:, 0:CAP], op=ALU.mult),
                          reads=[silk, "bank%d" % bu_], writes=["actT%d_%d" % (s, fc)])
        else:
            gts = gts2[e_ % 2]
            for s in range(NS):
                r = 32 * s + e_
                for q in range(NQ):
                    col = r * NQ + q
                    ysb, ysbk = ysb_r.next()
                    for hh in range(2):
                        yb = 4 + hh
                        for fc in range(16):
                            S.pe(lambda e, s=s, q=q, hh=hh, fc=fc, yb=yb: e.matmul(banks[yb][:], lhsT=actT[s][:, fc, q * 128:(q + 1) * 128],
                                                                                   rhs=wd_bf[:, fc, hh * 512:(hh + 1) * 512], start=(fc == 0), stop=(fc == 15)),
                                 reads=["actT%d_%d" % (s, fc), "wd_bf"], writes=["bank%d" % yb])
                        if hh == 0:
                            S.dve(lambda e, ysb=ysb, s=s, q=q, yb=yb, gts=gts: e.tensor_scalar(out=ysb[:, 0:512], in0=banks[yb][:],
                                                                                     scalar1=gts[:, s * NQ + q:s * NQ + q + 1], scalar2=None, op0=ALU.mult),
                                  reads=["bank%d" % yb, "gts%d_%d_%d" % (e_ % 2, s, q)], writes=[ysbk + "0"])
                        else:
                            S.act(lambda e, ysb=ysb, s=s, q=q, yb=yb, gts=gts: e.activation(out=ysb[:, 512:1024], in_=banks[yb][:], func=AF.Copy,
                                                                                  scale=gts[:, s * NQ + q:s * NQ + q + 1]),
                                  reads=["bank%d" % yb, "gts%d_%d_%d" % (e_ % 2, s, q)], writes=[ysbk + "1"])
                    S.dma("pool", lambda e, ysb=ysb, col=col: e.indirect_dma_start(
                        out=acc_d[:, :], out_offset=bass.IndirectOffsetOnAxis(ap=idx_i[:, col:col + 1], axis=0), in_=ysb[:], in_offset=None,
                        compute_op=ALU.add), reads=[ysbk + "0", ysbk + "1", "idx_i"], writes=["acc_all"])

    load_unit(units[0])
    gather_dma(0)
    for ui, u in enumerate(units):
        if u[1] == "gu" and u[2] == 0:
            gather_tr(u[0])
        if u[1] == "gu" and u[2] == 3 and u[0] + 1 < NE:
            gather_dma(u[0] + 1)
        if ui + 1 < len(units):
            load_unit(units[ui + 1])
        compute_unit(u)
    S.barrier()
    A.release(mD)

    gf = A.alloc("gf", [128, D], F32)
    epsb3 = A.alloc("epsb3", [128, 1], F32)
    S.pool(lambda e: e.memset(epsb3[:], EPS), writes=["epsb3"])
    S.dma("sp", lambda e: e.dma_start(out=gf[:], in_=gf_d[:, :]), writes=["gf"])
    xa_r = Rot(A, "xa", 6, [128, D], F32)
    xo_r = Rot(A, "xo", 6, [128, D], F32)
    se_r = Rot(A, "se", 6, [128, 4], F32)
    junkE = A.alloc("junkE", [128, D], BF16)
    for t in range(NTOK // 128):
        xa, xak = xa_r.next(); xo, xok = xo_r.next(); se, sek = se_r.next()
        S.dma("sp", lambda e, t=t, xa=xa: e.dma_start(out=xa[:], in_=acc_d[t * 128:(t + 1) * 128, :]), writes=[xak])
        S.act(lambda e, xa=xa, xo=xo, se=se: e.activation(out=xo[:], in_=xa[:], func=AF.Square, accum_out=se[:, 0:1]), reads=[xak],
              writes=[sek + "a", xok])
        S.act(lambda e, se=se: e.activation(out=se[:, 1:2], in_=se[:, 0:1], func=AF.Sqrt, bias=epsb3[:, 0:1], scale=1.0 / D),
              reads=[sek + "a", "epsb3"], writes=[sek + "b"])
        S.dve(lambda e, se=se: e.reciprocal(out=se[:, 2:3], in_=se[:, 1:2]), reads=[sek + "b"], writes=[sek + "c"])
        S.dve(lambda e, xa=xa, xo=xo, se=se: e.scalar_tensor_tensor(out=xo[:], in0=xa[:], scalar=se[:, 2:3], in1=gf[:], op0=ALU.mult, op1=ALU.mult),
              reads=[xak, sek + "c", "gf"], writes=[xok])
        S.dma("act", lambda e, t=t, xo=xo: e.dma_start(out=out_d[t * 128:(t + 1) * 128, :], in_=xo[:]), reads=[xok], writes=["o%d" % t])

    S.emit()
    return nc, S, A


def _f32(a):
    return np.ascontiguousarray(np.asarray(a, dtype=np.float32))


def prep_shared(inp):
    o = {}
    o["g1"] = _f32(inp["norm1_g"][0].reshape(KD, 128).T)
    o["w_in"] = _f32(inp["w_in"][0])
    o["b_gate"] = _f32(inp["b_gate"][0].reshape(16, 128).T)
    o["ln_g"] = _f32(np.broadcast_to(inp["gmlp_ln_g"][0][None, :], (128, GW)))
    o["ln_b"] = _f32(np.broadcast_to(inp["gmlp_ln_b"][0][None, :], (128, GW)))
    o["wsT"] = _f32(np.transpose(inp["gmlp_w_s"][0], (2, 0, 1)))
    o["bstab"] = _f32(np.repeat(inp["gmlp_b_s"][0].T[:, :, None], 64, axis=2).reshape(128, GW))
    lam_re = np.asarray(inp["s5_lam_re"][0]); lam_im = np.asarray(inp["s5_lam_im"][0]); log_dt = np.asarray(inp["s5_log_dt"][0])
    b_re = np.asarray(inp["s5_b_re"][0]); b_im = np.asarray(inp["s5_b_im"][0])
    c_re = np.asarray(inp["s5_c_re"][0]); c_im = np.asarray(inp["s5_c_im"][0])
    def col(a3):
        return _f32(a3.reshape(2, 8, 128).transpose(2, 0, 1).reshape(128, 16))
    o["lamre_c"] = col(lam_re)
    o["lamim_c"] = col(lam_im)
    ldt = np.repeat(log_dt[:, :, None], 64, axis=2)
    o["logdt_c"] = col(ldt)
    def row(a3):
        return _f32(np.broadcast_to(a3.reshape(1, 2048), (128, 2048)))
    o["lamre_r"] = row(lam_re); o["lamim_r"] = row(lam_im); o["logdt_r"] = row(ldt)
    def braw(bm):
        outp = np.zeros((128, 2, 8, 128), np.float32)
        for j in range(8):
            for gg in range(2):
                g = 2 * j + gg
                cl = 16 * (g % 8)
                outp[cl:cl + 16, :, j, gg * 64:(gg + 1) * 64] = np.transpose(bm[:, g, :, :], (2, 0, 1))
        return _f32(outp.reshape(128, 2048))
    o["braw_re"] = braw(b_re); o["braw_im"] = braw(b_im)
    def craw(cm):
        outp = np.zeros((128, 2, 8, 128), np.float32)
        for j in range(8):
            for gg in range(2):
                g = 2 * j + gg
                cl = 16 * (g % 8)
                outp[gg * 64:(gg + 1) * 64, :, j, cl:cl + 16] = np.transpose(cm[:, g, :, :], (2, 0, 1))
        return _f32(outp.reshape(128, 2048))
    o["craw_re"] = craw(c_re); o["craw_im"] = craw(c_im)
    o["dskip"] = _f32(np.asarray(inp["s5_d"][0]).reshape(2, 128).T)
    o["w_glu"] = _f32(inp["s5_w_glu"][0])
    o["b_glu"] = _f32(np.asarray(inp["s5_b_glu"][0]).reshape(2, 128).T)
    o["w_up_a"] = _f32(inp["w_up_a"][0]); o["w_up_b"] = _f32(inp["w_up_b"][0]); o["w_out"] = _f32(inp["w_out"][0])
    o["g2"] = _f32(np.asarray(inp["norm2_g"][0]).reshape(KD, 128).T)
    o["w_router"] = _f32(inp["w_router"][0])
    o["w_gate"] = _f32(inp["w_gate"][0]); o["w_up"] = _f32(inp["w_up"][0]); o["w_down"] = _f32(inp["w_down"][0])
    o["gf"] = _f32(np.broadcast_to(np.asarray(inp["final_g"])[None, :], (128, D)))
    return o


def kernel(**inputs):
    x = np.asarray(inputs["x"], dtype=np.float32)
    B, L, _ = x.shape
    NCORE = 8
    NS = B // NCORE
    shared = prep_shared(inputs)
    nc, S, A = build(NS, L)
    in_maps = []
    for c in range(NCORE):
        m = dict(shared)
        m["x"] = np.ascontiguousarray(x[c * NS:(c + 1) * NS])
        in_maps.append(m)
    res = run_bass_kernel_spmd(nc, in_maps, core_ids=list(range(NCORE)))
    outs = [np.asarray(r["out"]).reshape(NS, L, D) for r in res.results]
    return np.concatenate(outs, axis=0).astype(np.float32)
```

```python
import contextlib
import math
import numpy as np
import concourse.bass as bass
import concourse.mybir as mybir
from concourse.bass_utils import run_bass_kernel_spmd

F32 = mybir.dt.float32
BF16 = mybir.dt.bfloat16
I32 = mybir.dt.int32
I16 = mybir.dt.int16
AF = mybir.ActivationFunctionType
ALU = mybir.AluOpType
AX = mybir.AxisListType

D = 1024
KD = 8
GW = 512
S5W = 256
PT = 3328
NE = 16
FF = 2048
EPS = 1e-6
TB = 512
GT = 512
SB_BASE = 16512
SB_END = 229376

ENGS = ("pe", "act", "dve", "pool", "sp")
NDMA_SEMS = 8


class Op:
    __slots__ = ("id", "eng", "fn", "deps", "dma", "signal", "seq", "sem_idx", "waits", "prewait")

    def __init__(self, id, eng, fn, dma):
        self.id = id
        self.eng = eng
        self.fn = fn
        self.dma = dma
        self.deps = set()
        self.signal = False
        self.seq = 0
        self.sem_idx = -1
        self.waits = []
        self.prewait = None


class Sched:
    def __init__(self, nc):
        self.nc = nc
        self.ops = []
        self.last_writer = {}
        self.readers = {}
        self.last_on_eng = {}

    def add(self, eng, fn, reads=(), writes=(), dma=False):
        op = Op(len(self.ops), eng, fn, dma)
        deps = op.deps
        for t in reads:
            w = self.last_writer.get(t)
            if w is not None:
                deps.add(w)
            if t.startswith("bank"):
                for r in self.readers.get(t, ()):
                    if self.ops[r].eng != eng:
                        deps.add(r)
        for t in writes:
            w = self.last_writer.get(t)
            if w is not None:
                deps.add(w)
            for r in self.readers.get(t, ()):
                deps.add(r)
        for t in reads:
            self.readers.setdefault(t, []).append(op.id)
        for t in writes:
            self.last_writer[t] = op.id
            self.readers[t] = []
        deps.discard(op.id)
        self.ops.append(op)
        self.last_on_eng[eng] = op.id
        return op

    def pe(self, fn, reads=(), writes=()):
        return self.add("pe", fn, reads, writes)

    def act(self, fn, reads=(), writes=()):
        return self.add("act", fn, reads, writes)

    def dve(self, fn, reads=(), writes=()):
        return self.add("dve", fn, reads, writes)

    def pool(self, fn, reads=(), writes=()):
        return self.add("pool", fn, reads, writes)

    def any(self, eng, fn, reads=(), writes=()):
        return self.add(eng, fn, reads, writes)

    def dma(self, eng, fn, reads=(), writes=()):
        return self.add(eng, fn, reads, writes, dma=True)

    def barrier(self):
        pend = set()
        for t, w in self.last_writer.items():
            pend.add(w)
        for t, rs in self.readers.items():
            pend.update(rs)
        pend.update(self.last_on_eng.values())
        for op in self.ops:
            if op.dma:
                pend.add(op.id)
        ids = []
        for e in ENGS:
            op = Op(len(self.ops), e, (lambda eng: eng.nop()), False)
            op.deps = set(pend)
            self.ops.append(op)
            self.last_on_eng[e] = op.id
            ids.append(op.id)
        self.last_writer = {}
        self.readers = {}
        self._dma_done_upto = len(self.ops)

    def emit(self, final_wait_eng="sp"):
        nc = self.nc
        ops = self.ops

        def skip(dop, op):
            return dop.eng == "pe" and op.eng == "pe" and not dop.dma and not op.dma

        for op in ops:
            for d in op.deps:
                dop = ops[d]
                if skip(dop, op):
                    continue
                dop.signal = True
        for op in ops:
            if op.dma:
                op.signal = True
        eng_cnt = {e: 0 for e in ENGS}
        dma_cnt = {e: [0] * NDMA_SEMS for e in ENGS}
        dma_rr = {e: 0 for e in ENGS}
        for op in ops:
            if op.dma:
                k = dma_rr[op.eng] % NDMA_SEMS
                dma_rr[op.eng] += 1
                op.sem_idx = k
                op.prewait = dma_cnt[op.eng][k]
                dma_cnt[op.eng][k] += 16
                op.seq = dma_cnt[op.eng][k]
            elif op.signal:
                eng_cnt[op.eng] += 1
                op.seq = eng_cnt[op.eng]
        waited = {e: {} for e in ENGS}
        for op in ops:
            w = waited[op.eng]
            need = {}
            for d in op.deps:
                dop = ops[d]
                if skip(dop, op):
                    continue
                key = ("d", dop.eng, dop.sem_idx) if dop.dma else ("e", dop.eng)
                if dop.seq > need.get(key, 0):
                    need[key] = dop.seq
            if op.dma and op.prewait:
                key = ("d", op.eng, op.sem_idx)
                if op.prewait > need.get(key, 0):
                    need[key] = op.prewait
            for key, v in need.items():
                if w.get(key, 0) >= v:
                    continue
                w[key] = v
                op.waits.append((key, v))
        self.stats = dict(n_ops=len(ops), eng_cnt=dict(eng_cnt),
                          per_eng={e: sum(1 for o in ops if o.eng == e) for e in ENGS})
        with contextlib.ExitStack() as st:
            esem = {e: st.enter_context(nc.semaphore("se_" + e)) for e in ENGS}
            dsem = {e: [st.enter_context(nc.semaphore("sd_%s%d" % (e, i))) for i in range(NDMA_SEMS)]
                    for e in ENGS if dma_rr[e] > 0}
            block = st.enter_context(nc.Block())

            def semof(key):
                if key[0] == "e":
                    return esem[key[1]]
                return dsem[key[1]][key[2]]

            def run_engine(ename, eng):
                for op in ops:
                    if op.eng != ename:
                        continue
                    for key, v in op.waits:
                        eng.wait_ge(semof(key), v)
                    ins = op.fn(eng)
                    if op.dma:
                        ins.then_inc(dsem[ename][op.sem_idx], 16)
                    elif op.signal:
                        ins.then_inc(esem[ename], 1)
                if ename == final_wait_eng:
                    for e2 in dsem:
                        for i in range(NDMA_SEMS):
                            if dma_cnt[e2][i] > 0:
                                eng.wait_ge(dsem[e2][i], dma_cnt[e2][i])
                    for e2 in ENGS:
                        if eng_cnt[e2] > 0:
                            eng.wait_ge(esem[e2], eng_cnt[e2])

            @block.tensor
            def _(eng):
                run_engine("pe", eng)

            @block.scalar
            def _(eng):
                run_engine("act", eng)

            @block.vector
            def _(eng):
                run_engine("dve", eng)

            @block.gpsimd
            def _(eng):
                run_engine("pool", eng)

            @block.sync
            def _(eng):
                run_engine("sp", eng)


class Arena:
    def __init__(self, nc):
        self.nc = nc
        self.off = SB_BASE
        self.n = 0
        self.peak = SB_BASE

    def alloc(self, name, shape, dt):
        esz = 4 if dt in (F32, I32) else 2
        nbytes = esz * int(np.prod(shape[1:]))
        off = (self.off + 31) // 32 * 32
        assert off + nbytes <= SB_END, ("SBUF overflow", name, off, nbytes)
        self.n += 1
        t = self.nc.alloc_sbuf_tensor_at("%s_%d" % (name, self.n), list(shape), dt, offset=off)
        self.off = off + nbytes
        self.peak = max(self.peak, self.off)
        return t

    def mark(self):
        return self.off

    def release(self, m):
        self.off = m


class Rot:
    def __init__(self, arena, name, n, shape, dt):
        self.t = [arena.alloc("%s%d" % (name, i), shape, dt) for i in range(n)]
        self.name = name
        self.i = -1
        self.n = n

    def next(self):
        self.i += 1
        k = self.i % self.n
        return self.t[k], "%s#%d" % (self.name, k)


import os
HTQ = os.environ.get('HTQ', 'pool')
STQ = os.environ.get('STQ', 'pool')
P1STOP = int(os.environ.get('P1STOP', '9'))
S5POOL = int(os.environ.get('S5POOL', '2'))
HTGB = int(os.environ.get('HTGB', '1'))
VAR = os.environ.get('VAR', '')
TWO_PI_HI = 6.28125
TWO_PI_LO = 2.0 * math.pi - 6.28125
PI_CLAMP = 3.1415925


def build(NS, L, upto=99, dbg=False, cap=None):
    nc = bass.Bass("TRN2", target_bir_lowering=False)
    NG = L // GT
    NB = L // TB
    NT = L // 128
    CAP = cap if cap is not None else 2 * L // NE
    NQ = CAP // 128
    NTOK = NS * L

    def din(name, shape, dt=F32):
        return nc.dram_tensor(name, list(shape), dt, kind="ExternalInput").ap()

    def dscr(name, shape, dt):
        return nc.dram_tensor(name, list(shape), dt, kind="Internal").ap()

    x_d = din("x", [NS, L, D])
    g1_d = din("g1", [128, KD])
    win_d = din("w_in", [D, PT])
    bgate_d = din("b_gate", [128, 16])
    lng_d = din("ln_g", [128, GW])
    lnb_d = din("ln_b", [128, GW])
    wsT_d = din("wsT", [128, 8, 128])
    bstab_d = din("bstab", [128, GW])
    lamre_c_d = din("lamre_c", [128, 16])
    lamim_c_d = din("lamim_c", [128, 16])
    logdt_c_d = din("logdt_c", [128, 16])
    lamre_r_d = din("lamre_r", [128, 2048])
    lamim_r_d = din("lamim_r", [128, 2048])
    logdt_r_d = din("logdt_r", [128, 2048])
    braw_re_d = din("braw_re", [128, 2048])
    braw_im_d = din("braw_im", [128, 2048])
    craw_re_d = din("craw_re", [128, 2048])
    craw_im_d = din("craw_im", [128, 2048])
    dskip_d = din("dskip", [128, 2])
    wglu_d = din("w_glu", [S5W, S5W])
    bglu_d = din("b_glu", [128, 2])
    wupa_d = din("w_up_a", [GW, D])
    wupb_d = din("w_up_b", [S5W, D])
    wout_d = din("w_out", [D, D])
    g2_d = din("g2", [128, KD])
    wr_d = din("w_router", [D, NE])
    wg_d = din("w_gate", [NE, D, FF])
    wu_d = din("w_up", [NE, D, FF])
    wd_d = din("w_down", [NE, FF, D])
    gf_d = din("gf", [128, D])
    out_d = nc.dram_tensor("out", [NTOK, D], F32, kind="ExternalOutput").ap()

    hT_d = dscr("hT_scr", [NS, KD, 128, L], BF16)
    acc_d = dscr("acc_scr", [NTOK, D], F32)
    h2_d = dscr("h2_scr", [NTOK, D], BF16)
    dbg_outs = {}

    def dout(name, shape, dt=F32):
        t = nc.dram_tensor(name, list(shape), dt, kind="ExternalOutput").ap()
        dbg_outs[name] = t
        return t

    S = Sched(nc)
    A = Arena(nc)
    banks = [nc.alloc_psum_tensor("bank%d" % i, [128, 512], F32) for i in range(8)]

    def bankbf(i):
        return banks[i][:].bitcast(BF16)

    ident_bf = A.alloc("ident_bf", [128, 128], BF16)
    ident_f = A.alloc("ident_f", [128, 128], F32)
    halfpi = A.alloc("halfpi", [128, 1], F32)
    S.pool(lambda e: e.memset(ident_f[:], 1.0), writes=["ident_f"])
    S.pool(lambda e: e.affine_select(out=ident_f[:], in_=ident_f[:], pattern=[[-1, 128]], compare_op=ALU.is_equal,
                                    fill=0.0, base=0, channel_multiplier=1), reads=["ident_f"], writes=["ident_f"])
    S.dve(lambda e: e.tensor_copy(out=ident_bf[:], in_=ident_f[:]), reads=["ident_f"], writes=["ident_bf"])
    S.pool(lambda e: e.memset(halfpi[:], math.pi / 2.0), writes=["halfpi"])
    idx_i = A.alloc("idx_i", [128, 64 * NQ], I32)
    m0 = A.mark()
    brbT = [A.alloc("brbT%d" % s, [128, 2, L], BF16) for s in range(NS)]

    mA = A.mark()
    wins5 = A.alloc("wins5", [128, KD, S5W], BF16)
    g1 = A.alloc("g1", [128, KD], F32)
    bbT_re = A.alloc("bbT_re", [128, 16, 128], BF16)
    bbT_im = A.alloc("bbT_im", [128, 16, 128], BF16)
    cT_re = A.alloc("cT_re", [128, 16, 128], BF16)
    cT_imn = A.alloc("cT_imn", [128, 16, 128], BF16)
    cosT = A.alloc("cosT", [128, 16, TB], F32)
    sinT = A.alloc("sinT", [128, 16, TB], F32)
    rcol = A.alloc("rcol", [128, 16], F32)
    pblk_r = A.alloc("pblk_r", [128, 16], F32)
    pblk_i = A.alloc("pblk_i", [128, 16], F32)
    pblk_in = A.alloc("pblk_in", [128, 16], F32)
    dskip = A.alloc("dskip", [128, 2], F32)
    wglu = A.alloc("wglu", [128, 2, S5W], BF16)
    bglu = A.alloc("bglu", [128, 2], F32)

    S.dma("sp", lambda e: e.dma_start(out=g1[:], in_=g1_d[:, :]), writes=["g1"])
    S.dma("sp", lambda e: e.dma_start(out=dskip[:], in_=dskip_d[:, :]), writes=["dskip"])
    S.dma("sp", lambda e: e.dma_start(out=bglu[:], in_=bglu_d[:, :]), writes=["bglu"])

    mS = A.mark()
    stg = A.alloc("stg_s5w", [128, KD, S5W], F32)
    S.dma("sp", lambda e: e.dma_start(out=stg[:], in_=win_d[:, 2 * GW:2 * GW + S5W].rearrange("(k p) c -> p k c", p=128)),
          writes=["stg"])
    for k in range(KD):
        S.dve(lambda e, k=k: e.tensor_scalar(out=wins5[:, k, :], in0=stg[:, k, :], scalar1=g1[:, k:k + 1], scalar2=None,
                                             op0=ALU.mult), reads=["stg", "g1"], writes=["wins5"])
    stg2 = A.alloc("stg_glu", [128, 2, S5W], F32)
    S.dma("sp", lambda e: e.dma_start(out=stg2[:], in_=wglu_d[:, :].rearrange("(k p) c -> p k c", p=128)), writes=["stg2"])
    S.dve(lambda e: e.tensor_copy(out=wglu[:], in_=stg2[:]), reads=["stg2"], writes=["wglu"])

    def s5_scalars(tag, F, lamre_d, lamim_d, logdt_d):
        T = {}

        def al(n):
            T[n] = A.alloc(tag + n, [128, F], F32)
            return T[n]

        lamre = al("lamre"); lamim = al("lamim"); logdt = al("logdt")
        S.dma("sp", lambda e: e.dma_start(out=lamre[:], in_=lamre_d[:, :]), writes=[tag + "lamre"])
        S.dma("sp", lambda e: e.dma_start(out=lamim[:], in_=lamim_d[:, :]), writes=[tag + "lamim"])
        S.dma("sp", lambda e: e.dma_start(out=logdt[:], in_=logdt_d[:, :]), writes=[tag + "logdt"])
        dt = al("dt"); r = al("r"); th = al("th"); y = al("y"); kk = al("kk")
        s1 = al("s1"); c1 = al("c1"); t1 = al("t1"); t2 = al("t2"); cre = al("cre"); cim = al("cim")
        tk = lambda n: tag + n

        def dv(fn, reads, writes):
            S.dve(fn, reads=[tk(n) for n in reads], writes=[tk(n) for n in writes])

        def ac(fn, reads, writes):
            S.act(fn, reads=[tk(n) for n in reads], writes=[tk(n) for n in writes])

        def dve_exp(out_t, on, in_t, inn, offset, deg):
            dv(lambda e: e.tensor_scalar(out=t2[:], in0=in_t[:], scalar1=float(offset), scalar2=None, op0=ALU.add), [inn], ["t2"])
            dv(lambda e: e.tensor_scalar(out=out_t[:], in0=t2[:], scalar1=1.0 / math.factorial(deg), scalar2=None, op0=ALU.mult),
               ["t2"], [on])
            for kq in range(deg - 1, 0, -1):
                dv(lambda e, kq=kq: e.scalar_tensor_tensor(out=out_t[:], in0=out_t[:], scalar=1.0 / math.factorial(kq), in1=t2[:],
                                                           op0=ALU.add, op1=ALU.mult), [on, "t2"], [on])
            dv(lambda e: e.tensor_scalar(out=out_t[:], in0=out_t[:], scalar1=1.0, scalar2=math.exp(-offset), op0=ALU.add,
                                         op1=ALU.mult), [on], [on])
        dve_exp(dt, "dt", logdt, "logdt", 6.9375, 27)
        dv(lambda e: e.tensor_tensor(out=t1[:], in0=lamre[:], in1=dt[:], op=ALU.mult), ["lamre", "dt"], ["t1"])
        dv(lambda e: e.tensor_scalar(out=t1[:], in0=t1[:], scalar1=-1.0, scalar2=None, op0=ALU.mult), ["t1"], ["t1"])
        dve_exp(r, "r", t1, "t1", 0.0, 8)
        dv(lambda e: e.reciprocal(out=r[:], in_=r[:]), ["r"], ["r"])
        dv(lambda e: e.tensor_tensor(out=th[:], in0=lamim[:], in1=dt[:], op=ALU.mult), ["lamim", "dt"], ["th"])
        dv(lambda e: e.tensor_scalar(out=y[:], in0=th[:], scalar1=1.0 / (2.0 * math.pi), scalar2=None, op0=ALU.mult),
           ["th"], ["y"])
        dv(lambda e: e.tensor_scalar(out=kk[:], in0=y[:], scalar1=0.5, scalar2=None, op0=ALU.is_gt), ["y"], ["kk"])
        for m in range(1, 8):
            dv(lambda e, m=m: e.scalar_tensor_tensor(out=kk[:], in0=y[:], scalar=m + 0.5, in1=kk[:], op0=ALU.is_gt,
                                                     op1=ALU.add), ["y", "kk"], ["kk"])
        dv(lambda e: e.scalar_tensor_tensor(out=th[:], in0=kk[:], scalar=-TWO_PI_HI, in1=th[:], op0=ALU.mult, op1=ALU.add),
           ["kk", "th"], ["th"])
        dv(lambda e: e.scalar_tensor_tensor(out=th[:], in0=kk[:], scalar=-TWO_PI_LO, in1=th[:], op0=ALU.mult, op1=ALU.add),
           ["kk", "th"], ["th"])
        dv(lambda e: e.tensor_scalar(out=th[:], in0=th[:], scalar1=PI_CLAMP, scalar2=-PI_CLAMP, op0=ALU.min, op1=ALU.max),
           ["th"], ["th"])
        ac(lambda e: e.activation(out=s1[:], in_=th[:], func=AF.Sin), ["th"], ["s1"])
        dv(lambda e: e.scalar_tensor_tensor(out=t2[:], in0=th[:], scalar=-1.0, in1=th[:], op0=ALU.mult, op1=ALU.max), ["th"], ["t2"])
        S.act(lambda e: e.activation(out=c1[:], in_=t2[:], func=AF.Sin, bias=halfpi[:, 0:1], scale=-1.0),
              reads=[tk("t2"), "halfpi"], writes=[tk("c1")])
        lbr = y; lbi = kk
        dv(lambda e: e.tensor_tensor(out=lbr[:], in0=r[:], in1=c1[:], op=ALU.mult), ["r", "c1"], ["y"])
        dv(lambda e: e.tensor_tensor(out=lbi[:], in0=r[:], in1=s1[:], op=ALU.mult), ["r", "s1"], ["kk"])
        dv(lambda e: e.tensor_scalar(out=lbr[:], in0=lbr[:], scalar1=-1.0, scalar2=None, op0=ALU.add), ["y"], ["y"])
        dv(lambda e: e.tensor_tensor(out=t1[:], in0=lamre[:], in1=lamre[:], op=ALU.mult), ["lamre"], ["t1"])
        dv(lambda e: e.tensor_tensor(out=t2[:], in0=lamim[:], in1=lamim[:], op=ALU.mult), ["lamim"], ["t2"])
        dv(lambda e: e.tensor_tensor(out=t1[:], in0=t1[:], in1=t2[:], op=ALU.add), ["t1", "t2"], ["t1"])
        dv(lambda e: e.reciprocal(out=t1[:], in_=t1[:]), ["t1"], ["t1"])
        dv(lambda e: e.tensor_tensor(out=cre[:], in0=lbr[:], in1=lamre[:], op=ALU.mult), ["y", "lamre"], ["cre"])
        dv(lambda e: e.tensor_tensor(out=t2[:], in0=lbi[:], in1=lamim[:], op=ALU.mult), ["kk", "lamim"], ["t2"])
        dv(lambda e: e.tensor_tensor(out=cre[:], in0=cre[:], in1=t2[:], op=ALU.add), ["cre", "t2"], ["cre"])
        dv(lambda e: e.tensor_tensor(out=cre[:], in0=cre[:], in1=t1[:], op=ALU.mult), ["cre", "t1"], ["cre"])
        dv(lambda e: e.tensor_tensor(out=cim[:], in0=lbi[:], in1=lamre[:], op=ALU.mult), ["kk", "lamre"], ["cim"])
        dv(lambda e: e.tensor_tensor(out=t2[:], in0=lbr[:], in1=lamim[:], op=ALU.mult), ["y", "lamim"], ["t2"])
        dv(lambda e: e.tensor_tensor(out=cim[:], in0=cim[:], in1=t2[:], op=ALU.subtract), ["cim", "t2"], ["cim"])
        dv(lambda e: e.tensor_tensor(out=cim[:], in0=cim[:], in1=t1[:], op=ALU.mult), ["cim", "t1"], ["cim"])
        return dict(r=r, c1=c1, s1=s1, cre=cre, cim=cim), tk

    colT, ctk = s5_scalars("c_", 16, lamre_c_d, lamim_c_d, logdt_c_d)
    S.dve(lambda e: e.tensor_copy(out=rcol[:], in_=colT["r"][:]), reads=[ctk("r")], writes=["rcol"])
    pr = A.alloc("pr", [128, 16], F32); pi_ = A.alloc("pi", [128, 16], F32)
    pt1 = A.alloc("pt1", [128, 16], F32); pt2 = A.alloc("pt2", [128, 16], F32)
    S.dve(lambda e: e.tensor_copy(out=pr[:], in_=colT["c1"][:]), reads=[ctk("c1")], writes=["pr"])
    S.dve(lambda e: e.tensor_copy(out=pi_[:], in_=colT["s1"][:]), reads=[ctk("s1")], writes=["pi"])
    S.pool(lambda e: e.memset(cosT[:, :, 0:1], 1.0), writes=["cosT"])
    S.pool(lambda e: e.memset(sinT[:, :, 0:1], 0.0), writes=["sinT"])
    tq = [A.alloc("tq%d" % i, [128, 16, TB // 2], F32) for i in range(4)]
    k = 1
    while k < TB:
        def bc(t, k=k):
            return t[:, :].unsqueeze(2).to_broadcast([128, 16, k])
        Ck = cosT[:, :, 0:k]; Sk = sinT[:, :, 0:k]
        S.dve(lambda e, k=k, Ck=Ck: e.tensor_tensor(out=tq[0][:, :, 0:k], in0=Ck, in1=bc(pr, k), op=ALU.mult),
              reads=["cosT", "pr"], writes=["tq0"])
        S.dve(lambda e, k=k, Sk=Sk: e.tensor_tensor(out=tq[1][:, :, 0:k], in0=Sk, in1=bc(pi_, k), op=ALU.mult),
              reads=["sinT", "pi"], writes=["tq1"])
        S.dve(lambda e, k=k, Ck=Ck: e.tensor_tensor(out=tq[2][:, :, 0:k], in0=Ck, in1=bc(pi_, k), op=ALU.mult),
              reads=["cosT", "pi"], writes=["tq2"])
        S.dve(lambda e, k=k, Sk=Sk: e.tensor_tensor(out=tq[3][:, :, 0:k], in0=Sk, in1=bc(pr, k), op=ALU.mult),
              reads=["sinT", "pr"], writes=["tq3"])
        S.dve(lambda e, k=k: e.tensor_tensor(out=cosT[:, :, k:2 * k], in0=tq[0][:, :, 0:k], in1=tq[1][:, :, 0:k],
                                             op=ALU.subtract), reads=["tq0", "tq1"], writes=["cosT"])
        S.dve(lambda e, k=k: e.tensor_tensor(out=sinT[:, :, k:2 * k], in0=tq[2][:, :, 0:k], in1=tq[3][:, :, 0:k],
                                             op=ALU.add), reads=["tq2", "tq3"], writes=["sinT"])
        S.dve(lambda e: e.tensor_tensor(out=pt1[:], in0=pr[:], in1=pr[:], op=ALU.mult), reads=["pr"], writes=["pt1"])
        S.dve(lambda e: e.tensor_tensor(out=pt2[:], in0=pi_[:], in1=pi_[:], op=ALU.mult), reads=["pi"], writes=["pt2"])
        S.dve(lambda e: e.tensor_tensor(out=pt2[:], in0=pt1[:], in1=pt2[:], op=ALU.subtract), reads=["pt1", "pt2"],
              writes=["pt2"])
        S.dve(lambda e: e.tensor_tensor(out=pt1[:], in0=pr[:], in1=pi_[:], op=ALU.mult), reads=["pr", "pi"], writes=["pt1"])
        S.dve(lambda e: e.tensor_copy(out=pr[:], in_=pt2[:]), reads=["pt2"], writes=["pr"])
        S.dve(lambda e: e.tensor_scalar(out=pi_[:], in0=pt1[:], scalar1=2.0, scalar2=None, op0=ALU.mult), reads=["pt1"],
              writes=["pi"])
        k *= 2
    S.dve(lambda e: e.tensor_copy(out=pblk_r[:], in_=pr[:]), reads=["pr"], writes=["pblk"])
    S.dve(lambda e: e.tensor_copy(out=pblk_i[:], in_=pi_[:]), reads=["pi"], writes=["pblk"])
    S.dve(lambda e: e.tensor_scalar(out=pblk_in[:], in0=pi_[:], scalar1=-1.0, scalar2=None, op0=ALU.mult), reads=["pi"],
          writes=["pblk"])
    S.barrier()
    A.release(mS)
    for dd_ in range(2):
        mR = A.mark()
        hs = slice(dd_ * 1024, (dd_ + 1) * 1024)
        rowT, rtk = s5_scalars("r%d_" % dd_, 1024, lamre_r_d[:, hs], lamim_r_d[:, hs], logdt_r_d[:, hs])
        braw_re = rowT["r"]; braw_im = rowT["c1"]
        S.dma("sp", lambda e, braw_re=braw_re, hs=hs: e.dma_start(out=braw_re[:], in_=braw_re_d[:, hs]),
              reads=[rtk("cre"), rtk("cim")], writes=[rtk("r")])
        S.dma("sp", lambda e, braw_im=braw_im, hs=hs: e.dma_start(out=braw_im[:], in_=braw_im_d[:, hs]),
              reads=[rtk("cre"), rtk("cim")], writes=[rtk("c1")])
        u1 = rowT["s1"]
        cre = rowT["cre"]; cim = rowT["cim"]
        bbT_re_f = bbT_re[:, dd_ * 8:(dd_ + 1) * 8, :].rearrange("p a b -> p (a b)")
        bbT_im_f = bbT_im[:, dd_ * 8:(dd_ + 1) * 8, :].rearrange("p a b -> p (a b)")
        tA = A.alloc("rowtA", [128, 1024], F32)
        tAk = "rowtA%d" % dd_
        S.dve(lambda e, u1=u1, cim=cim, braw_im=braw_im: e.tensor_tensor(out=u1[:], in0=cim[:], in1=braw_im[:], op=ALU.mult),
              reads=[rtk("cim"), rtk("c1")], writes=[rtk("s1")])
        S.dve(lambda e, tA=tA, cre=cre, braw_re=braw_re: e.tensor_tensor(out=tA[:], in0=cre[:], in1=braw_re[:], op=ALU.mult),
              reads=[rtk("cre"), rtk("r")], writes=[tAk])
        S.dve(lambda e, tA=tA, u1=u1, bbT_re_f=bbT_re_f: e.tensor_tensor(out=bbT_re_f, in0=tA[:], in1=u1[:], op=ALU.subtract),
              reads=[tAk, rtk("s1")], writes=["bbT_re"])
        S.dve(lambda e, u1=u1, cim=cim, braw_re=braw_re: e.tensor_tensor(out=u1[:], in0=cim[:], in1=braw_re[:], op=ALU.mult),
              reads=[rtk("cim"), rtk("r"), "bbT_re"], writes=[rtk("s1")])
        S.dve(lambda e, tA=tA, cre=cre, braw_im=braw_im: e.tensor_tensor(out=tA[:], in0=cre[:], in1=braw_im[:], op=ALU.mult),
              reads=[rtk("cre"), rtk("c1"), "bbT_re"], writes=[tAk])
        S.dve(lambda e, tA=tA, u1=u1, bbT_im_f=bbT_im_f: e.tensor_tensor(out=bbT_im_f, in0=tA[:], in1=u1[:], op=ALU.add),
              reads=[tAk, rtk("s1")], writes=["bbT_im"])
        S.barrier()
        A.release(mR)
    cst = A.alloc("cst", [128, 2048], F32)
    S.dma("sp", lambda e: e.dma_start(out=cst[:], in_=craw_re_d[:, :]), writes=["cst"])
    S.dve(lambda e: e.tensor_copy(out=cT_re[:].rearrange("p a b -> p (a b)"), in_=cst[:]), reads=["cst"], writes=["cT_re"])
    S.dma("sp", lambda e: e.dma_start(out=cst[:], in_=craw_im_d[:, :]), reads=["cT_re"], writes=["cst"])
    S.dve(lambda e: e.tensor_scalar(out=cT_imn[:].rearrange("p a b -> p (a b)"), in0=cst[:], scalar1=-1.0, scalar2=None,
                                    op0=ALU.mult), reads=["cst"], writes=["cT_imn"])
    S.barrier()
    A.release(mS)
    sin_bf = A.alloc("sin_bf", [128, 2, L], BF16)
    yacc = A.alloc("yacc", [128, 2, L], F32)

    epsb = A.alloc("epsb", [128, 1], F32)
    S.pool(lambda e: e.memset(epsb[:], EPS), writes=["epsb"])
    mW0 = A.mark()
    xt1_r = Rot(A, "xt1", 4, [128, D], F32)
    xn_r = Rot(A, "xn", 1, [128, 4, D], BF16)
    hT_r = Rot(A, "hTg", 1, [128, KD, GT], BF16)
    junk = A.alloc("junk", [128, D], BF16)
    ss_r = Rot(A, "ss", 2, [128, 4], F32)
    rs_r = Rot(A, "rs", 2, [128, 4], F32)
    mW = A.mark()

    def phase1(s):
        for gi in range(NG):
            xn, xnk = xn_r.next()
            hTg, hTk = hT_r.next()
            ss, ssk = ss_r.next()
            rs, rsk = rs_r.next()
            for i in range(4):
                xt1, xt1k = xt1_r.next()
                S.dma("sp", lambda e, xt1=xt1, gi=gi, i=i: e.dma_start(
                    out=xt1[:], in_=x_d[s, gi * GT + i * 128:gi * GT + (i + 1) * 128, :]), writes=[xt1k])
                S.act(lambda e, xt1=xt1, xn=xn, ss=ss, i=i: e.activation(out=xn[:, i, :], in_=xt1[:], func=AF.Square,
                                                                         accum_out=ss[:, i:i + 1]),
                      reads=[xt1k], writes=[ssk + str(i), xnk + str(i)])
                S.act(lambda e, ss=ss, rs=rs, i=i: e.activation(out=rs[:, i:i + 1], in_=ss[:, i:i + 1], func=AF.Sqrt, bias=epsb[:, 0:1],
                                                               scale=1.0 / D),
                      reads=[ssk + str(i), "epsb"], writes=[rsk + str(i)])
                S.dve(lambda e, rs=rs, i=i: e.reciprocal(out=rs[:, i:i + 1], in_=rs[:, i:i + 1]), reads=[rsk + str(i)], writes=[rsk + str(i)])
                S.dve(lambda e, xt1=xt1, xn=xn, rs=rs, i=i: e.tensor_scalar(out=xn[:, i, :], in0=xt1[:],
                                                                            scalar1=rs[:, i:i + 1], scalar2=None, op0=ALU.mult),
                      reads=[xt1k, rsk + str(i)], writes=[xnk + str(i)])
            if P1STOP < 2:
                continue
            for i in range(4):
                pb = (gi * 4 + i) % 2
                for k in range(KD):
                    S.pe(lambda e, xn=xn, i=i, k=k, pb=pb: e.transpose(out=bankbf(pb)[:, k * 128:(k + 1) * 128],
                                                                        in_=xn[:, i, k * 128:(k + 1) * 128], identity=ident_bf[:]),
                         reads=[xnk + str(i), "ident_bf"], writes=["bank%d" % pb])
                eng = "act" if i % 2 == 0 else "dve"
                if eng == "act":
                    S.act(lambda e, hTg=hTg, i=i, pb=pb: e.activation(
                        out=hTg[:, :, i * 128:(i + 1) * 128], in_=bankbf(pb).rearrange("p (k t) -> p k t", k=KD), func=AF.Copy),
                        reads=["bank%d" % pb], writes=[hTk + str(i)])
                else:
                    S.dve(lambda e, hTg=hTg, i=i, pb=pb: e.tensor_copy(
                        out=hTg[:, :, i * 128:(i + 1) * 128], in_=bankbf(pb).rearrange("p (k t) -> p k t", k=KD)),
                        reads=["bank%d" % pb], writes=[hTk + str(i)])
            hTall = [hTk + str(i) for i in range(4)]
            if P1STOP < 3:
                continue
            for kt in range(2):
                for k in range(KD):
                    S.pe(lambda e, hTg=hTg, kt=kt, k=k: e.matmul(banks[2 + kt][:], lhsT=wins5[:, k, kt * 128:(kt + 1) * 128],
                                                                   rhs=hTg[:, k, :], start=(k == 0), stop=(k == KD - 1)),
                         reads=hTall + ["wins5"], writes=["bank%d" % (2 + kt)])
                if 'noact' in VAR:
                    continue
                if 'onlydve' not in VAR:
                    S.act(lambda e, kt=kt, gi=gi: e.activation(out=sin_bf[:, kt, gi * GT:(gi + 1) * GT], in_=banks[2 + kt][:],
                                                               func=AF.Copy), reads=["bank%d" % (2 + kt)], writes=["sin_bf", "ser%d" % kt])
                if 'nodve' in VAR:
                    continue
                S.dve(lambda e, kt=kt, gi=gi: e.tensor_scalar(out=yacc[:, kt, gi * GT:(gi + 1) * GT], in0=banks[2 + kt][:],
                                                              scalar1=dskip[:, kt:kt + 1], scalar2=None, op0=ALU.mult),
                      reads=["bank%d" % (2 + kt), "dskip"] + (["ser%d" % kt] if 'ser' in VAR else []), writes=["yacc"])
            if P1STOP < 4:
                continue
            S.dma(HTQ, lambda e, hTg=hTg, gi=gi: e.dma_start(
                out=hT_d[s, :, :, gi * GT:(gi + 1) * GT].rearrange("k p t -> p k t"), in_=hTg[:]),
                reads=hTall, writes=["hT_d%d_%d" % (s, gi)])

    A.release(mW0)
    NR = 3
    bu_r = Rot(A, "bu", NR, [128, 2, TB], F32)
    mm_r = Rot(A, "mm", 2, [128, 2, TB], F32)
    xt_r = Rot(A, "xt", NR, [128, 2, TB], F32)
    xb_r = Rot(A, "xb", 2, [128, 4, TB], BF16)
    init = [A.alloc("init%d" % i, [128, 16, 2], F32) for i in range(2)]
    ctmp = A.alloc("ctmp", [128, 16, 2], F32)
    sig_t = [mm_r.t[0][:, 0, 0:GT], mm_r.t[0][:, 1, 0:GT]]
    yg = sin_bf

    def s5(s):
        ulist = []
        for bi in range(NB):
            for d in range(2):
                for j in range(8):
                    ulist.append((bi, d, j, len(ulist)))
        ub = {}

        def upar(u):
            bi, d, j, uc = u
            b = bi if d == 0 else NB - 1 - bi
            return bi, d, j, uc, slice(b * TB, (b + 1) * TB), 6 + d, d * 8 + j, j // 4, (uc % NR) * 2

        def stage1(u):
            bi, d, j, uc, tsl, ybank, dj, kt, bk = upar(u)
            bu, buk = bu_r.next()
            S.pe(lambda e, dj=dj, kt=kt, tsl=tsl, bk=bk: e.matmul(banks[bk][:, 0:TB], lhsT=bbT_re[:, dj, :],
                                                                  rhs=sin_bf[:, kt, tsl], start=True, stop=True),
                 reads=["sin_bf", "bbT_re"], writes=["bank%d" % bk])
            S.pe(lambda e, dj=dj, kt=kt, tsl=tsl, bk=bk: e.matmul(banks[bk + 1][:, 0:TB], lhsT=bbT_im[:, dj, :],
                                                                  rhs=sin_bf[:, kt, tsl], start=True, stop=True),
                 reads=["sin_bf", "bbT_im"], writes=["bank%d" % (bk + 1)])

            def rv(ap, d=d):
                return ap if d == 0 else ap[:, ::-1]
            S.act(lambda e, bu=bu, bk=bk, rv=rv: e.activation(out=bu[:, 0, :], in_=rv(banks[bk][:, 0:TB]), func=AF.Copy),
                  reads=["bank%d" % bk], writes=[buk + "r"])
            S.act(lambda e, bu=bu, bk=bk, rv=rv: e.activation(out=bu[:, 1, :], in_=rv(banks[bk + 1][:, 0:TB]), func=AF.Copy),
                  reads=["bank%d" % (bk + 1)], writes=[buk + "i"])
            ub[uc] = (bu, buk, rv)

        def stage2(u):
            bi, d, j, uc, tsl, ybank, dj, kt, bk = upar(u)
            bu, buk, rv = ub.pop(uc)
            mm, mmk = mm_r.next(); xt, xtk = xt_r.next(); xb, xbk = xb_r.next()
            Cc = cosT[:, dj, :]; Sn = sinT[:, dj, :]
            mods = [
                (lambda e: e.tensor_tensor(out=mm[:, 0, :], in0=bu[:, 0, :], in1=Cc, op=ALU.mult), [buk + "r"], [mmk + "0"]),
                (lambda e: e.tensor_tensor(out=mm[:, 1, :], in0=bu[:, 1, :], in1=Sn, op=ALU.mult), [buk + "i"], [mmk + "1"]),
                (lambda e: e.tensor_tensor(out=mm[:, 0, :], in0=mm[:, 0, :], in1=mm[:, 1, :], op=ALU.add), [mmk + "0", mmk + "1"], [mmk + "0"]),
                (lambda e: e.tensor_tensor(out=mm[:, 1, :], in0=bu[:, 1, :], in1=Cc, op=ALU.mult), [buk + "i", mmk + "1"], [mmk + "1"]),
                (lambda e: e.tensor_tensor(out=bu[:, 0, :], in0=bu[:, 0, :], in1=Sn, op=ALU.mult), [buk + "r"], [buk + "r"]),
                (lambda e: e.tensor_tensor(out=mm[:, 1, :], in0=mm[:, 1, :], in1=bu[:, 0, :], op=ALU.subtract), [mmk + "1", buk + "r"], [mmk + "1"]),
            ]
            for oi, (fn_, rd_, wr_) in enumerate(mods):
                S.any("pool" if oi < S5POOL else "dve", fn_, reads=rd_, writes=wr_)
            rb = rcol[:, dj:dj + 1].to_broadcast([128, TB])
            ini = init[bi % 2]
            inik = "init%d_%d" % (bi % 2, dj)
            if bi == 0:
                i_re = 0.0; i_im = 0.0; ird = []
            else:
                i_re = ini[:, dj, 0:1]; i_im = ini[:, dj, 1:2]; ird = [inik]
            S.dve(lambda e, xt=xt, mm=mm, rb=rb, i_re=i_re: e.tensor_tensor_scan(
                out=xt[:, 0, :], data0=rb, data1=mm[:, 0, :], initial=i_re, op0=ALU.mult, op1=ALU.add),
                reads=[mmk + "0"] + ird, writes=[xtk + "r"])
            S.dve(lambda e, xt=xt, mm=mm, rb=rb, i_im=i_im: e.tensor_tensor_scan(
                out=xt[:, 1, :], data0=rb, data1=mm[:, 1, :], initial=i_im, op0=ALU.mult, op1=ALU.add),
                reads=[mmk + "1"] + ird, writes=[xtk + "i"])
            if bi < NB - 1:
                nin = init[(bi + 1) % 2]
                nk = "init%d_%d" % ((bi + 1) % 2, dj)
                ck = "ctmp%d" % dj
                S.dve(lambda e, xt=xt, dj=dj: e.tensor_scalar(out=ctmp[:, dj, 0:1], in0=xt[:, 0, TB - 1:TB],
                                                              scalar1=pblk_r[:, dj:dj + 1], scalar2=None, op0=ALU.mult),
                      reads=[xtk + "r"], writes=[ck + "a"])
                S.dve(lambda e, xt=xt, dj=dj: e.tensor_scalar(out=ctmp[:, dj, 1:2], in0=xt[:, 1, TB - 1:TB],
                                                              scalar1=pblk_r[:, dj:dj + 1], scalar2=None, op0=ALU.mult),
                      reads=[xtk + "i"], writes=[ck + "b"])
                S.dve(lambda e, xt=xt, dj=dj, nin=nin: e.scalar_tensor_tensor(
                    out=nin[:, dj, 0:1], in0=xt[:, 1, TB - 1:TB], scalar=pblk_in[:, dj:dj + 1], in1=ctmp[:, dj, 0:1],
                    op0=ALU.mult, op1=ALU.add), reads=[xtk + "i", ck + "a"], writes=[nk])
                S.dve(lambda e, xt=xt, dj=dj, nin=nin: e.scalar_tensor_tensor(
                    out=nin[:, dj, 1:2], in0=xt[:, 0, TB - 1:TB], scalar=pblk_i[:, dj:dj + 1], in1=ctmp[:, dj, 1:2],
                    op0=ALU.mult, op1=ALU.add), reads=[xtk + "r", ck + "b"], writes=[nk])
            S.dve(lambda e: e.tensor_tensor(out=rv(xb[:, 0, :]), in0=xt[:, 0, :], in1=Cc, op=ALU.mult), reads=[xtk + "r"], writes=[xbk + "0"])
            S.dve(lambda e: e.scalar_tensor_tensor(out=rv(xb[:, 1, :]), in0=xt[:, 1, :], scalar=-1.0, in1=Sn, op0=ALU.mult, op1=ALU.mult),
                  reads=[xtk + "i"], writes=[xbk + "1"])
            S.dve(lambda e: e.tensor_tensor(out=rv(xb[:, 2, :]), in0=xt[:, 0, :], in1=Sn, op=ALU.mult), reads=[xtk + "r"], writes=[xbk + "2"])
            S.dve(lambda e: e.tensor_tensor(out=rv(xb[:, 3, :]), in0=xt[:, 1, :], in1=Cc, op=ALU.mult), reads=[xtk + "i"], writes=[xbk + "3"])
            for pi_x, lt in enumerate((cT_re, cT_re, cT_imn, cT_imn)):
                S.pe(lambda e, pi_x=pi_x, lt=lt: e.matmul(banks[ybank][:, 0:TB], lhsT=lt[:, dj, :], rhs=xb[:, pi_x, :],
                                                          start=(j % 4 == 0 and pi_x == 0), stop=(j % 4 == 3 and pi_x == 3)),
                     reads=[xbk + str(pi_x), "cT_re", "cT_imn"], writes=["bank%d" % ybank])
            if j % 4 == 3:
                S.dve(lambda e, kt=kt, tsl=tsl, ybank=ybank: e.tensor_tensor(out=yacc[:, kt, tsl], in0=yacc[:, kt, tsl],
                                                                             in1=banks[ybank][:, 0:TB], op=ALU.add),
                      reads=["bank%d" % ybank, "yacc"], writes=["yacc"])

        for u in ulist[:2]:
            stage1(u)
        for i_, u in enumerate(ulist):
            if i_ + 2 < len(ulist):
                stage1(ulist[i_ + 2])
            stage2(u)
        S.barrier()
        sgi = 0
        for gi in range(NG):
            gsl = slice(gi * GT, (gi + 1) * GT)
            for kt in range(2):
                S.act(lambda e, kt=kt, gsl=gsl: e.activation(out=yg[:, kt, gsl], in_=yacc[:, kt, gsl], func=AF.Gelu),
                      reads=["yacc"], writes=["yg%d" % gi, "sin_bf"])
            for m in range(2):
                pb = m
                for kt in range(2):
                    S.pe(lambda e, m=m, kt=kt, gsl=gsl, pb=pb: e.matmul(banks[pb][:], lhsT=wglu[:, kt, m * 128:(m + 1) * 128],
                                                                        rhs=yg[:, kt, gsl], start=(kt == 0), stop=(kt == 1)),
                         reads=["yg%d" % gi, "wglu"], writes=["bank%d" % pb])
                sg = sig_t[sgi % 2]; sgk = "sigt%d" % (sgi % 2)
                sgi += 1
                S.act(lambda e, sg=sg, m=m, pb=pb: e.activation(out=sg, in_=banks[pb][:], func=AF.Sigmoid, bias=bglu[:, m:m + 1]),
                      reads=["bank%d" % pb, "bglu"], writes=[sgk])
                S.dve(lambda e, sg=sg, m=m, gsl=gsl: e.tensor_tensor(out=brbT[s][:, m, gsl], in0=yg[:, m, gsl], in1=sg, op=ALU.mult),
                      reads=[sgk, "yg%d" % gi], writes=["brbT%d" % s])

    for s in range(NS):
        if upto >= 1:
            phase1(s)
        S.barrier()
        if upto >= 2:
            s5(s)
        S.barrier()
    if dbg:
        d_sin = dout("dbg_yacc", [NS, 2, 128, L])
        d_brb = dout("dbg_brb", [NS, 2, 128, L], BF16)
        S.dma("sp", lambda e: e.dma_start(out=d_sin[NS - 1].rearrange("k p t -> p k t"), in_=yacc[:]), reads=["yacc"], writes=["dbg1"])
        for s in range(NS):
            S.dma("sp", lambda e, s=s: e.dma_start(out=d_brb[s].rearrange("k p t -> p k t"), in_=brbT[s][:]),
                  reads=["brbT%d" % s], writes=["dbg2%d" % s])
        d_tab = dout("dbg_cos", [128, 16, TB])
        S.dma("sp", lambda e: e.dma_start(out=d_tab[:, :, :], in_=cosT[:]), reads=["cosT"], writes=["dbg3"])
        d_bb = dout("dbg_bb", [128, 16, 128], BF16)
        S.dma("sp", lambda e: e.dma_start(out=d_bb[:, :, :], in_=bbT_re[:]), reads=["bbT_re"], writes=["dbg4"])
    S.barrier()
    A.release(mA)
    if upto <= 2:
        zt = A.alloc("zt", [128, D], F32)
        S.pool(lambda e: e.memset(zt[:], 0.0), writes=["zt"])
        for t in range(NTOK // 128):
            S.dma("sp", lambda e, t=t: e.dma_start(out=out_d[t * 128:(t + 1) * 128, :], in_=zt[:]), reads=["zt"], writes=["o%d" % t])
        S.emit()
        return nc, S, A

    HX = D + 32
    h2x_d = dscr("h2x_scr", [NTOK, D], BF16)
    pr_d = dscr("pr_scr", [NTOK, NE], F32)
    scoresT = A.alloc("scoresT", [64, L], F32)
    mB = A.mark()
    win_bf = A.alloc("win_bf", [128, KD, PT], BF16)
    wupa = A.alloc("wupa", [128, 4, D], BF16)
    wupb = A.alloc("wupb", [128, 2, D], BF16)
    wout = A.alloc("wout", [128, KD, D], BF16)
    wsT = A.alloc("wsT", [128, 8, 128], BF16)
    wr = A.alloc("wr", [128, KD, NE], BF16)
    lng = A.alloc("lng", [128, GW], F32)
    lnb = A.alloc("lnb", [128, GW], F32)
    bstab = A.alloc("bstab", [128, GW], F32)
    bgate = A.alloc("bgate", [128, 16], F32)
    g2 = A.alloc("g2", [128, KD], F32)
    g1b = A.alloc("g1b", [128, KD], F32)
    epsb2 = A.alloc("epsb2", [128, 1], F32)
    S.pool(lambda e: e.memset(epsb2[:], EPS), writes=["epsb2"])
    S.pool(lambda e: e.memset(scoresT[:], 0.0), writes=["scoresT"])
    for (t_, d_, nm) in ((lng, lng_d, "lng"), (lnb, lnb_d, "lnb"), (bstab, bstab_d, "bstab"), (bgate, bgate_d, "bgate"),
                         (g2, g2_d, "g2"), (g1b, g1_d, "g1b")):
        S.dma("sp", lambda e, t_=t_, d_=d_: e.dma_start(out=t_[:], in_=d_[:, :]), writes=[nm])
    mBs = A.mark()
    stgB = Rot(A, "stgB", 2, [128, PT], F32)
    for k in range(KD):
        st_, stk = stgB.next()
        S.dma("sp", lambda e, st_=st_, k=k: e.dma_start(out=st_[:], in_=win_d[k * 128:(k + 1) * 128, :]), writes=[stk])
        S.dve(lambda e, st_=st_, k=k: e.tensor_scalar(out=win_bf[:, k, :], in0=st_[:], scalar1=g1b[:, k:k + 1], scalar2=None,
                                                      op0=ALU.mult), reads=[stk, "g1b"], writes=["win_bf"])
    def load_cast(dst2d, src2d, ncol, nm, eng):
        st_, stk = stgB.next()
        S.dma("sp", lambda e: e.dma_start(out=st_[:, 0:ncol], in_=src2d), writes=[stk])
        if eng == "act":
            S.act(lambda e: e.activation(out=dst2d, in_=st_[:, 0:ncol], func=AF.Copy), reads=[stk], writes=[nm])
        else:
            S.dve(lambda e: e.tensor_copy(out=dst2d, in_=st_[:, 0:ncol]), reads=[stk], writes=[nm])
    for k in range(4):
        load_cast(wupa[:, k, :], wupa_d[k * 128:(k + 1) * 128, :], D, "wupa", "act")
    for k in range(2):
        load_cast(wupb[:, k, :], wupb_d[k * 128:(k + 1) * 128, :], D, "wupb", "dve")
    for k in range(KD):
        load_cast(wout[:, k, :], wout_d[k * 128:(k + 1) * 128, :], D, "wout", "act" if k % 2 else "dve")
    load_cast(wsT[:].rearrange("p a b -> p (a b)"), wsT_d[:, :, :].rearrange("p a b -> p (a b)"), 1024, "wsT", "dve")
    for k in range(KD):
        load_cast(wr[:, k, :], wr_d[k * 128:(k + 1) * 128, :], NE, "wr", "dve")
    S.barrier()
    A.release(mBs)

    hTg_r = Rot(A, "hTgB", HTGB, [128, KD, GT], BF16)
    xt_r2 = Rot(A, "xtB", 1, [128, D], F32)
    u_r = Rot(A, "u_sb", 3, [128, GW], F32)
    v_r = Rot(A, "v_sb", 3, [128, GW], F32)
    vn_r = Rot(A, "vn", 3, [128, GW], BF16)
    bra_r = Rot(A, "bra", 2, [128, GW], BF16)
    braT_r = Rot(A, "braT", 1, [128, 4, GT], BF16)
    sg_r = Rot(A, "sgB", 4, [128, GT], F32)
    mrg_r = Rot(A, "mrg", 1, [128, KD, GT], BF16)
    x1_r = Rot(A, "x1", 2, [128, D], F32)
    hn_r = Rot(A, "hn2", 3, [128, D], BF16)
    pr_r = Rot(A, "prB", 4, [128, NE], F32)
    h2T_r = Rot(A, "h2T", 1, [128, KD, 128], BF16)
    st_r = Rot(A, "stB", 8, [128, 8], F32)
    ex_r = Rot(A, "exB", 2, [128, NE], F32)
    pw_r = [[A.alloc("pw%d_%d" % (s, i), [128, 32 * NS], F32) for i in range(2)] for s in range(NS)]
    for s in range(NS):
        for i in range(2):
            S.pool(lambda e, s=s, i=i: e.memset(pw_r[s][i][:], 0.0), writes=["pw%d_%d" % (s, i)])

    hT_next = {}

    def phaseB(s):
        for gi in range(NG):
            phaseB_group(s, gi)

    def phaseB_group(s, gi):
        if True:
            def load_hT(s_, gi_):
                t_, k_ = hTg_r.next()
                S.dma("sp", lambda e: e.dma_start(
                    out=t_[:], in_=hT_d[s_, :, :, gi_ * GT:(gi_ + 1) * GT].rearrange("k p t -> p k t")),
                    reads=["hT_d%d_%d" % (s_, gi_)], writes=[k_])
                return t_, k_
            if (s, gi) in hT_next:
                hTg, hTk = hT_next.pop((s, gi))
            else:
                hTg, hTk = load_hT(s, gi)
            braT, braTk = braT_r.next()
            mrg, mrgk = mrg_r.next()
            tb = {}

            def gm1(i):
                tsl = slice(i * 128, (i + 1) * 128)
                u_sb, uk = u_r.next(); v_sb, vk = v_r.next(); vn, vnk = vn_r.next(); stt, stk = st_r.next()
                vh, vhk = v_sb, vk
                for (bk_, c0) in ((0, 0), (1, GW)):
                    for k in range(KD):
                        S.pe(lambda e, k=k, bk_=bk_, c0=c0: e.matmul(
                            banks[bk_][:], lhsT=hTg[:, k, tsl], rhs=win_bf[:, k, c0:c0 + GW], start=(k == 0), stop=(k == KD - 1)),
                            reads=[hTk, "win_bf"], writes=["bank%d" % bk_])
                S.act(lambda e: e.activation(out=u_sb[:], in_=banks[0][:], func=AF.Gelu), reads=["bank0"], writes=[uk])
                S.act(lambda e: e.activation(out=v_sb[:], in_=banks[1][:], func=AF.Gelu, accum_out=stt[:, 0:1]),
                      reads=["bank1"], writes=[vk, stk + "a"])
                S.dve(lambda e: e.tensor_scalar(out=stt[:, 1:2], in0=stt[:, 0:1], scalar1=-1.0 / GW, scalar2=None, op0=ALU.mult),
                      reads=[stk + "a"], writes=[stk + "b"])
                S.act(lambda e: e.activation(out=vn[:], in_=v_sb[:], func=AF.Square, bias=stt[:, 1:2],
                                             accum_out=stt[:, 2:3]), reads=[vk, stk + "b"], writes=[stk + "c", vnk])
                S.act(lambda e: e.activation(out=stt[:, 3:4], in_=stt[:, 2:3], func=AF.Sqrt, bias=epsb2[:, 0:1], scale=1.0 / GW),
                      reads=[stk + "c", "epsb2"], writes=[stk + "d"])
                S.dve(lambda e: e.reciprocal(out=stt[:, 4:5], in_=stt[:, 3:4]), reads=[stk + "d"], writes=[stk + "e"])
                S.dve(lambda e: e.tensor_scalar(out=vh[:], in0=v_sb[:], scalar1=stt[:, 1:2], scalar2=stt[:, 4:5],
                                                op0=ALU.add, op1=ALU.mult),
                      reads=[vk, stk + "b", stk + "e"], writes=[vhk])
                S.pool(lambda e: e.tensor_tensor(out=vh[:], in0=vh[:], in1=lng[:], op=ALU.mult), reads=[vhk, "lng"], writes=[vhk])
                S.dve(lambda e: e.tensor_tensor(out=vn[:], in0=vh[:], in1=lnb[:], op=ALU.add), reads=[vhk, "lnb"], writes=[vnk])
                tb[("g", i)] = (tsl, u_sb, uk, v_sb, vk, vn, vnk)

            def gm2(i):
                tsl, u_sb, uk, v_sb, vk, vn, vnk = tb.pop(("g", i))
                zb, zbk = v_sb, vk
                bra, brak = bra_r.next()
                for g in range(8):
                    S.pe(lambda e, g=g: e.matmul(banks[2][:, g * 64:(g + 1) * 64], lhsT=wsT[:, g, :], rhs=vn[:, g * 64:(g + 1) * 64],
                                                 start=True, stop=True), reads=[vnk, "wsT"], writes=["bank2"])
                S.dve(lambda e: e.tensor_tensor(out=zb[:], in0=banks[2][:], in1=bstab[:], op=ALU.add), reads=["bank2", "bstab"],
                      writes=[zbk])
                S.dve(lambda e: e.tensor_tensor(out=bra[:], in0=zb[:], in1=u_sb[:], op=ALU.mult),
                      reads=[zbk, uk], writes=[brak])
                tb[("t", i)] = (tsl, bra, brak)

            def gm3(i):
                tsl, bra, brak = tb.pop(("t", i))
                for k in range(4):
                    S.pe(lambda e, k=k: e.transpose(out=bankbf(3)[:, k * 128:(k + 1) * 128], in_=bra[:, k * 128:(k + 1) * 128],
                                                    identity=ident_bf[:]), reads=[brak, "ident_bf"], writes=["bank3"])
                S.act(lambda e: e.activation(out=braT[:, :, tsl], in_=bankbf(3)[:, 0:512].rearrange("p (k t) -> p k t", k=4),
                                             func=AF.Copy), reads=["bank3"], writes=[braTk + str(i)])

            gm1(0); gm1(1); gm1(2); gm2(0); gm1(3); gm3(0); gm2(1); gm3(1); gm2(2); gm3(2); gm2(3); gm3(3)
            braTall = [braTk + str(i) for i in range(4)]
            gsl = slice(gi * GT, (gi + 1) * GT)
            for m in range(KD):
                fs = slice(m * 128, (m + 1) * 128)
                bA, bGa, bB, bGb = (4, 5, 6, 7) if m % 2 == 0 else (0, 1, 2, 3)
                for k in range(4):
                    S.pe(lambda e, k=k, fs=fs, bA=bA: e.matmul(banks[bA][:], lhsT=wupa[:, k, fs], rhs=braT[:, k, :], start=(k == 0), stop=(k == 3)),
                         reads=braTall + ["wupa"], writes=["bank%d" % bA])
                for k in range(KD):
                    S.pe(lambda e, k=k, m=m, bGa=bGa: e.matmul(banks[bGa][:], lhsT=win_bf[:, k, 1280 + m * 128:1280 + (m + 1) * 128], rhs=hTg[:, k, :],
                                                               start=(k == 0), stop=(k == KD - 1)), reads=[hTk, "win_bf"], writes=["bank%d" % bGa])
                for k in range(2):
                    S.pe(lambda e, k=k, fs=fs, bB=bB: e.matmul(banks[bB][:], lhsT=wupb[:, k, fs], rhs=brbT[s][:, k, gsl], start=(k == 0), stop=(k == 1)),
                         reads=["wupb"], writes=["bank%d" % bB])
                for k in range(KD):
                    S.pe(lambda e, k=k, m=m, bGb=bGb: e.matmul(banks[bGb][:], lhsT=win_bf[:, k, 2304 + m * 128:2304 + (m + 1) * 128], rhs=hTg[:, k, :],
                                                               start=(k == 0), stop=(k == KD - 1)), reads=[hTk, "win_bf"], writes=["bank%d" % bGb])
                sga, sgak = sg_r.next(); sgb, sgbk = sg_r.next()
                S.act(lambda e, sga=sga, m=m, bGa=bGa: e.activation(out=sga[:], in_=banks[bGa][:], func=AF.Sigmoid, bias=bgate[:, m:m + 1]),
                      reads=["bank%d" % bGa, "bgate"], writes=[sgak])
                S.act(lambda e, sgb=sgb, m=m, bGb=bGb: e.activation(out=sgb[:], in_=banks[bGb][:], func=AF.Sigmoid, bias=bgate[:, 8 + m:9 + m]),
                      reads=["bank%d" % bGb, "bgate"], writes=[sgbk])
                S.dve(lambda e, sga=sga, bA=bA: e.tensor_tensor(out=sga[:], in0=sga[:], in1=banks[bA][:], op=ALU.mult), reads=[sgak, "bank%d" % bA], writes=[sgak])
                S.dve(lambda e, sgb=sgb, bB=bB: e.tensor_tensor(out=sgb[:], in0=sgb[:], in1=banks[bB][:], op=ALU.mult), reads=[sgbk, "bank%d" % bB], writes=[sgbk])
                S.pool(lambda e, sga=sga, sgb=sgb, m=m: e.tensor_tensor(out=mrg[:, m, :], in0=sga[:], in1=sgb[:], op=ALU.add),
                       reads=[sgak, sgbk], writes=[mrgk + str(m)])
            mrgall = [mrgk + str(m) for m in range(KD)]
            nxt = (s, gi + 1) if gi + 1 < NG else ((s + 1, 0) if s + 1 < NS else None)
            if nxt is not None:
                hT_next[nxt] = load_hT(*nxt)

            def wa(i):
                tsl = slice(i * 128, (i + 1) * 128)
                tok0 = s * L + gi * GT + i * 128
                xt, xtk = xt_r2.next(); x1, x1k = x1_r.next(); hn, hnk = hn_r.next(); stt, stk = st_r.next()
                S.dma("sp", lambda e: e.dma_start(out=xt[:], in_=x_d[s, gi * GT + i * 128:gi * GT + (i + 1) * 128, :]), writes=[xtk])
                for hh in range(2):
                    for m in range(KD):
                        S.pe(lambda e, m=m, hh=hh: e.matmul(banks[hh][:], lhsT=mrg[:, m, tsl], rhs=wout[:, m, hh * 512:(hh + 1) * 512],
                                                            start=(m == 0), stop=(m == KD - 1)),
                             reads=mrgall + ["wout"], writes=["bank%d" % hh])
                    S.dve(lambda e, hh=hh: e.tensor_tensor(out=x1[:, hh * 512:(hh + 1) * 512], in0=xt[:, hh * 512:(hh + 1) * 512],
                                                           in1=banks[hh][:], op=ALU.add),
                          reads=[xtk, "bank%d" % hh], writes=[x1k + str(hh)])
                x1all = [x1k + "0", x1k + "1"]
                S.dma(STQ, lambda e: e.dma_start(out=acc_d[tok0:tok0 + 128, :], in_=x1[:]), reads=x1all,
                      writes=["acc_d%d" % (tok0 // 128)])
                S.act(lambda e: e.activation(out=hn[:, 0:D], in_=x1[:], func=AF.Square, accum_out=stt[:, 0:1]),
                      reads=x1all, writes=[stk + "a", hnk + "h"])
                S.act(lambda e: e.activation(out=stt[:, 1:2], in_=stt[:, 0:1], func=AF.Sqrt, bias=epsb2[:, 0:1], scale=1.0 / D),
                      reads=[stk + "a", "epsb2"], writes=[stk + "b"])
                S.dve(lambda e: e.reciprocal(out=stt[:, 2:3], in_=stt[:, 1:2]), reads=[stk + "b"], writes=[stk + "c"])
                S.dve(lambda e: e.tensor_scalar(out=hn[:, 0:D], in0=x1[:], scalar1=stt[:, 2:3], scalar2=None, op0=ALU.mult),
                      reads=x1all + [stk + "c"], writes=[hnk + "h"])
                S.dma(STQ, lambda e: e.dma_start(out=h2x_d[tok0:tok0 + 128, :], in_=hn[:, 0:D]), reads=[hnk + "h"],
                      writes=["h2x_dh%d" % (tok0 // 128)])
                tb[("w", i)] = (tok0, hn, hnk, stt, stk)

            def wb(i):
                tok0, hn, hnk, stt, stk = tb[("w", i)]
                h2T, h2Tk = h2T_r.next(); ex, exk = ex_r.next()
                for k in range(KD):
                    S.pe(lambda e, k=k: e.transpose(out=bankbf(2)[:, k * 128:(k + 1) * 128], in_=hn[:, k * 128:(k + 1) * 128],
                                                    identity=ident_bf[:]), reads=[hnk + "h", "ident_bf"], writes=["bank2"])
                S.dve(lambda e: e.tensor_tensor(out=h2T[:], in0=bankbf(2).rearrange("p (k t) -> p k t", k=KD),
                                                in1=g2[:, :].unsqueeze(2).to_broadcast([128, KD, 128]), op=ALU.mult),
                      reads=["bank2", "g2"], writes=[h2Tk])
                tb[("l", i)] = (h2T, h2Tk, ex, exk)

            def wl(i):
                tok0, hn, hnk, stt, stk = tb[("w", i)]
                h2T, h2Tk, ex, exk = tb.pop(("l", i))
                for k in range(KD):
                    S.pe(lambda e, k=k: e.matmul(banks[3][:, 0:NE], lhsT=h2T[:, k, :], rhs=wr[:, k, :], start=(k == 0), stop=(k == KD - 1)),
                         reads=[h2Tk, "wr"], writes=["bank3"])
                S.dve(lambda e: e.tensor_reduce(out=stt[:, 3:4], in_=banks[3][:, 0:NE], axis=AX.X, op=ALU.max), reads=["bank3"],
                      writes=[stk + "d"])
                S.dve(lambda e: e.tensor_scalar(out=stt[:, 4:5], in0=stt[:, 3:4], scalar1=-1.0, scalar2=None, op0=ALU.mult),
                      reads=[stk + "d"], writes=[stk + "e"])
                S.act(lambda e: e.activation(out=ex[:], in_=banks[3][:, 0:NE], func=AF.Exp, bias=stt[:, 4:5], accum_out=stt[:, 5:6]),
                      reads=["bank3", stk + "e"], writes=[exk, stk + "f"])
                S.dve(lambda e: e.reciprocal(out=stt[:, 6:7], in_=stt[:, 5:6]), reads=[stk + "f"], writes=[stk + "g"])
                prt, prk = pr_r.next()
                pview = prt[:, :]
                S.dve(lambda e: e.tensor_scalar(out=pview, in0=ex[:], scalar1=stt[:, 6:7], scalar2=None, op0=ALU.mult),
                      reads=[exk, stk + "g"], writes=[prk])
                S.dma(STQ, lambda e: e.dma_start(out=pr_d[tok0:tok0 + 128, :], in_=prt[:]), reads=[prk],
                      writes=["pr_d%d" % (tok0 // 128)])
                pwt = pw_r[s][i % 2]; pwk = "pw%d_%d" % (s, i % 2)
                S.dve(lambda e: e.tensor_copy(out=pwt[:, 32 * s:32 * s + NE], in_=pview), reads=[prk], writes=[pwk])
                tb[("p", i)] = (pwt, pwk)

            def wc(i):
                nt = gi * 4 + i
                pwt, pwk = tb.pop(("p", i))
                S.pe(lambda e: e.transpose(out=banks[3][0:32 * NS, 128:256], in_=pwt[:], identity=ident_f[:]), reads=[pwk, "ident_f"],
                     writes=["bank3"])
                S.act(lambda e: e.activation(out=scoresT[32 * s:32 * s + NE, nt * 128:(nt + 1) * 128],
                                             in_=banks[3][32 * s:32 * s + NE, 128:256], func=AF.Copy), reads=["bank3"], writes=["scoresT"])

            wa(0); wa(1); wa(2); wb(0); wa(3); wl(0); wb(1); wc(0); wl(1); wb(2); wc(1); wl(2); wb(3); wc(2); wl(3); wc(3)
            if dbg and s == 0 and gi == 0:
                d_bra = dout("dbg_braT", [4, 128, GT], BF16)
                d_mrg = dout("dbg_mrg", [KD, 128, GT], BF16)
                S.dma("sp", lambda e, braT=braT: e.dma_start(out=d_bra.rearrange("k p t -> p k t"), in_=braT[:]), reads=braTall, writes=["dbgb1"])
                S.dma("sp", lambda e, mrg=mrg: e.dma_start(out=d_mrg.rearrange("k p t -> p k t"), in_=mrg[:]), reads=mrgall, writes=["dbgb2"])

    for s in range(NS):
        phaseB(s)
    S.barrier()
    A.release(mB)
    if dbg:
        d_sc = dout("dbg_scores", [64, L])
        S.dma("sp", lambda e: e.dma_start(out=d_sc[:, :], in_=scoresT[:]), reads=["scoresT"], writes=["dbg5"])
        S.barrier()
    if upto <= 3:
        xo_r = Rot(A, "xo", 2, [128, D], F32)
        for t in range(NTOK // 128):
            xo, xok = xo_r.next()
            S.dma("sp", lambda e, t=t, xo=xo: e.dma_start(out=xo[:], in_=acc_d[t * 128:(t + 1) * 128, :]), writes=[xok])
            S.dma("sp", lambda e, t=t, xo=xo: e.dma_start(out=out_d[t * 128:(t + 1) * 128, :], in_=xo[:]), reads=[xok], writes=["o%d" % t])
        S.emit()
        return nc, S, A

    NR_ = 64
    mC = A.mark()
    lo = A.alloc("lo", [NR_, 1], F32); hi = A.alloc("hi", [NR_, 1], F32); mid = A.alloc("mid", [NR_, 1], F32)
    cnt = A.alloc("cnt", [NR_, 1], F32); flg = A.alloc("flg", [NR_, 1], F32); dlt = A.alloc("dlt", [NR_, 1], F32)
    onec = A.alloc("onec", [NR_, 1], F32)
    junkC = A.alloc("junkC", [NR_, L], F32)
    csum = A.alloc("csum", [NR_, L], F32)
    csT = A.alloc("csT", [128, NT, NR_], F32)
    iota_c = A.alloc("iota_c", [128, CAP], I16)
    ones_bf = A.alloc("ones_bf", [128, 1], BF16)
    idx_f = A.alloc("idx_f", [128, NR_ * NQ], F32)
    le_r = Rot(A, "LE", 4, [128, CAP], BF16)
    S.pool(lambda e: e.memset(lo[:], 0.0), writes=["lo"])
    S.pool(lambda e: e.memset(hi[:], 1.0), writes=["hi"])
    S.pool(lambda e: e.memset(onec[:], 1.0), writes=["onec"])
    S.pool(lambda e: e.memset(ones_bf[:], 1.0), writes=["ones_bf"])
    S.pool(lambda e: e.iota(iota_c[:], pattern=[[1, CAP]], base=0, channel_multiplier=0, allow_small_or_imprecise_dtypes=True),
           writes=["iota_c"])
    for it in range(30):
        S.dve(lambda e: e.tensor_tensor(out=mid[:], in0=lo[:], in1=hi[:], op=ALU.add), reads=["lo", "hi"], writes=["mid"])
        S.dve(lambda e: e.tensor_scalar(out=mid[:], in0=mid[:], scalar1=0.5, scalar2=None, op0=ALU.mult), reads=["mid"], writes=["mid"])
        S.dve(lambda e: e.tensor_scalar(out=junkC[:], in0=scoresT[:], scalar1=mid[:, 0:1], scalar2=None, op0=ALU.is_gt, op1=ALU.add,
                                        accum_out=cnt[:, 0:1]), reads=["scoresT", "mid"], writes=["cnt", "junkC"])
        S.dve(lambda e: e.tensor_scalar(out=flg[:], in0=cnt[:], scalar1=float(CAP), scalar2=None, op0=ALU.is_ge), reads=["cnt"], writes=["flg"])
        S.dve(lambda e: e.tensor_tensor(out=dlt[:], in0=mid[:], in1=lo[:], op=ALU.subtract), reads=["mid", "lo"], writes=["dlt"])
        S.dve(lambda e: e.scalar_tensor_tensor(out=lo[:], in0=dlt[:], scalar=flg[:, 0:1], in1=lo[:], op0=ALU.mult, op1=ALU.add),
              reads=["dlt", "flg", "lo"], writes=["lo"])
        S.dve(lambda e: e.tensor_tensor(out=dlt[:], in0=hi[:], in1=mid[:], op=ALU.subtract), reads=["mid", "hi", "lo"], writes=["dlt"])
        S.dve(lambda e: e.scalar_tensor_tensor(out=hi[:], in0=dlt[:], scalar=flg[:, 0:1], in1=mid[:], op0=ALU.mult, op1=ALU.add),
              reads=["dlt", "flg", "mid"], writes=["hi"])
    S.dve(lambda e: e.tensor_scalar(out=junkC[:], in0=scoresT[:], scalar1=lo[:, 0:1], scalar2=None, op0=ALU.is_gt), reads=["scoresT", "lo"],
          writes=["junkC"])
    S.dve(lambda e: e.tensor_tensor_scan(out=csum[:], data0=onec[:, 0:1].to_broadcast([NR_, L]), data1=junkC[:], initial=0.0,
                                         op0=ALU.mult, op1=ALU.add), reads=["junkC", "onec"], writes=["csum"])
    for n0 in range(0, NT, 8):
        nn = min(8, NT - n0)
        pb = (n0 // 8) % 2
        for n in range(n0, n0 + nn):
            S.pe(lambda e, n=n, n0=n0, pb=pb: e.transpose(out=banks[pb][:, (n - n0) * NR_:(n - n0 + 1) * NR_], in_=csum[0:NR_, n * 128:(n + 1) * 128],
                                                          identity=ident_f[0:NR_, 0:NR_]), reads=["csum", "ident_f"], writes=["bank%d" % pb])
        S.act(lambda e, n0=n0, nn=nn, pb=pb: e.activation(out=csT[:, n0:n0 + nn, :], in_=banks[pb][:, 0:nn * NR_].rearrange("p (a b) -> p a b", b=NR_),
                                                          func=AF.Copy), reads=["bank%d" % pb], writes=["csT"])
    NRL = NS * NE
    assert NRL <= 32 and CAP <= 512
    onehot = A.alloc("onehot", [128, 32, 32], BF16)
    idxrow = A.alloc("idxrow", [32, CAP], F32)
    S.pool(lambda e: e.memset(onehot[:], 0.0), writes=["onehot"])
    for rl in range(NRL):
        S.pool(lambda e, rl=rl: e.memset(onehot[:, rl, rl:rl + 1], 1.0), reads=["onehot"], writes=["onehot"])
    S.dve(lambda e: e.memset(idx_f[:], 0.0), writes=["idx_f"])
    for rl in range(NRL):
        s_ = rl // NE; e_ = rl % NE
        r = 32 * s_ + e_
        for n in range(NT):
            LE, lek = le_r.next()
            S.dve(lambda e, LE=LE, n=n, r=r: e.tensor_scalar(out=LE[:], in0=iota_c[:], scalar1=csT[:, n, r:r + 1], scalar2=None, op0=ALU.is_ge),
                  reads=["iota_c", "csT"], writes=[lek])
            S.pe(lambda e, LE=LE, rl=rl, n=n: e.matmul(banks[2][0:32, 0:CAP], lhsT=onehot[:, rl, :], rhs=LE[:, 0:CAP],
                                                       start=(rl == 0 and n == 0), stop=(rl == NRL - 1 and n == NT - 1)),
                 reads=[lek, "onehot"], writes=["bank2"])
    S.act(lambda e: e.activation(out=idxrow[:], in_=banks[2][0:32, 0:CAP], func=AF.Copy), reads=["bank2"], writes=["idxrow"])
    for q in range(NQ):
        S.pe(lambda e, q=q: e.transpose(out=banks[3][:, q * 32:(q + 1) * 32], in_=idxrow[0:32, q * 128:(q + 1) * 128],
                                        identity=ident_f[0:32, 0:32]), reads=["idxrow", "ident_f"], writes=["bank3"])
    idx_f3 = idx_f[:].rearrange("p (r q) -> p r q", q=NQ)
    for s in range(NS):
        for q in range(NQ):
            S.dve(lambda e, s=s, q=q: e.tensor_scalar(out=idx_f3[:, 32 * s:32 * s + NE, q],
                                                     in0=banks[3][:, q * 32 + NE * s:q * 32 + NE * s + NE],
                                                     scalar1=float(s * L), scalar2=None, op0=ALU.add),
                  reads=["bank3", "idx_f"], writes=["idx_f"])
    S.dve(lambda e: e.tensor_copy(out=idx_i[:], in_=idx_f[:]), reads=["idx_f"], writes=["idx_i"])
    if dbg:
        d_idx = dout("dbg_idx", [128, NR_ * NQ], I32)
        S.dma("sp", lambda e: e.dma_start(out=d_idx[:, :], in_=idx_i[:]), reads=["idx_i"], writes=["dbg6"])
    S.barrier()
    A.release(m0)

    mD = A.mark()
    g2d = A.alloc("g2d", [128, KD], F32)
    S.dma("sp", lambda e: e.dma_start(out=g2d[:], in_=g2_d[:, :]), writes=["g2d"])
    stg_r = Rot(A, "stgD", 3, [128, KD, 512], F32)
    wg_r = Rot(A, "wg_bf", 2, [128, KD, 512], BF16)
    wu_r = Rot(A, "wu_bf", 2, [128, KD, 512], BF16)
    wd_bf = A.alloc("wd_bf", [128, 16, D], BF16)
    xsT = [A.alloc("xsT%d" % s, [128, KD, CAP], BF16) for s in range(NS)]
    actT = [A.alloc("actT%d" % s, [128, 16, CAP], BF16) for s in range(NS)]
    xsg_r = Rot(A, "xsg", NS * NQ, [128, D], BF16)
    pg_r = Rot(A, "pg", NS * NQ, [128, NE], F32)
    ysb_r = Rot(A, "ysb", 3, [128, D], F32)
    sil_r = Rot(A, "sil", 2, [128, CAP], F32)
    gts2 = [A.alloc("gts%d" % i, [128, NS * NQ], F32) for i in range(2)]
    cast_i = [0]

    def cast(dst, src, reads, writes):
        k = cast_i[0] % 3
        cast_i[0] += 1
        if k == 0:
            S.act(lambda e: e.activation(out=dst, in_=src, func=AF.Copy), reads=reads, writes=writes)
        elif k == 1:
            S.dve(lambda e: e.tensor_copy(out=dst, in_=src), reads=reads, writes=writes)
        else:
            S.pool(lambda e: e.tensor_copy(out=dst, in_=src), reads=reads, writes=writes)

    units = []
    for e_ in range(NE):
        for pc in range(4):
            units.append((e_, "gu", pc))
        units.append((e_, "d", 0))
    ubuf = {}

    def load_unit(u):
        e_, kind, pc = u
        if kind == "gu":
            wg, wgk = wg_r.next(); wu, wuk = wu_r.next()
            ubuf[u] = (wg, wgk, wu, wuk)
            for (w_, wk_, src_d) in ((wg, wgk, wg_d), (wu, wuk, wu_d)):
                st_, stk = stg_r.next()
                S.dma("sp", lambda e, st_=st_, src_d=src_d: e.dma_start(
                    out=st_[:], in_=src_d[e_, :, pc * 512:(pc + 1) * 512].rearrange("(k p) f -> p k f", p=128)), writes=[stk])
                cast(w_[:], st_[:], [stk], [wk_])
        else:
            for fh in range(2):
                for hh in range(2):
                    st_, stk = stg_r.next()
                    S.dma("sp", lambda e, st_=st_, fh=fh, hh=hh: e.dma_start(
                        out=st_[:], in_=wd_d[e_, fh * 1024:(fh + 1) * 1024, hh * 512:(hh + 1) * 512].rearrange("(k p) c -> p k c", p=128)),
                        writes=[stk])
                    cast(wd_bf[:, fh * 8:(fh + 1) * 8, hh * 512:(hh + 1) * 512], st_[:], [stk], ["wd_bf"])

    gbuf = {}

    def gather_dma(e_):
        gts = gts2[e_ % 2]
        for s in range(NS):
            r = 32 * s + e_
            for q in range(NQ):
                col = r * NQ + q
                xsg, xsgk = xsg_r.next()
                gbuf[(e_, s, q)] = (xsg, xsgk)
                S.dma("pool", lambda e, xsg=xsg, col=col: e.indirect_dma_start(
                    out=xsg[:], out_offset=None, in_=h2x_d[:, :], in_offset=bass.IndirectOffsetOnAxis(ap=idx_i[:, col:col + 1], axis=0)),
                    reads=["idx_i"], writes=[xsgk])
                pg, pgk = pg_r.next()
                S.dma("pool", lambda e, pg=pg, col=col: e.indirect_dma_start(
                    out=pg[:], out_offset=None, in_=pr_d[:, :], in_offset=bass.IndirectOffsetOnAxis(ap=idx_i[:, col:col + 1], axis=0)),
                    reads=["idx_i"], writes=[pgk])
                S.dve(lambda e, pg=pg, s=s, q=q, gts=gts: e.tensor_copy(out=gts[:, s * NQ + q:s * NQ + q + 1], in_=pg[:, e_:e_ + 1]),
                      reads=[pgk], writes=["gts%d_%d_%d" % (e_ % 2, s, q)])

    def gather_tr(e_):
        for s in range(NS):
            for q in range(NQ):
                xsg, xsgk = gbuf.pop((e_, s, q))
                pb = 6 + (q % 2)
                for k in range(KD):
                    S.pe(lambda e, xsg=xsg, k=k, pb=pb: e.transpose(out=bankbf(pb)[:, k * 128:(k + 1) * 128], in_=xsg[:, k * 128:(k + 1) * 128],
                                                                   identity=ident_bf[:]), reads=[xsgk, "ident_bf"], writes=["bank%d" % pb])
                S.dve(lambda e, s=s, q=q, pb=pb: e.tensor_tensor(out=xsT[s][:, :, q * 128:(q + 1) * 128],
                                                                 in0=bankbf(pb).rearrange("p (k t) -> p k t", k=KD),
                                                                 in1=g2d[:, :].unsqueeze(2).to_broadcast([128, KD, 128]), op=ALU.mult),
                      reads=["bank%d" % pb, "g2d"], writes=["xsT%d" % s])

    gu_i = [0]

    def compute_unit(u):
        e_, kind, pc = u
        if kind == "gu":
            wg, wgk, wu, wuk = ubuf[u]
            for s in range(NS):
                for ft in range(4):
                    fs = slice(ft * 128, (ft + 1) * 128)
                    ba = gu_i[0] % 2; bu_ = 2 + gu_i[0] % 2
                    gu_i[0] += 1
                    for k in range(KD):
                        S.pe(lambda e, wg=wg, k=k, fs=fs, s=s, ba=ba: e.matmul(banks[ba][:, 0:CAP], lhsT=wg[:, k, fs], rhs=xsT[s][:, k, :],
                                                                                start=(k == 0), stop=(k == KD - 1)),
                             reads=[wgk, "xsT%d" % s], writes=["bank%d" % ba])
                    for k in range(KD):
                        S.pe(lambda e, wu=wu, k=k, fs=fs, s=s, bu_=bu_: e.matmul(banks[bu_][:, 0:CAP], lhsT=wu[:, k, fs], rhs=xsT[s][:, k, :],
                                                                                  start=(k == 0), stop=(k == KD - 1)),
                             reads=[wuk, "xsT%d" % s], writes=["bank%d" % bu_])
                    sil, silk = sil_r.next()
                    S.act(lambda e, sil=sil, ba=ba: e.activation(out=sil[:], in_=banks[ba][:, 0:CAP], func=AF.Silu), reads=["bank%d" % ba], writes=[silk])
                    fc = pc * 4 + ft
                    S.dve(lambda e, sil=sil, bu_=bu_, s=s, fc=fc: e.tensor_tensor(out=actT[s][:, fc, :], in0=sil[:], in1=banks[bu_][:, 0:CAP], op=ALU.mult),
                          reads=[silk, "bank%d" % bu_], writes=["actT%d_%d" % (s, fc)])
        else:
            gts = gts2[e_ % 2]
            for s in range(NS):
                r = 32 * s + e_
                for q in range(NQ):
                    col = r * NQ + q
                    ysb, ysbk = ysb_r.next()
                    for hh in range(2):
                        yb = 4 + hh
                        for fc in range(16):
                            S.pe(lambda e, s=s, q=q, hh=hh, fc=fc, yb=yb: e.matmul(banks[yb][:], lhsT=actT[s][:, fc, q * 128:(q + 1) * 128],
                                                                                   rhs=wd_bf[:, fc, hh * 512:(hh + 1) * 512], start=(fc == 0), stop=(fc == 15)),
                                 reads=["actT%d_%d" % (s, fc), "wd_bf"], writes=["bank%d" % yb])
                        if hh == 0:
                            S.dve(lambda e, ysb=ysb, s=s, q=q, yb=yb, gts=gts: e.tensor_scalar(out=ysb[:, 0:512], in0=banks[yb][:],
                                                                                     scalar1=gts[:, s * NQ + q:s * NQ + q + 1], scalar2=None, op0=ALU.mult),
                                  reads=["bank%d" % yb, "gts%d_%d_%d" % (e_ % 2, s, q)], writes=[ysbk + "0"])
                        else:
                            S.act(lambda e, ysb=ysb, s=s, q=q, yb=yb, gts=gts: e.activation(out=ysb[:, 512:1024], in_=banks[yb][:], func=AF.Copy,
                                                                                  scale=gts[:, s * NQ + q:s * NQ + q + 1]),
                                  reads=["bank%d" % yb, "gts%d_%d_%d" % (e_ % 2, s, q)], writes=[ysbk + "1"])
                    S.dma("pool", lambda e, ysb=ysb, col=col: e.indirect_dma_start(
                        out=acc_d[:, :], out_offset=bass.IndirectOffsetOnAxis(ap=idx_i[:, col:col + 1], axis=0), in_=ysb[:], in_offset=None,
                        compute_op=ALU.add), reads=[ysbk + "0", ysbk + "1", "idx_i"], writes=["acc_all"])

    load_unit(units[0])
    gather_dma(0)
    for ui, u in enumerate(units):
        if u[1] == "gu" and u[2] == 0:
            gather_tr(u[0])
        if u[1] == "gu" and u[2] == 3 and u[0] + 1 < NE:
            gather_dma(u[0] + 1)
        if ui + 1 < len(units):
            load_unit(units[ui + 1])
        compute_unit(u)
    S.barrier()
    A.release(mD)

    gf = A.alloc("gf", [128, D], F32)
    epsb3 = A.alloc("epsb3", [128, 1], F32)
    S.pool(lambda e: e.memset(epsb3[:], EPS), writes=["epsb3"])
    S.dma("sp", lambda e: e.dma_start(out=gf[:], in_=gf_d[:, :]), writes=["gf"])
    xa_r = Rot(A, "xa", 6, [128, D], F32)
    xo_r = Rot(A, "xo", 6, [128, D], F32)
    se_r = Rot(A, "se", 6, [128, 4], F32)
    junkE = A.alloc("junkE", [128, D], BF16)
    for t in range(NTOK // 128):
        xa, xak = xa_r.next(); xo, xok = xo_r.next(); se, sek = se_r.next()
        S.dma("sp", lambda e, t=t, xa=xa: e.dma_start(out=xa[:], in_=acc_d[t * 128:(t + 1) * 128, :]), writes=[xak])
        S.act(lambda e, xa=xa, xo=xo, se=se: e.activation(out=xo[:], in_=xa[:], func=AF.Square, accum_out=se[:, 0:1]), reads=[xak],
              writes=[sek + "a", xok])
        S.act(lambda e, se=se: e.activation(out=se[:, 1:2], in_=se[:, 0:1], func=AF.Sqrt, bias=epsb3[:, 0:1], scale=1.0 / D),
              reads=[sek + "a", "epsb3"], writes=[sek + "b"])
        S.dve(lambda e, se=se: e.reciprocal(out=se[:, 2:3], in_=se[:, 1:2]), reads=[sek + "b"], writes=[sek + "c"])
        S.dve(lambda e, xa=xa, xo=xo, se=se: e.scalar_tensor_tensor(out=xo[:], in0=xa[:], scalar=se[:, 2:3], in1=gf[:], op0=ALU.mult, op1=ALU.mult),
              reads=[xak, sek + "c", "gf"], writes=[xok])
        S.dma(STQ, lambda e, t=t, xo=xo: e.dma_start(out=out_d[t * 128:(t + 1) * 128, :], in_=xo[:]), reads=[xok], writes=["o%d" % t])

    S.emit()
    return nc, S, A


def _f32(a):
    return np.ascontiguousarray(np.asarray(a, dtype=np.float32))


def prep_shared(inp):
    o = {}
    o["g1"] = _f32(inp["norm1_g"][0].reshape(KD, 128).T)
    o["w_in"] = _f32(inp["w_in"][0])
    o["b_gate"] = _f32(inp["b_gate"][0].reshape(16, 128).T)
    o["ln_g"] = _f32(np.broadcast_to(inp["gmlp_ln_g"][0][None, :], (128, GW)))
    o["ln_b"] = _f32(np.broadcast_to(inp["gmlp_ln_b"][0][None, :], (128, GW)))
    o["wsT"] = _f32(np.transpose(inp["gmlp_w_s"][0], (2, 0, 1)))
    o["bstab"] = _f32(np.repeat(inp["gmlp_b_s"][0].T[:, :, None], 64, axis=2).reshape(128, GW))
    lam_re = np.asarray(inp["s5_lam_re"][0]); lam_im = np.asarray(inp["s5_lam_im"][0]); log_dt = np.asarray(inp["s5_log_dt"][0])
    b_re = np.asarray(inp["s5_b_re"][0]); b_im = np.asarray(inp["s5_b_im"][0])
    c_re = np.asarray(inp["s5_c_re"][0]); c_im = np.asarray(inp["s5_c_im"][0])
    def col(a3):
        return _f32(a3.reshape(2, 8, 128).transpose(2, 0, 1).reshape(128, 16))
    o["lamre_c"] = col(lam_re)
    o["lamim_c"] = col(lam_im)
    ldt = np.repeat(log_dt[:, :, None], 64, axis=2)
    o["logdt_c"] = col(ldt)
    def row(a3):
        return _f32(np.broadcast_to(a3.reshape(1, 2048), (128, 2048)))
    o["lamre_r"] = row(lam_re); o["lamim_r"] = row(lam_im); o["logdt_r"] = row(ldt)
    def braw(bm):
        outp = np.zeros((128, 2, 8, 128), np.float32)
        for j in range(8):
            for gg in range(2):
                g = 2 * j + gg
                cl = 16 * (g % 8)
                outp[cl:cl + 16, :, j, gg * 64:(gg + 1) * 64] = np.transpose(bm[:, g, :, :], (2, 0, 1))
        return _f32(outp.reshape(128, 2048))
    o["braw_re"] = braw(b_re); o["braw_im"] = braw(b_im)
    def craw(cm):
        outp = np.zeros((128, 2, 8, 128), np.float32)
        for j in range(8):
            for gg in range(2):
                g = 2 * j + gg
                cl = 16 * (g % 8)
                outp[gg * 64:(gg + 1) * 64, :, j, cl:cl + 16] = np.transpose(cm[:, g, :, :], (2, 0, 1))
        return _f32(outp.reshape(128, 2048))
    o["craw_re"] = craw(c_re); o["craw_im"] = craw(c_im)
    o["dskip"] = _f32(np.asarray(inp["s5_d"][0]).reshape(2, 128).T)
    o["w_glu"] = _f32(inp["s5_w_glu"][0])
    o["b_glu"] = _f32(np.asarray(inp["s5_b_glu"][0]).reshape(2, 128).T)
    o["w_up_a"] = _f32(inp["w_up_a"][0]); o["w_up_b"] = _f32(inp["w_up_b"][0]); o["w_out"] = _f32(inp["w_out"][0])
    o["g2"] = _f32(np.asarray(inp["norm2_g"][0]).reshape(KD, 128).T)
    o["w_router"] = _f32(inp["w_router"][0])
    o["w_gate"] = _f32(inp["w_gate"][0]); o["w_up"] = _f32(inp["w_up"][0]); o["w_down"] = _f32(inp["w_down"][0])
    o["gf"] = _f32(np.broadcast_to(np.asarray(inp["final_g"])[None, :], (128, D)))
    return o


def kernel(**inputs):
    x = np.asarray(inputs["x"], dtype=np.float32)
    B, L, _ = x.shape
    NCORE = 8
    NS = B // NCORE
    shared = prep_shared(inputs)
    nc, S, A = build(NS, L)
    in_maps = []
    for c in range(NCORE):
        m = dict(shared)
        m["x"] = np.ascontiguousarray(x[c * NS:(c + 1) * NS])
        in_maps.append(m)
    res = run_bass_kernel_spmd(nc, in_maps, core_ids=list(range(NCORE)))
    outs = [np.asarray(r["out"]).reshape(NS, L, D) for r in res.results]
    return np.concatenate(outs, axis=0).astype(np.float32)
```

```python
import contextlib
import math
import numpy as np
import concourse.bass as bass
import concourse.mybir as mybir
from concourse.bass_utils import run_bass_kernel_spmd

F32 = mybir.dt.float32
BF16 = mybir.dt.bfloat16
I32 = mybir.dt.int32
I16 = mybir.dt.int16
AF = mybir.ActivationFunctionType
ALU = mybir.AluOpType
AX = mybir.AxisListType

D = 1024
KD = 8
GW = 512
S5W = 256
PT = 3328
NE = 16
FF = 2048
EPS = 1e-6
TB = 512
GT = 512
SB_BASE = 16512
SB_END = 229376

ENGS = ("pe", "act", "dve", "pool", "sp")
NDMA_SEMS = 8


class Op:
    __slots__ = ("id", "eng", "fn", "deps", "dma", "signal", "seq", "sem_idx", "waits", "prewait")

    def __init__(self, id, eng, fn, dma):
        self.id = id
        self.eng = eng
        self.fn = fn
        self.dma = dma
        self.deps = set()
        self.signal = False
        self.seq = 0
        self.sem_idx = -1
        self.waits = []
        self.prewait = None


class Sched:
    def __init__(self, nc):
        self.nc = nc
        self.ops = []
        self.last_writer = {}
        self.readers = {}
        self.last_on_eng = {}

    def add(self, eng, fn, reads=(), writes=(), dma=False):
        op = Op(len(self.ops), eng, fn, dma)
        deps = op.deps
        for t in reads:
            w = self.last_writer.get(t)
            if w is not None:
                deps.add(w)
            if t.startswith("bank"):
                for r in self.readers.get(t, ()):
                    if self.ops[r].eng != eng:
                        deps.add(r)
        for t in writes:
            w = self.last_writer.get(t)
            if w is not None:
                deps.add(w)
            for r in self.readers.get(t, ()):
                deps.add(r)
        for t in reads:
            self.readers.setdefault(t, []).append(op.id)
        for t in writes:
            self.last_writer[t] = op.id
            self.readers[t] = []
        deps.discard(op.id)
        self.ops.append(op)
        self.last_on_eng[eng] = op.id
        return op

    def pe(self, fn, reads=(), writes=()):
        return self.add("pe", fn, reads, writes)

    def act(self, fn, reads=(), writes=()):
        return self.add("act", fn, reads, writes)

    def dve(self, fn, reads=(), writes=()):
        return self.add("dve", fn, reads, writes)

    def pool(self, fn, reads=(), writes=()):
        return self.add("pool", fn, reads, writes)

    def any(self, eng, fn, reads=(), writes=()):
        return self.add(eng, fn, reads, writes)

    def dma(self, eng, fn, reads=(), writes=()):
        return self.add(eng, fn, reads, writes, dma=True)

    def barrier(self):
        pend = set()
        for t, w in self.last_writer.items():
            pend.add(w)
        for t, rs in self.readers.items():
            pend.update(rs)
        pend.update(self.last_on_eng.values())
        for op in self.ops:
            if op.dma:
                pend.add(op.id)
        ids = []
        for e in ENGS:
            op = Op(len(self.ops), e, (lambda eng: eng.nop()), False)
            op.deps = set(pend)
            self.ops.append(op)
            self.last_on_eng[e] = op.id
            ids.append(op.id)
        self.last_writer = {}
        self.readers = {}
        self._dma_done_upto = len(self.ops)

    def emit(self, final_wait_eng="sp"):
        nc = self.nc
        ops = self.ops

        def skip(dop, op):
            return dop.eng == "pe" and op.eng == "pe" and not dop.dma and not op.dma

        for op in ops:
            for d in op.deps:
                dop = ops[d]
                if skip(dop, op):
                    continue
                dop.signal = True
        for op in ops:
            if op.dma:
                op.signal = True
        eng_cnt = {e: 0 for e in ENGS}
        dma_cnt = {e: [0] * NDMA_SEMS for e in ENGS}
        dma_rr = {e: 0 for e in ENGS}
        for op in ops:
            if op.dma:
                k = dma_rr[op.eng] % NDMA_SEMS
                dma_rr[op.eng] += 1
                op.sem_idx = k
                op.prewait = dma_cnt[op.eng][k]
                dma_cnt[op.eng][k] += 16
                op.seq = dma_cnt[op.eng][k]
            elif op.signal:
                eng_cnt[op.eng] += 1
                op.seq = eng_cnt[op.eng]
        waited = {e: {} for e in ENGS}
        for op in ops:
            w = waited[op.eng]
            need = {}
            for d in op.deps:
                dop = ops[d]
                if skip(dop, op):
                    continue
                key = ("d", dop.eng, dop.sem_idx) if dop.dma else ("e", dop.eng)
                if dop.seq > need.get(key, 0):
                    need[key] = dop.seq
            if op.dma and op.prewait:
                key = ("d", op.eng, op.sem_idx)
                if op.prewait > need.get(key, 0):
                    need[key] = op.prewait
            for key, v in need.items():
                if w.get(key, 0) >= v:
                    continue
                w[key] = v
                op.waits.append((key, v))
        self.stats = dict(n_ops=len(ops), eng_cnt=dict(eng_cnt),
                          per_eng={e: sum(1 for o in ops if o.eng == e) for e in ENGS})
        with contextlib.ExitStack() as st:
            esem = {e: st.enter_context(nc.semaphore("se_" + e)) for e in ENGS}
            dsem = {e: [st.enter_context(nc.semaphore("sd_%s%d" % (e, i))) for i in range(NDMA_SEMS)]
                    for e in ENGS if dma_rr[e] > 0}
            block = st.enter_context(nc.Block())

            def semof(key):
                if key[0] == "e":
                    return esem[key[1]]
                return dsem[key[1]][key[2]]

            def run_engine(ename, eng):
                for op in ops:
                    if op.eng != ename:
                        continue
                    for key, v in op.waits:
                        eng.wait_ge(semof(key), v)
                    ins = op.fn(eng)
                    if op.dma:
                        ins.then_inc(dsem[ename][op.sem_idx], 16)
                    elif op.signal:
                        ins.then_inc(esem[ename], 1)
                if ename == final_wait_eng:
                    for e2 in dsem:
                        for i in range(NDMA_SEMS):
                            if dma_cnt[e2][i] > 0:
                                eng.wait_ge(dsem[e2][i], dma_cnt[e2][i])
                    for e2 in ENGS:
                        if eng_cnt[e2] > 0:
                            eng.wait_ge(esem[e2], eng_cnt[e2])

            @block.tensor
            def _(eng):
                run_engine("pe", eng)

            @block.scalar
            def _(eng):
                run_engine("act", eng)

            @block.vector
            def _(eng):
                run_engine("dve", eng)

            @block.gpsimd
            def _(eng):
                run_engine("pool", eng)

            @block.sync
            def _(eng):
                run_engine("sp", eng)


class Arena:
    def __init__(self, nc):
        self.nc = nc
        self.off = SB_BASE
        self.n = 0
        self.peak = SB_BASE

    def alloc(self, name, shape, dt):
        esz = 4 if dt in (F32, I32) else 2
        nbytes = esz * int(np.prod(shape[1:]))
        off = (self.off + 31) // 32 * 32
        assert off + nbytes <= SB_END, ("SBUF overflow", name, off, nbytes)
        self.n += 1
        t = self.nc.alloc_sbuf_tensor_at("%s_%d" % (name, self.n), list(shape), dt, offset=off)
        self.off = off + nbytes
        self.peak = max(self.peak, self.off)
        return t

    def mark(self):
        return self.off

    def release(self, m):
        self.off = m


class Rot:
    def __init__(self, arena, name, n, shape, dt):
        self.t = [arena.alloc("%s%d" % (name, i), shape, dt) for i in range(n)]
        self.name = name
        self.i = -1
        self.n = n

    def next(self):
        self.i += 1
        k = self.i % self.n
        return self.t[k], "%s#%d" % (self.name, k)


import os
HTQ = os.environ.get('HTQ', 'pool')
STQ = os.environ.get('STQ', 'pool')
P1STOP = int(os.environ.get('P1STOP', '9'))
S5POOL = int(os.environ.get('S5POOL', '2'))
HTGB = int(os.environ.get('HTGB', '1'))
VAR = os.environ.get('VAR', '')
TWO_PI_HI = 6.28125
TWO_PI_LO = 2.0 * math.pi - 6.28125
PI_CLAMP = 3.1415925


def build(NS, L, upto=99, dbg=False, cap=None):
    nc = bass.Bass("TRN2", target_bir_lowering=False)
    NG = L // GT
    NB = L // TB
    NT = L // 128
    CAP = cap if cap is not None else 2 * L // NE
    NQ = CAP // 128
    NTOK = NS * L

    def din(name, shape, dt=F32):
        return nc.dram_tensor(name, list(shape), dt, kind="ExternalInput").ap()

    def dscr(name, shape, dt):
        return nc.dram_tensor(name, list(shape), dt, kind="Internal").ap()

    x_d = din("x", [NS, L, D])
    g1_d = din("g1", [128, KD])
    win_d = din("w_in", [D, PT])
    bgate_d = din("b_gate", [128, 16])
    lng_d = din("ln_g", [128, GW])
    lnb_d = din("ln_b", [128, GW])
    wsT_d = din("wsT", [128, 8, 128])
    bstab_d = din("bstab", [128, GW])
    lamre_c_d = din("lamre_c", [128, 16])
    lamim_c_d = din("lamim_c", [128, 16])
    logdt_c_d = din("logdt_c", [128, 16])
    lamre_r_d = din("lamre_r", [128, 2048])
    lamim_r_d = din("lamim_r", [128, 2048])
    logdt_r_d = din("logdt_r", [128, 2048])
    braw_re_d = din("braw_re", [128, 2048])
    braw_im_d = din("braw_im", [128, 2048])
    craw_re_d = din("craw_re", [128, 2048])
    craw_im_d = din("craw_im", [128, 2048])
    dskip_d = din("dskip", [128, 2])
    wglu_d = din("w_glu", [S5W, S5W])
    bglu_d = din("b_glu", [128, 2])
    wupa_d = din("w_up_a", [GW, D])
    wupb_d = din("w_up_b", [S5W, D])
    wout_d = din("w_out", [D, D])
    g2_d = din("g2", [128, KD])
    wr_d = din("w_router", [D, NE])
    wg_d = din("w_gate", [NE, D, FF])
    wu_d = din("w_up", [NE, D, FF])
    wd_d = din("w_down", [NE, FF, D])
    gf_d = din("gf", [128, D])
    out_d = nc.dram_tensor("out", [NTOK, D], F32, kind="ExternalOutput").ap()

    hT_d = dscr("hT_scr", [NS, KD, 128, L], BF16)
    acc_d = dscr("acc_scr", [NTOK, D], F32)
    h2_d = dscr("h2_scr", [NTOK, D], BF16)
    dbg_outs = {}

    def dout(name, shape, dt=F32):
        t = nc.dram_tensor(name, list(shape), dt, kind="ExternalOutput").ap()
        dbg_outs[name] = t
        return t

    S = Sched(nc)
    A = Arena(nc)
    banks = [nc.alloc_psum_tensor("bank%d" % i, [128, 512], F32) for i in range(8)]

    def bankbf(i):
        return banks[i][:].bitcast(BF16)

    ident_bf = A.alloc("ident_bf", [128, 128], BF16)
    ident_f = A.alloc("ident_f", [128, 128], F32)
    halfpi = A.alloc("halfpi", [128, 1], F32)
    S.pool(lambda e: e.memset(ident_f[:], 1.0), writes=["ident_f"])
    S.pool(lambda e: e.affine_select(out=ident_f[:], in_=ident_f[:], pattern=[[-1, 128]], compare_op=ALU.is_equal,
                                    fill=0.0, base=0, channel_multiplier=1), reads=["ident_f"], writes=["ident_f"])
    S.dve(lambda e: e.tensor_copy(out=ident_bf[:], in_=ident_f[:]), reads=["ident_f"], writes=["ident_bf"])
    S.pool(lambda e: e.memset(halfpi[:], math.pi / 2.0), writes=["halfpi"])
    idx_i = A.alloc("idx_i", [128, 64 * NQ], I32)
    m0 = A.mark()
    brbT = [A.alloc("brbT%d" % s, [128, 2, L], BF16) for s in range(NS)]

    mA = A.mark()
    wins5 = A.alloc("wins5", [128, KD, S5W], BF16)
    g1 = A.alloc("g1", [128, KD], F32)
    bbT_re = A.alloc("bbT_re", [128, 16, 128], BF16)
    bbT_im = A.alloc("bbT_im", [128, 16, 128], BF16)
    cT_re = A.alloc("cT_re", [128, 16, 128], BF16)
    cT_imn = A.alloc("cT_imn", [128, 16, 128], BF16)
    cosT = A.alloc("cosT", [128, 16, TB], F32)
    sinT = A.alloc("sinT", [128, 16, TB], F32)
    rcol = A.alloc("rcol", [128, 16], F32)
    pblk_r = A.alloc("pblk_r", [128, 16], F32)
    pblk_i = A.alloc("pblk_i", [128, 16], F32)
    pblk_in = A.alloc("pblk_in", [128, 16], F32)
    dskip = A.alloc("dskip", [128, 2], F32)
    wglu = A.alloc("wglu", [128, 2, S5W], BF16)
    bglu = A.alloc("bglu", [128, 2], F32)

    S.dma("sp", lambda e: e.dma_start(out=g1[:], in_=g1_d[:, :]), writes=["g1"])
    S.dma("sp", lambda e: e.dma_start(out=dskip[:], in_=dskip_d[:, :]), writes=["dskip"])
    S.dma("sp", lambda e: e.dma_start(out=bglu[:], in_=bglu_d[:, :]), writes=["bglu"])

    mS = A.mark()
    stg = A.alloc("stg_s5w", [128, KD, S5W], F32)
    S.dma("sp", lambda e: e.dma_start(out=stg[:], in_=win_d[:, 2 * GW:2 * GW + S5W].rearrange("(k p) c -> p k c", p=128)),
          writes=["stg"])
    for k in range(KD):
        S.dve(lambda e, k=k: e.tensor_scalar(out=wins5[:, k, :], in0=stg[:, k, :], scalar1=g1[:, k:k + 1], scalar2=None,
                                             op0=ALU.mult), reads=["stg", "g1"], writes=["wins5"])
    stg2 = A.alloc("stg_glu", [128, 2, S5W], F32)
    S.dma("sp", lambda e: e.dma_start(out=stg2[:], in_=wglu_d[:, :].rearrange("(k p) c -> p k c", p=128)), writes=["stg2"])
    S.dve(lambda e: e.tensor_copy(out=wglu[:], in_=stg2[:]), reads=["stg2"], writes=["wglu"])

    def s5_scalars(tag, F, lamre_d, lamim_d, logdt_d):
        T = {}

        def al(n):
            T[n] = A.alloc(tag + n, [128, F], F32)
            return T[n]

        lamre = al("lamre"); lamim = al("lamim"); logdt = al("logdt")
        S.dma("sp", lambda e: e.dma_start(out=lamre[:], in_=lamre_d[:, :]), writes=[tag + "lamre"])
        S.dma("sp", lambda e: e.dma_start(out=lamim[:], in_=lamim_d[:, :]), writes=[tag + "lamim"])
        S.dma("sp", lambda e: e.dma_start(out=logdt[:], in_=logdt_d[:, :]), writes=[tag + "logdt"])
        dt = al("dt"); r = al("r"); th = al("th"); y = al("y"); kk = al("kk")
        s1 = al("s1"); c1 = al("c1"); t1 = al("t1"); t2 = al("t2"); cre = al("cre"); cim = al("cim")
        tk = lambda n: tag + n

        def dv(fn, reads, writes):
            S.dve(fn, reads=[tk(n) for n in reads], writes=[tk(n) for n in writes])

        def ac(fn, reads, writes):
            S.act(fn, reads=[tk(n) for n in reads], writes=[tk(n) for n in writes])

        def dve_exp(out_t, on, in_t, inn, offset, deg):
            dv(lambda e: e.tensor_scalar(out=t2[:], in0=in_t[:], scalar1=float(offset), scalar2=None, op0=ALU.add), [inn], ["t2"])
            dv(lambda e: e.tensor_scalar(out=out_t[:], in0=t2[:], scalar1=1.0 / math.factorial(deg), scalar2=None, op0=ALU.mult),
               ["t2"], [on])
            for kq in range(deg - 1, 0, -1):
                dv(lambda e, kq=kq: e.scalar_tensor_tensor(out=out_t[:], in0=out_t[:], scalar=1.0 / math.factorial(kq), in1=t2[:],
                                                           op0=ALU.add, op1=ALU.mult), [on, "t2"], [on])
            dv(lambda e: e.tensor_scalar(out=out_t[:], in0=out_t[:], scalar1=1.0, scalar2=math.exp(-offset), op0=ALU.add,
                                         op1=ALU.mult), [on], [on])
        dve_exp(dt, "dt", logdt, "logdt", 6.9375, 27)
        dv(lambda e: e.tensor_tensor(out=t1[:], in0=lamre[:], in1=dt[:], op=ALU.mult), ["lamre", "dt"], ["t1"])
        dv(lambda e: e.tensor_scalar(out=t1[:], in0=t1[:], scalar1=-1.0, scalar2=None, op0=ALU.mult), ["t1"], ["t1"])
        dve_exp(r, "r", t1, "t1", 0.0, 8)
        dv(lambda e: e.reciprocal(out=r[:], in_=r[:]), ["r"], ["r"])
        dv(lambda e: e.tensor_tensor(out=th[:], in0=lamim[:], in1=dt[:], op=ALU.mult), ["lamim", "dt"], ["th"])
        dv(lambda e: e.tensor_scalar(out=y[:], in0=th[:], scalar1=1.0 / (2.0 * math.pi), scalar2=None, op0=ALU.mult),
           ["th"], ["y"])
        dv(lambda e: e.tensor_scalar(out=kk[:], in0=y[:], scalar1=0.5, scalar2=None, op0=ALU.is_gt), ["y"], ["kk"])
        for m in range(1, 8):
            dv(lambda e, m=m: e.scalar_tensor_tensor(out=kk[:], in0=y[:], scalar=m + 0.5, in1=kk[:], op0=ALU.is_gt,
                                                     op1=ALU.add), ["y", "kk"], ["kk"])
        dv(lambda e: e.scalar_tensor_tensor(out=th[:], in0=kk[:], scalar=-TWO_PI_HI, in1=th[:], op0=ALU.mult, op1=ALU.add),
           ["kk", "th"], ["th"])
        dv(lambda e: e.scalar_tensor_tensor(out=th[:], in0=kk[:], scalar=-TWO_PI_LO, in1=th[:], op0=ALU.mult, op1=ALU.add),
           ["kk", "th"], ["th"])
        dv(lambda e: e.tensor_scalar(out=th[:], in0=th[:], scalar1=PI_CLAMP, scalar2=-PI_CLAMP, op0=ALU.min, op1=ALU.max),
           ["th"], ["th"])
        ac(lambda e: e.activation(out=s1[:], in_=th[:], func=AF.Sin), ["th"], ["s1"])
        dv(lambda e: e.scalar_tensor_tensor(out=t2[:], in0=th[:], scalar=-1.0, in1=th[:], op0=ALU.mult, op1=ALU.max), ["th"], ["t2"])
        S.act(lambda e: e.activation(out=c1[:], in_=t2[:], func=AF.Sin, bias=halfpi[:, 0:1], scale=-1.0),
              reads=[tk("t2"), "halfpi"], writes=[tk("c1")])
        lbr = y; lbi = kk
        dv(lambda e: e.tensor_tensor(out=lbr[:], in0=r[:], in1=c1[:], op=ALU.mult), ["r", "c1"], ["y"])
        dv(lambda e: e.tensor_tensor(out=lbi[:], in0=r[:], in1=s1[:], op=ALU.mult), ["r", "s1"], ["kk"])
        dv(lambda e: e.tensor_scalar(out=lbr[:], in0=lbr[:], scalar1=-1.0, scalar2=None, op0=ALU.add), ["y"], ["y"])
        dv(lambda e: e.tensor_tensor(out=t1[:], in0=lamre[:], in1=lamre[:], op=ALU.mult), ["lamre"], ["t1"])
        dv(lambda e: e.tensor_tensor(out=t2[:], in0=lamim[:], in1=lamim[:], op=ALU.mult), ["lamim"], ["t2"])
        dv(lambda e: e.tensor_tensor(out=t1[:], in0=t1[:], in1=t2[:], op=ALU.add), ["t1", "t2"], ["t1"])
        dv(lambda e: e.reciprocal(out=t1[:], in_=t1[:]), ["t1"], ["t1"])
        dv(lambda e: e.tensor_tensor(out=cre[:], in0=lbr[:], in1=lamre[:], op=ALU.mult), ["y", "lamre"], ["cre"])
        dv(lambda e: e.tensor_tensor(out=t2[:], in0=lbi[:], in1=lamim[:], op=ALU.mult), ["kk", "lamim"], ["t2"])
        dv(lambda e: e.tensor_tensor(out=cre[:], in0=cre[:], in1=t2[:], op=ALU.add), ["cre", "t2"], ["cre"])
        dv(lambda e: e.tensor_tensor(out=cre[:], in0=cre[:], in1=t1[:], op=ALU.mult), ["cre", "t1"], ["cre"])
        dv(lambda e: e.tensor_tensor(out=cim[:], in0=lbi[:], in1=lamre[:], op=ALU.mult), ["kk", "lamre"], ["cim"])
        dv(lambda e: e.tensor_tensor(out=t2[:], in0=lbr[:], in1=lamim[:], op=ALU.mult), ["y", "lamim"], ["t2"])
        dv(lambda e: e.tensor_tensor(out=cim[:], in0=cim[:], in1=t2[:], op=ALU.subtract), ["cim", "t2"], ["cim"])
        dv(lambda e: e.tensor_tensor(out=cim[:], in0=cim[:], in1=t1[:], op=ALU.mult), ["cim", "t1"], ["cim"])
        return dict(r=r, c1=c1, s1=s1, cre=cre, cim=cim), tk

    colT, ctk = s5_scalars("c_", 16, lamre_c_d, lamim_c_d, logdt_c_d)
    S.dve(lambda e: e.tensor_copy(out=rcol[:], in_=colT["r"][:]), reads=[ctk("r")], writes=["rcol"])
    pr = A.alloc("pr", [128, 16], F32); pi_ = A.alloc("pi", [128, 16], F32)
    pt1 = A.alloc("pt1", [128, 16], F32); pt2 = A.alloc("pt2", [128, 16], F32)
    S.dve(lambda e: e.tensor_copy(out=pr[:], in_=colT["c1"][:]), reads=[ctk("c1")], writes=["pr"])
    S.dve(lambda e: e.tensor_copy(out=pi_[:], in_=colT["s1"][:]), reads=[ctk("s1")], writes=["pi"])
    S.pool(lambda e: e.memset(cosT[:, :, 0:1], 1.0), writes=["cosT"])
    S.pool(lambda e: e.memset(sinT[:, :, 0:1], 0.0), writes=["sinT"])
    tq = [A.alloc("tq%d" % i, [128, 16, TB // 2], F32) for i in range(4)]
    k = 1
    while k < TB:
        def bc(t, k=k):
            return t[:, :].unsqueeze(2).to_broadcast([128, 16, k])
        Ck = cosT[:, :, 0:k]; Sk = sinT[:, :, 0:k]
        S.dve(lambda e, k=k, Ck=Ck: e.tensor_tensor(out=tq[0][:, :, 0:k], in0=Ck, in1=bc(pr, k), op=ALU.mult),
              reads=["cosT", "pr"], writes=["tq0"])
        S.dve(lambda e, k=k, Sk=Sk: e.tensor_tensor(out=tq[1][:, :, 0:k], in0=Sk, in1=bc(pi_, k), op=ALU.mult),
              reads=["sinT", "pi"], writes=["tq1"])
        S.dve(lambda e, k=k, Ck=Ck: e.tensor_tensor(out=tq[2][:, :, 0:k], in0=Ck, in1=bc(pi_, k), op=ALU.mult),
              reads=["cosT", "pi"], writes=["tq2"])
        S.dve(lambda e, k=k, Sk=Sk: e.tensor_tensor(out=tq[3][:, :, 0:k], in0=Sk, in1=bc(pr, k), op=ALU.mult),
              reads=["sinT", "pr"], writes=["tq3"])
        S.dve(lambda e, k=k: e.tensor_tensor(out=cosT[:, :, k:2 * k], in0=tq[0][:, :, 0:k], in1=tq[1][:, :, 0:k],
                                             op=ALU.subtract), reads=["tq0", "tq1"], writes=["cosT"])
        S.dve(lambda e, k=k: e.tensor_tensor(out=sinT[:, :, k:2 * k], in0=tq[2][:, :, 0:k], in1=tq[3][:, :, 0:k],
                                             op=ALU.add), reads=["tq2", "tq3"], writes=["sinT"])
        S.dve(lambda e: e.tensor_tensor(out=pt1[:], in0=pr[:], in1=pr[:], op=ALU.mult), reads=["pr"], writes=["pt1"])
        S.dve(lambda e: e.tensor_tensor(out=pt2[:], in0=pi_[:], in1=pi_[:], op=ALU.mult), reads=["pi"], writes=["pt2"])
        S.dve(lambda e: e.tensor_tensor(out=pt2[:], in0=pt1[:], in1=pt2[:], op=ALU.subtract), reads=["pt1", "pt2"],
              writes=["pt2"])
        S.dve(lambda e: e.tensor_tensor(out=pt1[:], in0=pr[:], in1=pi_[:], op=ALU.mult), reads=["pr", "pi"], writes=["pt1"])
        S.dve(lambda e: e.tensor_copy(out=pr[:], in_=pt2[:]), reads=["pt2"], writes=["pr"])
        S.dve(lambda e: e.tensor_scalar(out=pi_[:], in0=pt1[:], scalar1=2.0, scalar2=None, op0=ALU.mult), reads=["pt1"],
              writes=["pi"])
        k *= 2
    S.dve(lambda e: e.tensor_copy(out=pblk_r[:], in_=pr[:]), reads=["pr"], writes=["pblk"])
    S.dve(lambda e: e.tensor_copy(out=pblk_i[:], in_=pi_[:]), reads=["pi"], writes=["pblk"])
    S.dve(lambda e: e.tensor_scalar(out=pblk_in[:], in0=pi_[:], scalar1=-1.0, scalar2=None, op0=ALU.mult), reads=["pi"],
          writes=["pblk"])
    S.barrier()
    A.release(mS)
    for dd_ in range(2):
        mR = A.mark()
        hs = slice(dd_ * 1024, (dd_ + 1) * 1024)
        rowT, rtk = s5_scalars("r%d_" % dd_, 1024, lamre_r_d[:, hs], lamim_r_d[:, hs], logdt_r_d[:, hs])
        braw_re = rowT["r"]; braw_im = rowT["c1"]
        S.dma("sp", lambda e, braw_re=braw_re, hs=hs: e.dma_start(out=braw_re[:], in_=braw_re_d[:, hs]),
              reads=[rtk("cre"), rtk("cim")], writes=[rtk("r")])
        S.dma("sp", lambda e, braw_im=braw_im, hs=hs: e.dma_start(out=braw_im[:], in_=braw_im_d[:, hs]),
              reads=[rtk("cre"), rtk("cim")], writes=[rtk("c1")])
        u1 = rowT["s1"]
        cre = rowT["cre"]; cim = rowT["cim"]
        bbT_re_f = bbT_re[:, dd_ * 8:(dd_ + 1) * 8, :].rearrange("p a b -> p (a b)")
        bbT_im_f = bbT_im[:, dd_ * 8:(dd_ + 1) * 8, :].rearrange("p a b -> p (a b)")
        tA = A.alloc("rowtA", [128, 1024], F32)
        tAk = "rowtA%d" % dd_
        S.dve(lambda e, u1=u1, cim=cim, braw_im=braw_im: e.tensor_tensor(out=u1[:], in0=cim[:], in1=braw_im[:], op=ALU.mult),
              reads=[rtk("cim"), rtk("c1")], writes=[rtk("s1")])
        S.dve(lambda e, tA=tA, cre=cre, braw_re=braw_re: e.tensor_tensor(out=tA[:], in0=cre[:], in1=braw_re[:], op=ALU.mult),
              reads=[rtk("cre"), rtk("r")], writes=[tAk])
        S.dve(lambda e, tA=tA, u1=u1, bbT_re_f=bbT_re_f: e.tensor_tensor(out=bbT_re_f, in0=tA[:], in1=u1[:], op=ALU.subtract),
              reads=[tAk, rtk("s1")], writes=["bbT_re"])
        S.dve(lambda e, u1=u1, cim=cim, braw_re=braw_re: e.tensor_tensor(out=u1[:], in0=cim[:], in1=braw_re[:], op=ALU.mult),
              reads=[rtk("cim"), rtk("r"), "bbT_re"], writes=[rtk("s1")])
        S.dve(lambda e, tA=tA, cre=cre, braw_im=braw_im: e.tensor_tensor(out=tA[:], in0=cre[:], in1=braw_im[:], op=ALU.mult),
              reads=[rtk("cre"), rtk("c1"), "bbT_re"], writes=[tAk])
        S.dve(lambda e, tA=tA, u1=u1, bbT_im_f=bbT_im_f: e.tensor_tensor(out=bbT_im_f, in0=tA[:], in1=u1[:], op=ALU.add),
              reads=[tAk, rtk("s1")], writes=["bbT_im"])
        S.barrier()
        A.release(mR)
    cst = A.alloc("cst", [128, 2048], F32)
    S.dma("sp", lambda e: e.dma_start(out=cst[:], in_=craw_re_d[:, :]), writes=["cst"])
    S.dve(lambda e: e.tensor_copy(out=cT_re[:].rearrange("p a b -> p (a b)"), in_=cst[:]), reads=["cst"], writes=["cT_re"])
    S.dma("sp", lambda e: e.dma_start(out=cst[:], in_=craw_im_d[:, :]), reads=["cT_re"], writes=["cst"])
    S.dve(lambda e: e.tensor_scalar(out=cT_imn[:].rearrange("p a b -> p (a b)"), in0=cst[:], scalar1=-1.0, scalar2=None,
                                    op0=ALU.mult), reads=["cst"], writes=["cT_imn"])
    S.barrier()
    A.release(mS)
    sin_bf = A.alloc("sin_bf", [128, 2, L], BF16)
    yacc = A.alloc("yacc", [128, 2, L], F32)

    epsb = A.alloc("epsb", [128, 1], F32)
    S.pool(lambda e: e.memset(epsb[:], EPS), writes=["epsb"])
    mW0 = A.mark()
    xt1_r = Rot(A, "xt1", 4, [128, D], F32)
    xn_r = Rot(A, "xn", 1, [128, 4, D], BF16)
    hT_r = Rot(A, "hTg", 1, [128, KD, GT], BF16)
    junk = A.alloc("junk", [128, D], BF16)
    ss_r = Rot(A, "ss", 2, [128, 4], F32)
    rs_r = Rot(A, "rs", 2, [128, 4], F32)
    mW = A.mark()

    def phase1(s):
        for gi in range(NG):
            xn, xnk = xn_r.next()
            hTg, hTk = hT_r.next()
            ss, ssk = ss_r.next()
            rs, rsk = rs_r.next()
            for i in range(4):
                xt1, xt1k = xt1_r.next()
                S.dma("sp", lambda e, xt1=xt1, gi=gi, i=i: e.dma_start(
                    out=xt1[:], in_=x_d[s, gi * GT + i * 128:gi * GT + (i + 1) * 128, :]), writes=[xt1k])
                S.act(lambda e, xt1=xt1, xn=xn, ss=ss, i=i: e.activation(out=xn[:, i, :], in_=xt1[:], func=AF.Square,
                                                                         accum_out=ss[:, i:i + 1]),
                      reads=[xt1k], writes=[ssk + str(i), xnk + str(i)])
                S.act(lambda e, ss=ss, rs=rs, i=i: e.activation(out=rs[:, i:i + 1], in_=ss[:, i:i + 1], func=AF.Sqrt, bias=epsb[:, 0:1],
                                                               scale=1.0 / D),
                      reads=[ssk + str(i), "epsb"], writes=[rsk + str(i)])
                S.dve(lambda e, rs=rs, i=i: e.reciprocal(out=rs[:, i:i + 1], in_=rs[:, i:i + 1]), reads=[rsk + str(i)], writes=[rsk + str(i)])
                S.dve(lambda e, xt1=xt1, xn=xn, rs=rs, i=i: e.tensor_scalar(out=xn[:, i, :], in0=xt1[:],
                                                                            scalar1=rs[:, i:i + 1], scalar2=None, op0=ALU.mult),
                      reads=[xt1k, rsk + str(i)], writes=[xnk + str(i)])
            if P1STOP < 2:
                continue
            for i in range(4):
                pb = (gi * 4 + i) % 2
                for k in range(KD):
                    S.pe(lambda e, xn=xn, i=i, k=k, pb=pb: e.transpose(out=bankbf(pb)[:, k * 128:(k + 1) * 128],
                                                                        in_=xn[:, i, k * 128:(k + 1) * 128], identity=ident_bf[:]),
                         reads=[xnk + str(i), "ident_bf"], writes=["bank%d" % pb])
                eng = "act" if i % 2 == 0 else "dve"
                if eng == "act":
                    S.act(lambda e, hTg=hTg, i=i, pb=pb: e.activation(
                        out=hTg[:, :, i * 128:(i + 1) * 128], in_=bankbf(pb).rearrange("p (k t) -> p k t", k=KD), func=AF.Copy),
                        reads=["bank%d" % pb], writes=[hTk + str(i)])
                else:
                    S.dve(lambda e, hTg=hTg, i=i, pb=pb: e.tensor_copy(
                        out=hTg[:, :, i * 128:(i + 1) * 128], in_=bankbf(pb).rearrange("p (k t) -> p k t", k=KD)),
                        reads=["bank%d" % pb], writes=[hTk + str(i)])
            hTall = [hTk + str(i) for i in range(4)]
            if P1STOP < 3:
                continue
            for kt in range(2):
                for k in range(KD):
                    S.pe(lambda e, hTg=hTg, kt=kt, k=k: e.matmul(banks[2 + kt][:], lhsT=wins5[:, k, kt * 128:(kt + 1) * 128],
                                                                   rhs=hTg[:, k, :], start=(k == 0), stop=(k == KD - 1)),
                         reads=hTall + ["wins5"], writes=["bank%d" % (2 + kt)])
                if 'noact' in VAR:
                    continue
                if 'onlydve' not in VAR:
                    S.act(lambda e, kt=kt, gi=gi: e.activation(out=sin_bf[:, kt, gi * GT:(gi + 1) * GT], in_=banks[2 + kt][:],
                                                               func=AF.Copy), reads=["bank%d" % (2 + kt)], writes=["sin_bf", "ser%d" % kt])
                if 'nodve' in VAR:
                    continue
                S.dve(lambda e, kt=kt, gi=gi: e.tensor_scalar(out=yacc[:, kt, gi * GT:(gi + 1) * GT], in0=banks[2 + kt][:],
                                                              scalar1=dskip[:, kt:kt + 1], scalar2=None, op0=ALU.mult),
                      reads=["bank%d" % (2 + kt), "dskip"] + (["ser%d" % kt] if 'ser' in VAR else []), writes=["yacc"])
            if P1STOP < 4:
                continue
            S.dma(HTQ, lambda e, hTg=hTg, gi=gi: e.dma_start(
                out=hT_d[s, :, :, gi * GT:(gi + 1) * GT].rearrange("k p t -> p k t"), in_=hTg[:]),
                reads=hTall, writes=["hT_d%d_%d" % (s, gi)])

    A.release(mW0)
    NR = 3
    bu_r = Rot(A, "bu", NR, [128, 2, TB], F32)
    mm_r = Rot(A, "mm", 2, [128, 2, TB], F32)
    xt_r = Rot(A, "xt", NR, [128, 2, TB], F32)
    xb_r = Rot(A, "xb", 2, [128, 4, TB], BF16)
    init = [A.alloc("init%d" % i, [128, 16, 2], F32) for i in range(2)]
    ctmp = A.alloc("ctmp", [128, 16, 2], F32)
    sig_t = [mm_r.t[0][:, 0, 0:GT], mm_r.t[0][:, 1, 0:GT]]
    yg = sin_bf

    def s5(s):
        ulist = []
        for bi in range(NB):
            for d in range(2):
                for j in range(8):
                    ulist.append((bi, d, j, len(ulist)))
        ub = {}

        def upar(u):
            bi, d, j, uc = u
            b = bi if d == 0 else NB - 1 - bi
            return bi, d, j, uc, slice(b * TB, (b + 1) * TB), 6 + d, d * 8 + j, j // 4, (uc % NR) * 2

        def stage1(u):
            bi, d, j, uc, tsl, ybank, dj, kt, bk = upar(u)
            bu, buk = bu_r.next()
            S.pe(lambda e, dj=dj, kt=kt, tsl=tsl, bk=bk: e.matmul(banks[bk][:, 0:TB], lhsT=bbT_re[:, dj, :],
                                                                  rhs=sin_bf[:, kt, tsl], start=True, stop=True),
                 reads=["sin_bf", "bbT_re"], writes=["bank%d" % bk])
            S.pe(lambda e, dj=dj, kt=kt, tsl=tsl, bk=bk: e.matmul(banks[bk + 1][:, 0:TB], lhsT=bbT_im[:, dj, :],
                                                                  rhs=sin_bf[:, kt, tsl], start=True, stop=True),
                 reads=["sin_bf", "bbT_im"], writes=["bank%d" % (bk + 1)])

            def rv(ap, d=d):
                return ap if d == 0 else ap[:, ::-1]
            S.act(lambda e, bu=bu, bk=bk, rv=rv: e.activation(out=bu[:, 0, :], in_=rv(banks[bk][:, 0:TB]), func=AF.Copy),
                  reads=["bank%d" % bk], writes=[buk + "r"])
            S.act(lambda e, bu=bu, bk=bk, rv=rv: e.activation(out=bu[:, 1, :], in_=rv(banks[bk + 1][:, 0:TB]), func=AF.Copy),
                  reads=["bank%d" % (bk + 1)], writes=[buk + "i"])
            ub[uc] = (bu, buk, rv)

        def stage2(u):
            bi, d, j, uc, tsl, ybank, dj, kt, bk = upar(u)
            bu, buk, rv = ub.pop(uc)
            mm, mmk = mm_r.next(); xt, xtk = xt_r.next(); xb, xbk = xb_r.next()
            Cc = cosT[:, dj, :]; Sn = sinT[:, dj, :]
            mods = [
                (lambda e: e.tensor_tensor(out=mm[:, 0, :], in0=bu[:, 0, :], in1=Cc, op=ALU.mult), [buk + "r"], [mmk + "0"]),
                (lambda e: e.tensor_tensor(out=mm[:, 1, :], in0=bu[:, 1, :], in1=Sn, op=ALU.mult), [buk + "i"], [mmk + "1"]),
                (lambda e: e.tensor_tensor(out=mm[:, 0, :], in0=mm[:, 0, :], in1=mm[:, 1, :], op=ALU.add), [mmk + "0", mmk + "1"], [mmk + "0"]),
                (lambda e: e.tensor_tensor(out=mm[:, 1, :], in0=bu[:, 1, :], in1=Cc, op=ALU.mult), [buk + "i", mmk + "1"], [mmk + "1"]),
                (lambda e: e.tensor_tensor(out=bu[:, 0, :], in0=bu[:, 0, :], in1=Sn, op=ALU.mult), [buk + "r"], [buk + "r"]),
                (lambda e: e.tensor_tensor(out=mm[:, 1, :], in0=mm[:, 1, :], in1=bu[:, 0, :], op=ALU.subtract), [mmk + "1", buk + "r"], [mmk + "1"]),
            ]
            for oi, (fn_, rd_, wr_) in enumerate(mods):
                S.any("pool" if oi < S5POOL else "dve", fn_, reads=rd_, writes=wr_)
            rb = rcol[:, dj:dj + 1].to_broadcast([128, TB])
            ini = init[bi % 2]
            inik = "init%d_%d" % (bi % 2, dj)
            if bi == 0:
                i_re = 0.0; i_im = 0.0; ird = []
            else:
                i_re = ini[:, dj, 0:1]; i_im = ini[:, dj, 1:2]; ird = [inik]
            S.dve(lambda e, xt=xt, mm=mm, rb=rb, i_re=i_re: e.tensor_tensor_scan(
                out=xt[:, 0, :], data0=rb, data1=mm[:, 0, :], initial=i_re, op0=ALU.mult, op1=ALU.add),
                reads=[mmk + "0"] + ird, writes=[xtk + "r"])
            S.dve(lambda e, xt=xt, mm=mm, rb=rb, i_im=i_im: e.tensor_tensor_scan(
                out=xt[:, 1, :], data0=rb, data1=mm[:, 1, :], initial=i_im, op0=ALU.mult, op1=ALU.add),
                reads=[mmk + "1"] + ird, writes=[xtk + "i"])
            if bi < NB - 1:
                nin = init[(bi + 1) % 2]
                nk = "init%d_%d" % ((bi + 1) % 2, dj)
                ck = "ctmp%d" % dj
                S.dve(lambda e, xt=xt, dj=dj: e.tensor_scalar(out=ctmp[:, dj, 0:1], in0=xt[:, 0, TB - 1:TB],
                                                              scalar1=pblk_r[:, dj:dj + 1], scalar2=None, op0=ALU.mult),
                      reads=[xtk + "r"], writes=[ck + "a"])
                S.dve(lambda e, xt=xt, dj=dj: e.tensor_scalar(out=ctmp[:, dj, 1:2], in0=xt[:, 1, TB - 1:TB],
                                                              scalar1=pblk_r[:, dj:dj + 1], scalar2=None, op0=ALU.mult),
                      reads=[xtk + "i"], writes=[ck + "b"])
                S.dve(lambda e, xt=xt, dj=dj, nin=nin: e.scalar_tensor_tensor(
                    out=nin[:, dj, 0:1], in0=xt[:, 1, TB - 1:TB], scalar=pblk_in[:, dj:dj + 1], in1=ctmp[:, dj, 0:1],
                    op0=ALU.mult, op1=ALU.add), reads=[xtk + "i", ck + "a"], writes=[nk])
                S.dve(lambda e, xt=xt, dj=dj, nin=nin: e.scalar_tensor_tensor(
                    out=nin[:, dj, 1:2], in0=xt[:, 0, TB - 1:TB], scalar=pblk_i[:, dj:dj + 1], in1=ctmp[:, dj, 1:2],
                    op0=ALU.mult, op1=ALU.add), reads=[xtk + "r", ck + "b"], writes=[nk])
            S.dve(lambda e: e.tensor_tensor(out=rv(xb[:, 0, :]), in0=xt[:, 0, :], in1=Cc, op=ALU.mult), reads=[xtk + "r"], writes=[xbk + "0"])
            S.dve(lambda e: e.scalar_tensor_tensor(out=rv(xb[:, 1, :]), in0=xt[:, 1, :], scalar=-1.0, in1=Sn, op0=ALU.mult, op1=ALU.mult),
                  reads=[xtk + "i"], writes=[xbk + "1"])
            S.dve(lambda e: e.tensor_tensor(out=rv(xb[:, 2, :]), in0=xt[:, 0, :], in1=Sn, op=ALU.mult), reads=[xtk + "r"], writes=[xbk + "2"])
            S.dve(lambda e: e.tensor_tensor(out=rv(xb[:, 3, :]), in0=xt[:, 1, :], in1=Cc, op=ALU.mult), reads=[xtk + "i"], writes=[xbk + "3"])
            for pi_x, lt in enumerate((cT_re, cT_re, cT_imn, cT_imn)):
                S.pe(lambda e, pi_x=pi_x, lt=lt: e.matmul(banks[ybank][:, 0:TB], lhsT=lt[:, dj, :], rhs=xb[:, pi_x, :],
                                                          start=(j % 4 == 0 and pi_x == 0), stop=(j % 4 == 3 and pi_x == 3)),
                     reads=[xbk + str(pi_x), "cT_re", "cT_imn"], writes=["bank%d" % ybank])
            if j % 4 == 3:
                S.dve(lambda e, kt=kt, tsl=tsl, ybank=ybank: e.tensor_tensor(out=yacc[:, kt, tsl], in0=yacc[:, kt, tsl],
                                                                             in1=banks[ybank][:, 0:TB], op=ALU.add),
                      reads=["bank%d" % ybank, "yacc"], writes=["yacc"])

        for u in ulist[:2]:
            stage1(u)
        for i_, u in enumerate(ulist):
            if i_ + 2 < len(ulist):
                stage1(ulist[i_ + 2])
            stage2(u)
        S.barrier()
        sgi = 0
        for gi in range(NG):
            gsl = slice(gi * GT, (gi + 1) * GT)
            for kt in range(2):
                S.act(lambda e, kt=kt, gsl=gsl: e.activation(out=yg[:, kt, gsl], in_=yacc[:, kt, gsl], func=AF.Gelu),
                      reads=["yacc"], writes=["yg%d" % gi, "sin_bf"])
            for m in range(2):
                pb = m
                for kt in range(2):
                    S.pe(lambda e, m=m, kt=kt, gsl=gsl, pb=pb: e.matmul(banks[pb][:], lhsT=wglu[:, kt, m * 128:(m + 1) * 128],
                                                                        rhs=yg[:, kt, gsl], start=(kt == 0), stop=(kt == 1)),
                         reads=["yg%d" % gi, "wglu"], writes=["bank%d" % pb])
                sg = sig_t[sgi % 2]; sgk = "sigt%d" % (sgi % 2)
                sgi += 1
                S.act(lambda e, sg=sg, m=m, pb=pb: e.activation(out=sg, in_=banks[pb][:], func=AF.Sigmoid, bias=bglu[:, m:m + 1]),
                      reads=["bank%d" % pb, "bglu"], writes=[sgk])
                S.dve(lambda e, sg=sg, m=m, gsl=gsl: e.tensor_tensor(out=brbT[s][:, m, gsl], in0=yg[:, m, gsl], in1=sg, op=ALU.mult),
                      reads=[sgk, "yg%d" % gi], writes=["brbT%d" % s])

    for s in range(NS):
        if upto >= 1:
            phase1(s)
        S.barrier()
        if upto >= 2:
            s5(s)
        S.barrier()
    if dbg:
        d_sin = dout("dbg_yacc", [NS, 2, 128, L])
        d_brb = dout("dbg_brb", [NS, 2, 128, L], BF16)
        S.dma("sp", lambda e: e.dma_start(out=d_sin[NS - 1].rearrange("k p t -> p k t"), in_=yacc[:]), reads=["yacc"], writes=["dbg1"])
        for s in range(NS):
            S.dma("sp", lambda e, s=s: e.dma_start(out=d_brb[s].rearrange("k p t -> p k t"), in_=brbT[s][:]),
                  reads=["brbT%d" % s], writes=["dbg2%d" % s])
        d_tab = dout("dbg_cos", [128, 16, TB])
        S.dma("sp", lambda e: e.dma_start(out=d_tab[:, :, :], in_=cosT[:]), reads=["cosT"], writes=["dbg3"])
        d_bb = dout("dbg_bb", [128, 16, 128], BF16)
        S.dma("sp", lambda e: e.dma_start(out=d_bb[:, :, :], in_=bbT_re[:]), reads=["bbT_re"], writes=["dbg4"])
    S.barrier()
    A.release(mA)
    if upto <= 2:
        zt = A.alloc("zt", [128, D], F32)
        S.pool(lambda e: e.memset(zt[:], 0.0), writes=["zt"])
        for t in range(NTOK // 128):
            S.dma("sp", lambda e, t=t: e.dma_start(out=out_d[t * 128:(t + 1) * 128, :], in_=zt[:]), reads=["zt"], writes=["o%d" % t])
        S.emit()
        return nc, S, A

    HX = D + 32
    h2x_d = dscr("h2x_scr", [NTOK, D], BF16)
    pr_d = dscr("pr_scr", [NTOK, NE], F32)
    scoresT = A.alloc("scoresT", [64, L], F32)
    mB = A.mark()
    win_bf = A.alloc("win_bf", [128, KD, PT], BF16)
    wupa = A.alloc("wupa", [128, 4, D], BF16)
    wupb = A.alloc("wupb", [128, 2, D], BF16)
    wout = A.alloc("wout", [128, KD, D], BF16)
    wsT = A.alloc("wsT", [128, 8, 128], BF16)
    wr = A.alloc("wr", [128, KD, NE], BF16)
    lng = A.alloc("lng", [128, GW], F32)
    lnb = A.alloc("lnb", [128, GW], F32)
    bstab = A.alloc("bstab", [128, GW], F32)
    bgate = A.alloc("bgate", [128, 16], F32)
    g2 = A.alloc("g2", [128, KD], F32)
    g1b = A.alloc("g1b", [128, KD], F32)
    epsb2 = A.alloc("epsb2", [128, 1], F32)
    S.pool(lambda e: e.memset(epsb2[:], EPS), writes=["epsb2"])
    S.pool(lambda e: e.memset(scoresT[:], 0.0), writes=["scoresT"])
    for (t_, d_, nm) in ((lng, lng_d, "lng"), (lnb, lnb_d, "lnb"), (bstab, bstab_d, "bstab"), (bgate, bgate_d, "bgate"),
                         (g2, g2_d, "g2"), (g1b, g1_d, "g1b")):
        S.dma("sp", lambda e, t_=t_, d_=d_: e.dma_start(out=t_[:], in_=d_[:, :]), writes=[nm])
    mBs = A.mark()
    stgB = Rot(A, "stgB", 2, [128, PT], F32)
    for k in range(KD):
        st_, stk = stgB.next()
        S.dma("sp", lambda e, st_=st_, k=k: e.dma_start(out=st_[:], in_=win_d[k * 128:(k + 1) * 128, :]), writes=[stk])
        S.dve(lambda e, st_=st_, k=k: e.tensor_scalar(out=win_bf[:, k, :], in0=st_[:], scalar1=g1b[:, k:k + 1], scalar2=None,
                                                      op0=ALU.mult), reads=[stk, "g1b"], writes=["win_bf"])
    def load_cast(dst2d, src2d, ncol, nm, eng):
        st_, stk = stgB.next()
        S.dma("sp", lambda e: e.dma_start(out=st_[:, 0:ncol], in_=src2d), writes=[stk])
        if eng == "act":
            S.act(lambda e: e.activation(out=dst2d, in_=st_[:, 0:ncol], func=AF.Copy), reads=[stk], writes=[nm])
        else:
            S.dve(lambda e: e.tensor_copy(out=dst2d, in_=st_[:, 0:ncol]), reads=[stk], writes=[nm])
    for k in range(4):
        load_cast(wupa[:, k, :], wupa_d[k * 128:(k + 1) * 128, :], D, "wupa", "act")
    for k in range(2):
        load_cast(wupb[:, k, :], wupb_d[k * 128:(k + 1) * 128, :], D, "wupb", "dve")
    for k in range(KD):
        load_cast(wout[:, k, :], wout_d[k * 128:(k + 1) * 128, :], D, "wout", "act" if k % 2 else "dve")
    load_cast(wsT[:].rearrange("p a b -> p (a b)"), wsT_d[:, :, :].rearrange("p a b -> p (a b)"), 1024, "wsT", "dve")
    for k in range(KD):
        load_cast(wr[:, k, :], wr_d[k * 128:(k + 1) * 128, :], NE, "wr", "dve")
    S.barrier()
    A.release(mBs)

    hTg_r = Rot(A, "hTgB", HTGB, [128, KD, GT], BF16)
    xt_r2 = Rot(A, "xtB", 1, [128, D], F32)
    u_r = Rot(A, "u_sb", 3, [128, GW], F32)
    v_r = Rot(A, "v_sb", 3, [128, GW], F32)
    vn_r = Rot(A, "vn", 3, [128, GW], BF16)
    bra_r = Rot(A, "bra", 2, [128, GW], BF16)
    braT_r = Rot(A, "braT", 1, [128, 4, GT], BF16)
    sg_r = Rot(A, "sgB", 4, [128, GT], F32)
    mrg_r = Rot(A, "mrg", 1, [128, KD, GT], BF16)
    x1_r = Rot(A, "x1", 2, [128, D], F32)
    hn_r = Rot(A, "hn2", 3, [128, D], BF16)
    pr_r = Rot(A, "prB", 4, [128, NE], F32)
    h2T_r = Rot(A, "h2T", 1, [128, KD, 128], BF16)
    st_r = Rot(A, "stB", 8, [128, 8], F32)
    ex_r = Rot(A, "exB", 2, [128, NE], F32)
    pw_r = [[A.alloc("pw%d_%d" % (s, i), [128, 32 * NS], F32) for i in range(2)] for s in range(NS)]
    for s in range(NS):
        for i in range(2):
            S.pool(lambda e, s=s, i=i: e.memset(pw_r[s][i][:], 0.0), writes=["pw%d_%d" % (s, i)])

    hT_next = {}

    def phaseB(s):
        for gi in range(NG):
            phaseB_group(s, gi)

    def phaseB_group(s, gi):
        if True:
            def load_hT(s_, gi_):
                t_, k_ = hTg_r.next()
                S.dma("sp", lambda e: e.dma_start(
                    out=t_[:], in_=hT_d[s_, :, :, gi_ * GT:(gi_ + 1) * GT].rearrange("k p t -> p k t")),
                    reads=["hT_d%d_%d" % (s_, gi_)], writes=[k_])
                return t_, k_
            if (s, gi) in hT_next:
                hTg, hTk = hT_next.pop((s, gi))
            else:
                hTg, hTk = load_hT(s, gi)
            braT, braTk = braT_r.next()
            mrg, mrgk = mrg_r.next()
            tb = {}

            def gm1(i):
                tsl = slice(i * 128, (i + 1) * 128)
                u_sb, uk = u_r.next(); v_sb, vk = v_r.next(); vn, vnk = vn_r.next(); stt, stk = st_r.next()
                vh, vhk = v_sb, vk
                for (bk_, c0) in ((0, 0), (1, GW)):
                    for k in range(KD):
                        S.pe(lambda e, k=k, bk_=bk_, c0=c0: e.matmul(
                            banks[bk_][:], lhsT=hTg[:, k, tsl], rhs=win_bf[:, k, c0:c0 + GW], start=(k == 0), stop=(k == KD - 1)),
                            reads=[hTk, "win_bf"], writes=["bank%d" % bk_])
                S.act(lambda e: e.activation(out=u_sb[:], in_=banks[0][:], func=AF.Gelu), reads=["bank0"], writes=[uk])
                S.act(lambda e: e.activation(out=v_sb[:], in_=banks[1][:], func=AF.Gelu, accum_out=stt[:, 0:1]),
                      reads=["bank1"], writes=[vk, stk + "a"])
                S.dve(lambda e: e.tensor_scalar(out=stt[:, 1:2], in0=stt[:, 0:1], scalar1=-1.0 / GW, scalar2=None, op0=ALU.mult),
                      reads=[stk + "a"], writes=[stk + "b"])
                S.act(lambda e: e.activation(out=vn[:], in_=v_sb[:], func=AF.Square, bias=stt[:, 1:2],
                                             accum_out=stt[:, 2:3]), reads=[vk, stk + "b"], writes=[stk + "c", vnk])
                S.act(lambda e: e.activation(out=stt[:, 3:4], in_=stt[:, 2:3], func=AF.Sqrt, bias=epsb2[:, 0:1], scale=1.0 / GW),
                      reads=[stk + "c", "epsb2"], writes=[stk + "d"])
                S.dve(lambda e: e.reciprocal(out=stt[:, 4:5], in_=stt[:, 3:4]), reads=[stk + "d"], writes=[stk + "e"])
                S.dve(lambda e: e.tensor_scalar(out=vh[:], in0=v_sb[:], scalar1=stt[:, 1:2], scalar2=stt[:, 4:5],
                                                op0=ALU.add, op1=ALU.mult),
                      reads=[vk, stk + "b", stk + "e"], writes=[vhk])
                S.pool(lambda e: e.tensor_tensor(out=vh[:], in0=vh[:], in1=lng[:], op=ALU.mult), reads=[vhk, "lng"], writes=[vhk])
                S.dve(lambda e: e.tensor_tensor(out=vn[:], in0=vh[:], in1=lnb[:], op=ALU.add), reads=[vhk, "lnb"], writes=[vnk])
                tb[("g", i)] = (tsl, u_sb, uk, v_sb, vk, vn, vnk)

            def gm2(i):
                tsl, u_sb, uk, v_sb, vk, vn, vnk = tb.pop(("g", i))
                zb, zbk = v_sb, vk
                bra, brak = bra_r.next()
                for g in range(8):
                    S.pe(lambda e, g=g: e.matmul(banks[2][:, g * 64:(g + 1) * 64], lhsT=wsT[:, g, :], rhs=vn[:, g * 64:(g + 1) * 64],
                                                 start=True, stop=True), reads=[vnk, "wsT"], writes=["bank2"])
                S.dve(lambda e: e.tensor_tensor(out=zb[:], in0=banks[2][:], in1=bstab[:], op=ALU.add), reads=["bank2", "bstab"],
                      writes=[zbk])
                S.dve(lambda e: e.tensor_tensor(out=bra[:], in0=zb[:], in1=u_sb[:], op=ALU.mult),
                      reads=[zbk, uk], writes=[brak])
                tb[("t", i)] = (tsl, bra, brak)

            def gm3(i):
                tsl, bra, brak = tb.pop(("t", i))
                for k in range(4):
                    S.pe(lambda e, k=k: e.transpose(out=bankbf(3)[:, k * 128:(k + 1) * 128], in_=bra[:, k * 128:(k + 1) * 128],
                                                    identity=ident_bf[:]), reads=[brak, "ident_bf"], writes=["bank3"])
                S.act(lambda e: e.activation(out=braT[:, :, tsl], in_=bankbf(3)[:, 0:512].rearrange("p (k t) -> p k t", k=4),
                                             func=AF.Copy), reads=["bank3"], writes=[braTk + str(i)])

            gm1(0); gm1(1); gm1(2); gm2(0); gm1(3); gm3(0); gm2(1); gm3(1); gm2(2); gm3(2); gm2(3); gm3(3)
            braTall = [braTk + str(i) for i in range(4)]
            gsl = slice(gi * GT, (gi + 1) * GT)
            for m in range(KD):
                fs = slice(m * 128, (m + 1) * 128)
                bA, bGa, bB, bGb = (4, 5, 6, 7) if m % 2 == 0 else (0, 1, 2, 3)
                for k in range(4):
                    S.pe(lambda e, k=k, fs=fs, bA=bA: e.matmul(banks[bA][:], lhsT=wupa[:, k, fs], rhs=braT[:, k, :], start=(k == 0), stop=(k == 3)),
                         reads=braTall + ["wupa"], writes=["bank%d" % bA])
                for k in range(KD):
                    S.pe(lambda e, k=k, m=m, bGa=bGa: e.matmul(banks[bGa][:], lhsT=win_bf[:, k, 1280 + m * 128:1280 + (m + 1) * 128], rhs=hTg[:, k, :],
                                                               start=(k == 0), stop=(k == KD - 1)), reads=[hTk, "win_bf"], writes=["bank%d" % bGa])
                for k in range(2):
                    S.pe(lambda e, k=k, fs=fs, bB=bB: e.matmul(banks[bB][:], lhsT=wupb[:, k, fs], rhs=brbT[s][:, k, gsl], start=(k == 0), stop=(k == 1)),
                         reads=["wupb"], writes=["bank%d" % bB])
                for k in range(KD):
                    S.pe(lambda e, k=k, m=m, bGb=bGb: e.matmul(banks[bGb][:], lhsT=win_bf[:, k, 2304 + m * 128:2304 + (m + 1) * 128], rhs=hTg[:, k, :],
                                                               start=(k == 0), stop=(k == KD - 1)), reads=[hTk, "win_bf"], writes=["bank%d" % bGb])
                sga, sgak = sg_r.next(); sgb, sgbk = sg_r.next()
                S.act(lambda e, sga=sga, m=m, bGa=bGa: e.activation(out=sga[:], in_=banks[bGa][:], func=AF.Sigmoid, bias=bgate[:, m:m + 1]),
                      reads=["bank%d" % bGa, "bgate"], writes=[sgak])
                S.act(lambda e, sgb=sgb, m=m, bGb=bGb: e.activation(out=sgb[:], in_=banks[bGb][:], func=AF.Sigmoid, bias=bgate[:, 8 + m:9 + m]),
                      reads=["bank%d" % bGb, "bgate"], writes=[sgbk])
                S.dve(lambda e, sga=sga, bA=bA: e.tensor_tensor(out=sga[:], in0=sga[:], in1=banks[bA][:], op=ALU.mult), reads=[sgak, "bank%d" % bA], writes=[sgak])
                S.dve(lambda e, sgb=sgb, bB=bB: e.tensor_tensor(out=sgb[:], in0=sgb[:], in1=banks[bB][:], op=ALU.mult), reads=[sgbk, "bank%d" % bB], writes=[sgbk])
                S.pool(lambda e, sga=sga, sgb=sgb, m=m: e.tensor_tensor(out=mrg[:, m, :], in0=sga[:], in1=sgb[:], op=ALU.add),
                       reads=[sgak, sgbk], writes=[mrgk + str(m)])
            mrgall = [mrgk + str(m) for m in range(KD)]
            nxt = (s, gi + 1) if gi + 1 < NG else ((s + 1, 0) if s + 1 < NS else None)
            if nxt is not None:
                hT_next[nxt] = load_hT(*nxt)

            def wa(i):
                tsl = slice(i * 128, (i + 1) * 128)
                tok0 = s * L + gi * GT + i * 128
                xt, xtk = xt_r2.next(); x1, x1k = x1_r.next(); hn, hnk = hn_r.next(); stt, stk = st_r.next()
                S.dma("sp", lambda e: e.dma_start(out=xt[:], in_=x_d[s, gi * GT + i * 128:gi * GT + (i + 1) * 128, :]), writes=[xtk])
                for hh in range(2):
                    for m in range(KD):
                        S.pe(lambda e, m=m, hh=hh: e.matmul(banks[hh][:], lhsT=mrg[:, m, tsl], rhs=wout[:, m, hh * 512:(hh + 1) * 512],
                                                            start=(m == 0), stop=(m == KD - 1)),
                             reads=mrgall + ["wout"], writes=["bank%d" % hh])
                    S.dve(lambda e, hh=hh: e.tensor_tensor(out=x1[:, hh * 512:(hh + 1) * 512], in0=xt[:, hh * 512:(hh + 1) * 512],
                                                           in1=banks[hh][:], op=ALU.add),
                          reads=[xtk, "bank%d" % hh], writes=[x1k + str(hh)])
                x1all = [x1k + "0", x1k + "1"]
                S.dma(STQ, lambda e: e.dma_start(out=acc_d[tok0:tok0 + 128, :], in_=x1[:]), reads=x1all,
                      writes=["acc_d%d" % (tok0 // 128)])
                S.act(lambda e: e.activation(out=hn[:, 0:D], in_=x1[:], func=AF.Square, accum_out=stt[:, 0:1]),
                      reads=x1all, writes=[stk + "a", hnk + "h"])
                S.act(lambda e: e.activation(out=stt[:, 1:2], in_=stt[:, 0:1], func=AF.Sqrt, bias=epsb2[:, 0:1], scale=1.0 / D),
                      reads=[stk + "a", "epsb2"], writes=[stk + "b"])
                S.dve(lambda e: e.reciprocal(out=stt[:, 2:3], in_=stt[:, 1:2]), reads=[stk + "b"], writes=[stk + "c"])
                S.dve(lambda e: e.tensor_scalar(out=hn[:, 0:D], in0=x1[:], scalar1=stt[:, 2:3], scalar2=None, op0=ALU.mult),
                      reads=x1all + [stk + "c"], writes=[hnk + "h"])
                S.dma(STQ, lambda e: e.dma_start(out=h2x_d[tok0:tok0 + 128, :], in_=hn[:, 0:D]), reads=[hnk + "h"],
                      writes=["h2x_dh%d" % (tok0 // 128)])
                tb[("w", i)] = (tok0, hn, hnk, stt, stk)

            def wb(i):
                tok0, hn, hnk, stt, stk = tb[("w", i)]
                h2T, h2Tk = h2T_r.next(); ex, exk = ex_r.next()
                for k in range(KD):
                    S.pe(lambda e, k=k: e.transpose(out=bankbf(2)[:, k * 128:(k + 1) * 128], in_=hn[:, k * 128:(k + 1) * 128],
                                                    identity=ident_bf[:]), reads=[hnk + "h", "ident_bf"], writes=["bank2"])
                S.dve(lambda e: e.tensor_tensor(out=h2T[:], in0=bankbf(2).rearrange("p (k t) -> p k t", k=KD),
                                                in1=g2[:, :].unsqueeze(2).to_broadcast([128, KD, 128]), op=ALU.mult),
                      reads=["bank2", "g2"], writes=[h2Tk])
                tb[("l", i)] = (h2T, h2Tk, ex, exk)

            def wl(i):
                tok0, hn, hnk, stt, stk = tb[("w", i)]
                h2T, h2Tk, ex, exk = tb.pop(("l", i))
                for k in range(KD):
                    S.pe(lambda e, k=k: e.matmul(banks[3][:, 0:NE], lhsT=h2T[:, k, :], rhs=wr[:, k, :], start=(k == 0), stop=(k == KD - 1)),
                         reads=[h2Tk, "wr"], writes=["bank3"])
                S.dve(lambda e: e.tensor_reduce(out=stt[:, 3:4], in_=banks[3][:, 0:NE], axis=AX.X, op=ALU.max), reads=["bank3"],
                      writes=[stk + "d"])
                S.dve(lambda e: e.tensor_scalar(out=stt[:, 4:5], in0=stt[:, 3:4], scalar1=-1.0, scalar2=None, op0=ALU.mult),
                      reads=[stk + "d"], writes=[stk + "e"])
                S.act(lambda e: e.activation(out=ex[:], in_=banks[3][:, 0:NE], func=AF.Exp, bias=stt[:, 4:5], accum_out=stt[:, 5:6]),
                      reads=["bank3", stk + "e"], writes=[exk, stk + "f"])
                S.dve(lambda e: e.reciprocal(out=stt[:, 6:7], in_=stt[:, 5:6]), reads=[stk + "f"], writes=[stk + "g"])
                prt, prk = pr_r.next()
                pview = prt[:, :]
                S.dve(lambda e: e.tensor_scalar(out=pview, in0=ex[:], scalar1=stt[:, 6:7], scalar2=None, op0=ALU.mult),
                      reads=[exk, stk + "g"], writes=[prk])
                S.dma(STQ, lambda e: e.dma_start(out=pr_d[tok0:tok0 + 128, :], in_=prt[:]), reads=[prk],
                      writes=["pr_d%d" % (tok0 // 128)])
                pwt = pw_r[s][i % 2]; pwk = "pw%d_%d" % (s, i % 2)
                S.dve(lambda e: e.tensor_copy(out=pwt[:, 32 * s:32 * s + NE], in_=pview), reads=[prk], writes=[pwk])
                tb[("p", i)] = (pwt, pwk)

            def wc(i):
                nt = gi * 4 + i
                pwt, pwk = tb.pop(("p", i))
                S.pe(lambda e: e.transpose(out=banks[3][0:32 * NS, 128:256], in_=pwt[:], identity=ident_f[:]), reads=[pwk, "ident_f"],
                     writes=["bank3"])
                S.act(lambda e: e.activation(out=scoresT[32 * s:32 * s + NE, nt * 128:(nt + 1) * 128],
                                             in_=banks[3][32 * s:32 * s + NE, 128:256], func=AF.Copy), reads=["bank3"], writes=["scoresT"])

            wa(0); wa(1); wa(2); wb(0); wa(3); wl(0); wb(1); wc(0); wl(1); wb(2); wc(1); wl(2); wb(3); wc(2); wl(3); wc(3)
            if dbg and s == 0 and gi == 0:
                d_bra = dout("dbg_braT", [4, 128, GT], BF16)
                d_mrg = dout("dbg_mrg", [KD, 128, GT], BF16)
                S.dma("sp", lambda e, braT=braT: e.dma_start(out=d_bra.rearrange("k p t -> p k t"), in_=braT[:]), reads=braTall, writes=["dbgb1"])
                S.dma("sp", lambda e, mrg=mrg: e.dma_start(out=d_mrg.rearrange("k p t -> p k t"), in_=mrg[:]), reads=mrgall, writes=["dbgb2"])

    for s in range(NS):
        phaseB(s)
    S.barrier()
    A.release(mB)
    if dbg:
        d_sc = dout("dbg_scores", [64, L])
        S.dma("sp", lambda e: e.dma_start(out=d_sc[:, :], in_=scoresT[:]), reads=["scoresT"], writes=["dbg5"])
        S.barrier()
    if upto <= 3:
        xo_r = Rot(A, "xo", 2, [128, D], F32)
        for t in range(NTOK // 128):
            xo, xok = xo_r.next()
            S.dma("sp", lambda e, t=t, xo=xo: e.dma_start(out=xo[:], in_=acc_d[t * 128:(t + 1) * 128, :]), writes=[xok])
            S.dma("sp", lambda e, t=t, xo=xo: e.dma_start(out=out_d[t * 128:(t + 1) * 128, :], in_=xo[:]), reads=[xok], writes=["o%d" % t])
        S.emit()
        return nc, S, A

    NR_ = 64
    mC = A.mark()
    lo = A.alloc("lo", [NR_, 1], F32); hi = A.alloc("hi", [NR_, 1], F32); mid = A.alloc("mid", [NR_, 1], F32)
    cnt = A.alloc("cnt", [NR_, 1], F32); flg = A.alloc("flg", [NR_, 1], F32); dlt = A.alloc("dlt", [NR_, 1], F32)
    onec = A.alloc("onec", [NR_, 1], F32)
    junkC = A.alloc("junkC", [NR_, L], F32)
    csum = A.alloc("csum", [NR_, L], F32)
    csT = A.alloc("csT", [128, NT, NR_], F32)
    iota_c = A.alloc("iota_c", [128, CAP], I16)
    ones_bf = A.alloc("ones_bf", [128, 1], BF16)
    idx_f = A.alloc("idx_f", [128, NR_ * NQ], F32)
    le_r = Rot(A, "LE", 4, [128, CAP], BF16)
    S.pool(lambda e: e.memset(lo[:], 0.0), writes=["lo"])
    S.pool(lambda e: e.memset(hi[:], 1.0), writes=["hi"])
    S.pool(lambda e: e.memset(onec[:], 1.0), writes=["onec"])
    S.pool(lambda e: e.memset(ones_bf[:], 1.0), writes=["ones_bf"])
    S.pool(lambda e: e.iota(iota_c[:], pattern=[[1, CAP]], base=0, channel_multiplier=0, allow_small_or_imprecise_dtypes=True),
           writes=["iota_c"])
    for it in range(30):
        S.dve(lambda e: e.tensor_tensor(out=mid[:], in0=lo[:], in1=hi[:], op=ALU.add), reads=["lo", "hi"], writes=["mid"])
        S.dve(lambda e: e.tensor_scalar(out=mid[:], in0=mid[:], scalar1=0.5, scalar2=None, op0=ALU.mult), reads=["mid"], writes=["mid"])
        S.dve(lambda e: e.tensor_scalar(out=junkC[:], in0=scoresT[:], scalar1=mid[:, 0:1], scalar2=None, op0=ALU.is_gt, op1=ALU.add,
                                        accum_out=cnt[:, 0:1]), reads=["scoresT", "mid"], writes=["cnt", "junkC"])
        S.dve(lambda e: e.tensor_scalar(out=flg[:], in0=cnt[:], scalar1=float(CAP), scalar2=None, op0=ALU.is_ge), reads=["cnt"], writes=["flg"])
        S.dve(lambda e: e.tensor_tensor(out=dlt[:], in0=mid[:], in1=lo[:], op=ALU.subtract), reads=["mid", "lo"], writes=["dlt"])
        S.dve(lambda e: e.scalar_tensor_tensor(out=lo[:], in0=dlt[:], scalar=flg[:, 0:1], in1=lo[:], op0=ALU.mult, op1=ALU.add),
              reads=["dlt", "flg", "lo"], writes=["lo"])
        S.dve(lambda e: e.tensor_tensor(out=dlt[:], in0=hi[:], in1=mid[:], op=ALU.subtract), reads=["mid", "hi", "lo"], writes=["dlt"])
        S.dve(lambda e: e.scalar_tensor_tensor(out=hi[:], in0=dlt[:], scalar=flg[:, 0:1], in1=mid[:], op0=ALU.mult, op1=ALU.add),
              reads=["dlt", "flg", "mid"], writes=["hi"])
    S.dve(lambda e: e.tensor_scalar(out=junkC[:], in0=scoresT[:], scalar1=lo[:, 0:1], scalar2=None, op0=ALU.is_gt), reads=["scoresT", "lo"],
          writes=["junkC"])
    S.dve(lambda e: e.tensor_tensor_scan(out=csum[:], data0=onec[:, 0:1].to_broadcast([NR_, L]), data1=junkC[:], initial=0.0,
                                         op0=ALU.mult, op1=ALU.add), reads=["junkC", "onec"], writes=["csum"])
    for n0 in range(0, NT, 8):
        nn = min(8, NT - n0)
        pb = (n0 // 8) % 2
        for n in range(n0, n0 + nn):
            S.pe(lambda e, n=n, n0=n0, pb=pb: e.transpose(out=banks[pb][:, (n - n0) * NR_:(n - n0 + 1) * NR_], in_=csum[0:NR_, n * 128:(n + 1) * 128],
                                                          identity=ident_f[0:NR_, 0:NR_]), reads=["csum", "ident_f"], writes=["bank%d" % pb])
        S.act(lambda e, n0=n0, nn=nn, pb=pb: e.activation(out=csT[:, n0:n0 + nn, :], in_=banks[pb][:, 0:nn * NR_].rearrange("p (a b) -> p a b", b=NR_),
                                                          func=AF.Copy), reads=["bank%d" % pb], writes=["csT"])
    NRL = NS * NE
    assert NRL <= 32 and CAP <= 512
    onehot = A.alloc("onehot", [128, 32, 32], BF16)
    idxrow = A.alloc("idxrow", [32, CAP], F32)
    S.pool(lambda e: e.memset(onehot[:], 0.0), writes=["onehot"])
    for rl in range(NRL):
        S.pool(lambda e, rl=rl: e.memset(onehot[:, rl, rl:rl + 1], 1.0), reads=["onehot"], writes=["onehot"])
    S.dve(lambda e: e.memset(idx_f[:], 0.0), writes=["idx_f"])
    for rl in range(NRL):
        s_ = rl // NE; e_ = rl % NE
        r = 32 * s_ + e_
        for n in range(NT):
            LE, lek = le_r.next()
            S.dve(lambda e, LE=LE, n=n, r=r: e.tensor_scalar(out=LE[:], in0=iota_c[:], scalar1=csT[:, n, r:r + 1], scalar2=None, op0=ALU.is_ge),
                  reads=["iota_c", "csT"], writes=[lek])
            S.pe(lambda e, LE=LE, rl=rl, n=n: e.matmul(banks[2][0:32, 0:CAP], lhsT=onehot[:, rl, :], rhs=LE[:, 0:CAP],
                                                       start=(rl == 0 and n == 0), stop=(rl == NRL - 1 and n == NT - 1)),
                 reads=[lek, "onehot"], writes=["bank2"])
    S.act(lambda e: e.activation(out=idxrow[:], in_=banks[2][0:32, 0:CAP], func=AF.Copy), reads=["bank2"], writes=["idxrow"])
    for q in range(NQ):
        S.pe(lambda e, q=q: e.transpose(out=banks[3][:, q * 32:(q + 1) * 32], in_=idxrow[0:32, q * 128:(q + 1) * 128],
                                        identity=ident_f[0:32, 0:32]), reads=["idxrow", "ident_f"], writes=["bank3"])
    idx_f3 = idx_f[:].rearrange("p (r q) -> p r q", q=NQ)
    for s in range(NS):
        for q in range(NQ):
            S.dve(lambda e, s=s, q=q: e.tensor_scalar(out=idx_f3[:, 32 * s:32 * s + NE, q],
                                                     in0=banks[3][:, q * 32 + NE * s:q * 32 + NE * s + NE],
                                                     scalar1=float(s * L), scalar2=None, op0=ALU.add),
                  reads=["bank3", "idx_f"], writes=["idx_f"])
    S.dve(lambda e: e.tensor_copy(out=idx_i[:], in_=idx_f[:]), reads=["idx_f"], writes=["idx_i"])
    if dbg:
        d_idx = dout("dbg_idx", [128, NR_ * NQ], I32)
        S.dma("sp", lambda e: e.dma_start(out=d_idx[:, :], in_=idx_i[:]), reads=["idx_i"], writes=["dbg6"])
    S.barrier()
    A.release(m0)

    mD = A.mark()
    g2d = A.alloc("g2d", [128, KD], F32)
    S.dma("sp", lambda e: e.dma_start(out=g2d[:], in_=g2_d[:, :]), writes=["g2d"])
    stg_r = Rot(A, "stgD", 3, [128, KD, 512], F32)
    wg_r = Rot(A, "wg_bf", 2, [128, KD, 512], BF16)
    wu_r = Rot(A, "wu_bf", 2, [128, KD, 512], BF16)
    wd_bf = A.alloc("wd_bf", [128, 16, D], BF16)
    xsT = [A.alloc("xsT%d" % s, [128, KD, CAP], BF16) for s in range(NS)]
    actT = [A.alloc("actT%d" % s, [128, 16, CAP], BF16) for s in range(NS)]
    xsg_r = Rot(A, "xsg", NS * NQ, [128, D], BF16)
    pg_r = Rot(A, "pg", NS * NQ, [128, NE], F32)
    ysb_r = Rot(A, "ysb", 3, [128, D], F32)
    sil_r = Rot(A, "sil", 2, [128, CAP], F32)
    gts2 = [A.alloc("gts%d" % i, [128, NS * NQ], F32) for i in range(2)]
    cast_i = [0]

    def cast(dst, src, reads, writes):
        k = cast_i[0] % 2
        cast_i[0] += 1
        if k == 0:
            S.act(lambda e: e.activation(out=dst, in_=src, func=AF.Copy), reads=reads, writes=writes)
        elif k == 1:
            S.dve(lambda e: e.tensor_copy(out=dst, in_=src), reads=reads, writes=writes)
        else:
            S.pool(lambda e: e.tensor_copy(out=dst, in_=src), reads=reads, writes=writes)

    units = []
    for e_ in range(NE):
        for pc in range(4):
            units.append((e_, "gu", pc))
        units.append((e_, "d", 0))
    ubuf = {}

    def load_unit(u):
        e_, kind, pc = u
        if kind == "gu":
            wg, wgk = wg_r.next(); wu, wuk = wu_r.next()
            ubuf[u] = (wg, wgk, wu, wuk)
            for (w_, wk_, src_d) in ((wg, wgk, wg_d), (wu, wuk, wu_d)):
                st_, stk = stg_r.next()
                S.dma("sp", lambda e, st_=st_, src_d=src_d: e.dma_start(
                    out=st_[:], in_=src_d[e_, :, pc * 512:(pc + 1) * 512].rearrange("(k p) f -> p k f", p=128)), writes=[stk])
                cast(w_[:], st_[:], [stk], [wk_])
        else:
            for fh in range(2):
                for hh in range(2):
                    st_, stk = stg_r.next()
                    S.dma("sp", lambda e, st_=st_, fh=fh, hh=hh: e.dma_start(
                        out=st_[:], in_=wd_d[e_, fh * 1024:(fh + 1) * 1024, hh * 512:(hh + 1) * 512].rearrange("(k p) c -> p k c", p=128)),
                        writes=[stk])
                    cast(wd_bf[:, fh * 8:(fh + 1) * 8, hh * 512:(hh + 1) * 512], st_[:], [stk], ["wd_bf"])

    gbuf = {}

    def gather_dma(e_):
        gts = gts2[e_ % 2]
        for s in range(NS):
            r = 32 * s + e_
            for q in range(NQ):
                col = r * NQ + q
                xsg, xsgk = xsg_r.next()
                gbuf[(e_, s, q)] = (xsg, xsgk)
                S.dma("pool", lambda e, xsg=xsg, col=col: e.indirect_dma_start(
                    out=xsg[:], out_offset=None, in_=h2x_d[:, :], in_offset=bass.IndirectOffsetOnAxis(ap=idx_i[:, col:col + 1], axis=0)),
                    reads=["idx_i"], writes=[xsgk])
                pg, pgk = pg_r.next()
                S.dma("pool", lambda e, pg=pg, col=col: e.indirect_dma_start(
                    out=pg[:], out_offset=None, in_=pr_d[:, :], in_offset=bass.IndirectOffsetOnAxis(ap=idx_i[:, col:col + 1], axis=0)),
                    reads=["idx_i"], writes=[pgk])
                S.dve(lambda e, pg=pg, s=s, q=q, gts=gts: e.tensor_copy(out=gts[:, s * NQ + q:s * NQ + q + 1], in_=pg[:, e_:e_ + 1]),
                      reads=[pgk], writes=["gts%d_%d_%d" % (e_ % 2, s, q)])

    def gather_tr(e_):
        for s in range(NS):
            for q in range(NQ):
                xsg, xsgk = gbuf.pop((e_, s, q))
                pb = 6 + (q % 2)
                for k in range(KD):
                    S.pe(lambda e, xsg=xsg, k=k, pb=pb: e.transpose(out=bankbf(pb)[:, k * 128:(k + 1) * 128], in_=xsg[:, k * 128:(k + 1) * 128],
                                                                   identity=ident_bf[:]), reads=[xsgk, "ident_bf"], writes=["bank%d" % pb])
                S.dve(lambda e, s=s, q=q, pb=pb: e.tensor_tensor(out=xsT[s][:, :, q * 128:(q + 1) * 128],
                                                                 in0=bankbf(pb).rearrange("p (k t) -> p k t", k=KD),
                                                                 in1=g2d[:, :].unsqueeze(2).to_broadcast([128, KD, 128]), op=ALU.mult),
                      reads=["bank%d" % pb, "g2d"], writes=["xsT%d" % s])

    gu_i = [0]

    def compute_unit(u):
        e_, kind, pc = u
        if kind == "gu":
            wg, wgk, wu, wuk = ubuf[u]
            for s in range(NS):
                for ft in range(4):
                    fs = slice(ft * 128, (ft + 1) * 128)
                    ba = gu_i[0] % 2; bu_ = 2 + gu_i[0] % 2
                    gu_i[0] += 1
                    for k in range(KD):
                        S.pe(lambda e, wg=wg, k=k, fs=fs, s=s, ba=ba: e.matmul(banks[ba][:, 0:CAP], lhsT=wg[:, k, fs], rhs=xsT[s][:, k, :],
                                                                                start=(k == 0), stop=(k == KD - 1)),
                             reads=[wgk, "xsT%d" % s], writes=["bank%d" % ba])
                    for k in range(KD):
                        S.pe(lambda e, wu=wu, k=k, fs=fs, s=s, bu_=bu_: e.matmul(banks[bu_][:, 0:CAP], lhsT=wu[:, k, fs], rhs=xsT[s][:, k, :],
                                                                                  start=(k == 0), stop=(k == KD - 1)),
                             reads=[wuk, "xsT%d" % s], writes=["bank%d" % bu_])
                    sil, silk = sil_r.next()
                    S.act(lambda e, sil=sil, ba=ba: e.activation(out=sil[:], in_=banks[ba][:, 0:CAP], func=AF.Silu), reads=["bank%d" % ba], writes=[silk])
                    fc = pc * 4 + ft
                    S.dve(lambda e, sil=sil, bu_=bu_, s=s, fc=fc: e.tensor_tensor(out=actT[s][:, fc, :], in0=sil[:], in1=banks[bu_][:, 0:CAP], op=ALU.mult),
                          reads=[silk, "bank%d" % bu_], writes=["actT%d_%d" % (s, fc)])
        else:
            gts = gts2[e_ % 2]
            for s in range(NS):
                r = 32 * s + e_
                for q in range(NQ):
                    col = r * NQ + q
                    ysb, ysbk = ysb_r.next()
                    for hh in range(2):
                        yb = 4 + hh
                        for fc in range(16):
                            S.pe(lambda e, s=s, q=q, hh=hh, fc=fc, yb=yb: e.matmul(banks[yb][:], lhsT=actT[s][:, fc, q * 128:(q + 1) * 128],
                                                                                   rhs=wd_bf[:, fc, hh * 512:(hh + 1) * 512], start=(fc == 0), stop=(fc == 15)),
                                 reads=["actT%d_%d" % (s, fc), "wd_bf"], writes=["bank%d" % yb])
                        if hh == 0:
                            S.dve(lambda e, ysb=ysb, s=s, q=q, yb=yb, gts=gts: e.tensor_scalar(out=ysb[:, 0:512], in0=banks[yb][:],
                                                                                     scalar1=gts[:, s * NQ + q:s * NQ + q + 1], scalar2=None, op0=ALU.mult),
                                  reads=["bank%d" % yb, "gts%d_%d_%d" % (e_ % 2, s, q)], writes=[ysbk + "0"])
                        else:
                            S.act(lambda e, ysb=ysb, s=s, q=q, yb=yb, gts=gts: e.activation(out=ysb[:, 512:1024], in_=banks[yb][:], func=AF.Copy,
                                                                                  scale=gts[:, s * NQ + q:s * NQ + q + 1]),
                                  reads=["bank%d" % yb, "gts%d_%d_%d" % (e_ % 2, s, q)], writes=[ysbk + "1"])
                    S.dma("pool", lambda e, ysb=ysb, col=col: e.indirect_dma_start(
                        out=acc_d[:, :], out_offset=bass.IndirectOffsetOnAxis(ap=idx_i[:, col:col + 1], axis=0), in_=ysb[:], in_offset=None,
                        compute_op=ALU.add), reads=[ysbk + "0", ysbk + "1", "idx_i"], writes=["acc_all"])

    load_unit(units[0])
    gather_dma(0)
    for ui, u in enumerate(units):
        if u[1] == "gu" and u[2] == 0:
            gather_tr(u[0])
        if u[1] == "gu" and u[2] == 3 and u[0] + 1 < NE:
            gather_dma(u[0] + 1)
        if ui + 1 < len(units):
            load_unit(units[ui + 1])
        compute_unit(u)
    S.barrier()
    A.release(mD)

    gf = A.alloc("gf", [128, D], F32)
    epsb3 = A.alloc("epsb3", [128, 1], F32)
    S.pool(lambda e: e.memset(epsb3[:], EPS), writes=["epsb3"])
    S.dma("sp", lambda e: e.dma_start(out=gf[:], in_=gf_d[:, :]), writes=["gf"])
    xa_r = Rot(A, "xa", 6, [128, D], F32)
    xo_r = Rot(A, "xo", 6, [128, D], F32)
    se_r = Rot(A, "se", 6, [128, 4], F32)
    junkE = A.alloc("junkE", [128, D], BF16)
    for t in range(NTOK // 128):
        xa, xak = xa_r.next(); xo, xok = xo_r.next(); se, sek = se_r.next()
        S.dma("sp", lambda e, t=t, xa=xa: e.dma_start(out=xa[:], in_=acc_d[t * 128:(t + 1) * 128, :]), writes=[xak])
        S.act(lambda e, xa=xa, xo=xo, se=se: e.activation(out=xo[:], in_=xa[:], func=AF.Square, accum_out=se[:, 0:1]), reads=[xak],
              writes=[sek + "a", xok])
        S.act(lambda e, se=se: e.activation(out=se[:, 1:2], in_=se[:, 0:1], func=AF.Sqrt, bias=epsb3[:, 0:1], scale=1.0 / D),
              reads=[sek + "a", "epsb3"], writes=[sek + "b"])
        S.dve(lambda e, se=se: e.reciprocal(out=se[:, 2:3], in_=se[:, 1:2]), reads=[sek + "b"], writes=[sek + "c"])
        S.dve(lambda e, xa=xa, xo=xo, se=se: e.scalar_tensor_tensor(out=xo[:], in0=xa[:], scalar=se[:, 2:3], in1=gf[:], op0=ALU.mult, op1=ALU.mult),
              reads=[xak, sek + "c", "gf"], writes=[xok])
        S.dma(STQ, lambda e, t=t, xo=xo: e.dma_start(out=out_d[t * 128:(t + 1) * 128, :], in_=xo[:]), reads=[xok], writes=["o%d" % t])

    S.emit()
    return nc, S, A


def _f32(a):
    return np.ascontiguousarray(np.asarray(a, dtype=np.float32))


def prep_shared(inp):
    o = {}
    o["g1"] = _f32(inp["norm1_g"][0].reshape(KD, 128).T)
    o["w_in"] = _f32(inp["w_in"][0])
    o["b_gate"] = _f32(inp["b_gate"][0].reshape(16, 128).T)
    o["ln_g"] = _f32(np.broadcast_to(inp["gmlp_ln_g"][0][None, :], (128, GW)))
    o["ln_b"] = _f32(np.broadcast_to(inp["gmlp_ln_b"][0][None, :], (128, GW)))
    o["wsT"] = _f32(np.transpose(inp["gmlp_w_s"][0], (2, 0, 1)))
    o["bstab"] = _f32(np.repeat(inp["gmlp_b_s"][0].T[:, :, None], 64, axis=2).reshape(128, GW))
    lam_re = np.asarray(inp["s5_lam_re"][0]); lam_im = np.asarray(inp["s5_lam_im"][0]); log_dt = np.asarray(inp["s5_log_dt"][0])
    b_re = np.asarray(inp["s5_b_re"][0]); b_im = np.asarray(inp["s5_b_im"][0])
    c_re = np.asarray(inp["s5_c_re"][0]); c_im = np.asarray(inp["s5_c_im"][0])
    def col(a3):
        return _f32(a3.reshape(2, 8, 128).transpose(2, 0, 1).reshape(128, 16))
    o["lamre_c"] = col(lam_re)
    o["lamim_c"] = col(lam_im)
    ldt = np.repeat(log_dt[:, :, None], 64, axis=2)
    o["logdt_c"] = col(ldt)
    def row(a3):
        return _f32(np.broadcast_to(a3.reshape(1, 2048), (128, 2048)))
    o["lamre_r"] = row(lam_re); o["lamim_r"] = row(lam_im); o["logdt_r"] = row(ldt)
    def braw(bm):
        outp = np.zeros((128, 2, 8, 128), np.float32)
        for j in range(8):
            for gg in range(2):
                g = 2 * j + gg
                cl = 16 * (g % 8)
                outp[cl:cl + 16, :, j, gg * 64:(gg + 1) * 64] = np.transpose(bm[:, g, :, :], (2, 0, 1))
        return _f32(outp.reshape(128, 2048))
    o["braw_re"] = braw(b_re); o["braw_im"] = braw(b_im)
    def craw(cm):
        outp = np.zeros((128, 2, 8, 128), np.float32)
        for j in range(8):
            for gg in range(2):
                g = 2 * j + gg
                cl = 16 * (g % 8)
                outp[gg * 64:(gg + 1) * 64, :, j, cl:cl + 16] = np.transpose(cm[:, g, :, :], (2, 0, 1))
        return _f32(outp.reshape(128, 2048))
    o["craw_re"] = craw(c_re); o["craw_im"] = craw(c_im)
    o["dskip"] = _f32(np.asarray(inp["s5_d"][0]).reshape(2, 128).T)
    o["w_glu"] = _f32(inp["s5_w_glu"][0])
    o["b_glu"] = _f32(np.asarray(inp["s5_b_glu"][0]).reshape(2, 128).T)
    o["w_up_a"] = _f32(inp["w_up_a"][0]); o["w_up_b"] = _f32(inp["w_up_b"][0]); o["w_out"] = _f32(inp["w_out"][0])
    o["g2"] = _f32(np.asarray(inp["norm2_g"][0]).reshape(KD, 128).T)
    o["w_router"] = _f32(inp["w_router"][0])
    o["w_gate"] = _f32(inp["w_gate"][0]); o["w_up"] = _f32(inp["w_up"][0]); o["w_down"] = _f32(inp["w_down"][0])
    o["gf"] = _f32(np.broadcast_to(np.asarray(inp["final_g"])[None, :], (128, D)))
    return o


def kernel(**inputs):
    x = np.asarray(inputs["x"], dtype=np.float32)
    B, L, _ = x.shape
    NCORE = 8
    NS = B // NCORE
    shared = prep_shared(inputs)
    nc, S, A = build(NS, L)
    in_maps = []
    for c in range(NCORE):
        m = dict(shared)
        m["x"] = np.ascontiguousarray(x[c * NS:(c + 1) * NS])
        in_maps.append(m)
    res = run_bass_kernel_spmd(nc, in_maps, core_ids=list(range(NCORE)))
    outs = [np.asarray(r["out"]).reshape(NS, L, D) for r in res.results]
    return np.concatenate(outs, axis=0).astype(np.float32)
```
